# Optimizing a Trainium2 kernel written in Bass

```python
import math
import jax, jax.numpy as jnp
from jax import lax
import numpy as np

D_MODEL = 2048
BATCH = 8
SEQ = 2048
DEPTH = 2

DIFF_HEAD_DIM = 128
ATTN_WIDTH = D_MODEL // 2
DIFF_HEADS = ATTN_WIDTH // (2 * DIFF_HEAD_DIM)
QK_WIDTH = DIFF_HEADS * 2 * DIFF_HEAD_DIM
POOL_WINDOWS = (2, 4, 8, 16)
POOL_WIDTH = D_MODEL // 4
POOL_GROUP = POOL_WIDTH // len(POOL_WINDOWS)
CONV_WIDTH = D_MODEL // 4
CONV_TAPS = 3
IN_WIDTH = 2 * QK_WIDTH + ATTN_WIDTH + POOL_WIDTH + 3 * CONV_WIDTH
SPLIT_POINTS = (QK_WIDTH, 2 * QK_WIDTH, 2 * QK_WIDTH + ATTN_WIDTH,
                2 * QK_WIDTH + ATTN_WIDTH + POOL_WIDTH,
                2 * QK_WIDTH + ATTN_WIDTH + POOL_WIDTH + CONV_WIDTH,
                2 * QK_WIDTH + ATTN_WIDTH + POOL_WIDTH + 2 * CONV_WIDTH)
ROPE_THETA = 10000.0
Q_BLOCK = 128
D_FF_DENSE = 5632
N_EXPERTS = 8
TOP_K = 2
D_FF_EXPERT = 7168
N_DENSE = (DEPTH + 1) // 2
N_MOE = DEPTH // 2
EPS = 1e-6
NEG_INF = -1e30

kernel_name = 'hybrid_diffattn_pool_conv_moe'


def rms_norm(x, g):
    xf = x.astype(jnp.float32)
    y = xf * lax.rsqrt(jnp.mean(xf * xf, axis=-1, keepdims=True) + EPS)
    return y.astype(x.dtype) * g


def rotary(x, cos, sin):
    x1, x2 = jnp.split(x, 2, axis=-1)
    return jnp.concatenate([x1 * cos - x2 * sin, x1 * sin + x2 * cos], axis=-1)


def diff_attention(q, k, v, lam):
    B, S, H, _, d = q.shape
    nb = S // Q_BLOCK
    scale = d ** -0.5
    qb = q.reshape(B, nb, Q_BLOCK, H, 2, d).transpose(1, 0, 2, 3, 4, 5)
    key_pos = jnp.arange(S)

    def block(args):
        i, qi = args
        s = jnp.einsum('bqhmd,bkhmd->bhmqk', qi, k).astype(jnp.float32) * scale
        q_pos = i * Q_BLOCK + jnp.arange(Q_BLOCK)
        mask = key_pos[None, :] <= q_pos[:, None]
        p = jax.nn.softmax(jnp.where(mask, s, NEG_INF), axis=-1)
        a = p[:, :, 0] - lam * p[:, :, 1]
        return jnp.einsum('bhqk,bkhe->bqhe', a.astype(v.dtype), v)

    o = lax.map(block, (jnp.arange(nb), qb))
    return o.transpose(1, 0, 2, 3, 4).reshape(B, S, H, 2 * d)


def pool_mixer(u, w_pool, pool_scale):
    B, S, _ = u.shape
    uf = u.astype(jnp.float32).reshape(B, S, len(POOL_WINDOWS), POOL_GROUP)
    outs = []
    for gi, w in enumerate(POOL_WINDOWS):
        g = uf[:, :, gi]
        c = jnp.cumsum(g, axis=1)
        c_shift = jnp.pad(c, ((0, 0), (w, 0), (0, 0)))[:, :S]
        count = jnp.minimum(jnp.arange(S) + 1, w).astype(jnp.float32)
        outs.append((c - c_shift) / count[None, :, None] - g)
    pooled = jnp.stack(outs, axis=2).astype(u.dtype)
    y = jnp.einsum('bsgc,gcd->bsgd', pooled, w_pool)
    return y.reshape(B, S, POOL_WIDTH) * pool_scale


def short_conv_mixer(gate_b, gate_c, h, conv_w):
    S = h.shape[1]
    u = jnp.pad(gate_c * h, ((0, 0), (CONV_TAPS - 1, 0), (0, 0)))
    y = sum(conv_w[j] * u[:, j:j + S] for j in range(CONV_TAPS))
    return gate_b * y


def hybrid_mixer(h, cos, sin, lam_init, w_in, q_g, k_g, lam_vecs, sub_g, w_pool, pool_scale, conv_w, w_out):
    B, S, _ = h.shape
    z = h @ w_in
    q, k, v, u_pool, gate_b, gate_c, u_conv = jnp.split(z, SPLIT_POINTS, axis=-1)
    q = rotary(rms_norm(q.reshape(B, S, DIFF_HEADS, 2, DIFF_HEAD_DIM), q_g), cos, sin)
    k = rotary(rms_norm(k.reshape(B, S, DIFF_HEADS, 2, DIFF_HEAD_DIM), k_g), cos, sin)
    v = v.reshape(B, S, DIFF_HEADS, 2 * DIFF_HEAD_DIM)
    lf = lam_vecs.astype(jnp.float32)
    lam = jnp.exp(jnp.sum(lf[0] * lf[1])) - jnp.exp(jnp.sum(lf[2] * lf[3])) + lam_init
    attn = rms_norm(diff_attention(q, k, v, lam), sub_g) * (1.0 - lam_init)
    attn = attn.reshape(B, S, ATTN_WIDTH)
    pool = pool_mixer(u_pool, w_pool, pool_scale)
    conv = short_conv_mixer(gate_b, gate_c, u_conv, conv_w)
    return jnp.concatenate([attn, pool, conv], axis=-1) @ w_out


def swiglu(h, wg, wu, wd):
    return (jax.nn.silu(h @ wg) * (h @ wu)) @ wd


def moe_swiglu(h, router_w, wg, wu, wd):
    B, S, D = h.shape
    t = h.reshape(B * S, D)
    logits = (t @ router_w).astype(jnp.float32)
    top_vals, top_idx = lax.top_k(logits, TOP_K)
    gates = jax.nn.softmax(top_vals, axis=-1)
    combine = jnp.sum(jax.nn.one_hot(top_idx, N_EXPERTS, dtype=jnp.float32) * gates[..., None], axis=1)
    combine = combine.astype(t.dtype)
    out = jnp.zeros_like(t)
    for e in range(N_EXPERTS):
        out = out + combine[:, e:e + 1] * swiglu(t, wg[e], wu[e], wd[e])
    return out.reshape(B, S, D)


def setup_inputs(seed: int = 0) -> dict:
    key = jax.random.key(seed)
    ks = jax.random.split(key, 20)
    f32 = jnp.float32
    nrm = lambda k, shape, s: jax.random.normal(k, shape, f32) * s
    d = DIFF_HEAD_DIM
    offsets = jax.random.randint(ks[1], (BATCH, 1), 0, 4096, dtype=jnp.int32)
    return {
        'x': nrm(ks[0], (BATCH, SEQ, D_MODEL), 1.0),
        'positions': offsets + jnp.arange(SEQ, dtype=jnp.int32)[None, :],
        'attn_norm_g': 1.0 + nrm(ks[2], (DEPTH, D_MODEL), 0.02),
        'w_in': nrm(ks[3], (DEPTH, D_MODEL, IN_WIDTH), D_MODEL ** -0.5),
        'q_norm_g': 1.0 + nrm(ks[4], (DEPTH, d), 0.02),
        'k_norm_g': 1.0 + nrm(ks[5], (DEPTH, d), 0.02),
        'lambda_vecs': nrm(ks[6], (DEPTH, 4, d), 0.1),
        'attn_out_norm_g': 1.0 + nrm(ks[7], (DEPTH, 2 * d), 0.02),
        'w_pool': nrm(ks[8], (DEPTH, len(POOL_WINDOWS), POOL_GROUP, POOL_GROUP), POOL_GROUP ** -0.5),
        'pool_scale': 1.0 + nrm(ks[9], (DEPTH, POOL_WIDTH), 0.1),
        'conv_w': nrm(ks[10], (DEPTH, CONV_TAPS, CONV_WIDTH), CONV_TAPS ** -0.5),
        'w_out': nrm(ks[11], (DEPTH, D_MODEL, D_MODEL), D_MODEL ** -0.5),
        'ffn_norm_g': 1.0 + nrm(ks[12], (DEPTH, D_MODEL), 0.02),
        'dense_w_gate': nrm(ks[13], (N_DENSE, D_MODEL, D_FF_DENSE), D_MODEL ** -0.5),
        'dense_w_up': nrm(ks[14], (N_DENSE, D_MODEL, D_FF_DENSE), D_MODEL ** -0.5),
        'dense_w_down': nrm(ks[15], (N_DENSE, D_FF_DENSE, D_MODEL), D_FF_DENSE ** -0.5),
        'router_w': nrm(ks[16], (N_MOE, D_MODEL, N_EXPERTS), D_MODEL ** -0.5),
        'moe_w_gate': nrm(ks[17], (N_MOE, N_EXPERTS, D_MODEL, D_FF_EXPERT), D_MODEL ** -0.5),
        'moe_w_up': nrm(ks[18], (N_MOE, N_EXPERTS, D_MODEL, D_FF_EXPERT), D_MODEL ** -0.5),
        'moe_w_down': nrm(ks[19], (N_MOE, N_EXPERTS, D_FF_EXPERT, D_MODEL), D_FF_EXPERT ** -0.5),
    }


def reference(x, positions, attn_norm_g, w_in, q_norm_g, k_norm_g, lambda_vecs, attn_out_norm_g,
              w_pool, pool_scale, conv_w, w_out, ffn_norm_g, dense_w_gate, dense_w_up, dense_w_down,
              router_w, moe_w_gate, moe_w_up, moe_w_down):
    inv_freq = 1.0 / (ROPE_THETA ** (jnp.arange(0, DIFF_HEAD_DIM, 2, dtype=jnp.float32) / DIFF_HEAD_DIM))
    ang = positions.astype(jnp.float32)[..., None] * inv_freq
    cos = jnp.cos(ang).astype(x.dtype)[:, :, None, None, :]
    sin = jnp.sin(ang).astype(x.dtype)[:, :, None, None, :]
    for l in range(DEPTH):
        lam_init = 0.8 - 0.6 * math.exp(-0.3 * l)
        h = rms_norm(x, attn_norm_g[l])
        x = x + hybrid_mixer(h, cos, sin, lam_init, w_in[l], q_norm_g[l], k_norm_g[l], lambda_vecs[l],
                             attn_out_norm_g[l], w_pool[l], pool_scale[l], conv_w[l], w_out[l])
        h = rms_norm(x, ffn_norm_g[l])
        if l % 2 == 0:
            j = l // 2
            x = x + swiglu(h, dense_w_gate[j], dense_w_up[j], dense_w_down[j])
        else:
            j = l // 2
            x = x + moe_swiglu(h, router_w[j], moe_w_gate[j], moe_w_up[j], moe_w_down[j])
    return x
```

```python
import math
from contextlib import ExitStack

import numpy as np
import concourse.bass as bass
import concourse.mybir as mybir
from concourse.bass_utils import run_bass_kernel_spmd

F32 = mybir.dt.float32
BF16 = mybir.dt.bfloat16
I32 = mybir.dt.int32
AF = mybir.ActivationFunctionType
ALU = mybir.AluOpType
AX = mybir.AxisListType

S = 2048
D = 2048
NT = S // 128
DEPTH = 2
HD = 128
NH = 4
IN_W = 5120
FF_D = 5632
FF_E = 7168
NE = 8
EPS = 1e-6
N_CORES = 8
QOFF, KOFF, VOFF, POFF, BOFF, COFF, UOFF = 0, 1024, 2048, 3072, 3584, 4096, 4608


class Tok:
    __slots__ = ("sem", "sid", "val")

    def __init__(self, sem, sid, val):
        self.sem, self.sid, self.val = sem, sid, val


class Buf:
    def __init__(self, k, name, merge=False):
        self.k, self.name, self.merge = k, name, merge
        self.w = {}
        self.r = {}
        self.dsem = None
        self.dcnt = 0

    def sem(self):
        if self.dsem is None:
            if self.k.free_dsems:
                self.dsem, self.dcnt = self.k.free_dsems.pop()
            else:
                self.dsem = self.k.new_sem("d_" + self.name)
        return self.dsem


class Eng:
    def __init__(self, k, name, eng):
        self.k, self.name, self.eng = k, name, eng
        self.sem = k.new_sem("e_" + name)
        self.cnt = 0
        self.waited = {}

    def wait(self, tok):
        if tok is None:
            return
        if self.waited.get(tok.sid, 0) < tok.val:
            self.eng.wait_ge(tok.sem, tok.val)
            self.waited[tok.sid] = tok.val

    def mark(self, ins):
        self.cnt += 1
        ins.then_inc(self.sem[0], 1)
        return Tok(self.sem[0], self.sem[1], self.cnt)


class K:
    def __init__(self, debug=None):
        self.debug = debug
        self.nc = bass.Bass("TRN2", target_bir_lowering=False)
        self.es = ExitStack()
        self.nsem = 0
        self.dma_toks = {}
        self.free_dsems = []
        self.live_bufs = []
        nc = self.nc
        self.PE = Eng(self, "pe", nc.tensor)
        self.ACT = Eng(self, "act", nc.scalar)
        self.DVE = Eng(self, "dve", nc.vector)
        self.POOL = Eng(self, "pool", nc.gpsimd)
        self.SP = Eng(self, "sp", nc.sync)
        self.engs = [self.PE, self.ACT, self.DVE, self.POOL, self.SP]

    def new_sem(self, name):
        self.nsem += 1
        h = self.es.enter_context(self.nc.semaphore(f"{name}_{self.nsem}"))
        return (h, self.nsem)

    def buf(self, name, merge=False, persist=False):
        b = Buf(self, name, merge)
        if not persist:
            self.live_bufs.append(b)
        return b

    def end_phase(self):
        self.barrier()
        for b in self.live_bufs:
            if b.dsem is not None:
                self.free_dsems.append((b.dsem, b.dcnt))
                b.dsem = None
        self.live_bufs = []

    def _deps(self, E, reads, writes):
        for b in reads:
            for t in b.w.values():
                E.wait(t)
        for b in writes:
            for t in b.w.values():
                E.wait(t)
            for t in b.r.values():
                E.wait(t)

    def _record(self, tok, reads, writes):
        for b in reads:
            b.r[tok.sid] = tok
        for b in writes:
            if b.merge:
                b.w[tok.sid] = tok
            else:
                b.w = {tok.sid: tok}
            b.r = {}

    def op(self, E, fn, reads=(), writes=()):
        self._deps(E, reads, writes)
        ins = fn()
        tok = E.mark(ins)
        self._record(tok, reads, writes)
        return tok

    def mm(self, out, pairs, reads, writes, transpose=False):
        E = self.PE
        self._deps(E, reads, writes)
        n = len(pairs)
        ins = None
        for i, (l, r) in enumerate(pairs):
            ins = self.nc.tensor.matmul(out, l, r, start=(i == 0), stop=(i == n - 1))
        tok = E.mark(ins)
        self._record(tok, reads, writes)
        return tok

    def mm_multi(self, items, reads, writes):
        E = self.PE
        self._deps(E, reads, writes)
        ins = None
        for (o, l, r, st, sp) in items:
            ins = self.nc.tensor.matmul(o, l, r, start=st, stop=sp)
        tok = E.mark(ins)
        self._record(tok, reads, writes)
        return tok

    def transposes(self, items, ident, reads, writes):
        E = self.PE
        self._deps(E, reads, writes)
        ins = None
        for (o, i) in items:
            ins = self.nc.tensor.transpose(o, i, ident)
        tok = E.mark(ins)
        self._record(tok, reads, writes)
        return tok

    def dma(self, Q, out, in_, sbuf, reads=(), writes=()):
        self._deps(Q, reads, writes)
        ins = Q.eng.dma_start(out=out, in_=in_)
        sem = sbuf.sem()
        sbuf.dcnt += 16
        ins.then_inc(sem[0], 16)
        tok = Tok(sem[0], sem[1], sbuf.dcnt)
        self.dma_toks[sem[1]] = tok
        self._record(tok, reads, writes)
        return tok

    def barrier(self):
        toks = [Tok(e.sem[0], e.sem[1], e.cnt) for e in self.engs if e.cnt > 0]
        toks += list(self.dma_toks.values())
        for e in self.engs:
            for t in toks:
                e.wait(t)

    def act(self, out, in_, func, reads, writes, scale=1.0, bias=None, accum_out=None):
        kw = {}
        if bias is not None:
            kw["bias"] = bias
        if accum_out is not None:
            kw["accum_out"] = accum_out
        return self.op(self.ACT, lambda: self.nc.scalar.activation(out=out, in_=in_, func=func, scale=scale, **kw),
                       reads, writes)

    def tt(self, out, in0, in1, op, reads, writes):
        return self.op(self.DVE, lambda: self.nc.vector.tensor_tensor(out=out, in0=in0, in1=in1, op=op), reads, writes)

    def ts(self, out, in0, s1, s2, op0, op1, reads, writes):
        if s2 is None:
            return self.op(self.DVE, lambda: self.nc.vector.tensor_scalar(out=out, in0=in0, scalar1=s1, scalar2=None,
                                                                          op0=op0), reads, writes)
        return self.op(self.DVE, lambda: self.nc.vector.tensor_scalar(out=out, in0=in0, scalar1=s1, scalar2=s2,
                                                                      op0=op0, op1=op1), reads, writes)

    def stt(self, out, in0, scalar, in1, op0, op1, reads, writes):
        return self.op(self.DVE, lambda: self.nc.vector.scalar_tensor_tensor(out=out, in0=in0, scalar=scalar, in1=in1,
                                                                             op0=op0, op1=op1), reads, writes)

    def recip(self, out, in_, reads, writes):
        return self.op(self.DVE, lambda: self.nc.vector.reciprocal(out=out, in_=in_), reads, writes)

    def vcopy(self, out, in_, reads, writes):
        return self.op(self.DVE, lambda: self.nc.vector.tensor_copy(out=out, in_=in_), reads, writes)

    def acopy(self, out, in_, reads, writes):
        return self.op(self.ACT, lambda: self.nc.scalar.copy(out=out, in_=in_), reads, writes)

    def vmemset(self, ap, val, writes):
        return self.op(self.DVE, lambda: self.nc.vector.memset(ap, val), (), writes)

    def sb(self, st, name, shape, dt):
        self.nsb = getattr(self, "nsb", 0) + 1
        return st.enter_context(self.nc.sbuf_tensor(f"{name}_{self.nsb}", shape, dt))

    def declare(self, skip=()):
        nc = self.nc
        d = {}
        self.inputs_declared = []

        def inp(name, shape, dt=F32):
            if name in skip:
                return
            self.inputs_declared.append(name)
            d[name] = nc.dram_tensor(name, shape, dt, kind="ExternalInput").ap()

        inp("x", [S, D])
        inp("pos", [1, S], I32)
        inp("attn_norm_g", [DEPTH, D])
        inp("w_in", [DEPTH, D, IN_W])
        inp("q_norm_g", [DEPTH, HD])
        inp("k_norm_g", [DEPTH, HD])
        inp("lambda_vecs", [DEPTH, 4, HD])
        inp("attn_out_norm_g", [DEPTH, 2 * HD])
        inp("w_pool", [DEPTH, 4, 128, 128])
        inp("pool_scale", [DEPTH, 512])
        inp("conv_w", [DEPTH, 3, 512])
        inp("w_out", [DEPTH, D, D])
        inp("ffn_norm_g", [DEPTH, D])
        inp("dense_w_gate", [1, D, FF_D])
        inp("dense_w_up", [1, D, FF_D])
        inp("dense_w_down", [1, FF_D, D])
        inp("router_w", [1, D, NE])
        inp("moe_w_gate", [1, NE, D, FF_E])
        inp("moe_w_up", [1, NE, D, FF_E])
        inp("moe_w_down", [1, NE, FF_E, D])
        inp("c_ident", [128, 128])
        inp("c_perm", [128, 128])
        inp("c_tri", [128, 128])
        inp("c_invf", [128, 1])
        inp("c_invc", [128, 64])
        d["out"] = nc.dram_tensor("out", [S, D], F32, kind="ExternalOutput").ap()
        skind = "ExternalOutput" if self.debug else "Internal"
        d["qT"] = nc.dram_tensor("s_qT", [8, 128, S], BF16, kind=skind).ap()
        d["kT"] = nc.dram_tensor("s_kT", [8, 128, S], BF16, kind=skind).ap()
        d["V"] = nc.dram_tensor("s_V", [S, 1024], BF16, kind=skind).ap()
        d["mixT"] = nc.dram_tensor("s_mixT", [16, 128, S], BF16, kind=skind).ap()
        self.d = d

    def build(self, upto="all", skip=()):
        nc = self.nc
        self.declare(skip)
        d = self.d
        with self.es:
            gst = ExitStack()
            with gst:
                g = self.g = {}
                g["ident_b"] = self.sb(gst, "ident_b", [128, 128], BF16)
                g["ident_f"] = self.sb(gst, "ident_f", [128, 128], F32)
                g["perm_b"] = self.sb(gst, "perm_b", [128, 128], BF16)
                g["tri_b"] = self.sb(gst, "tri_b", [128, 128], BF16)
                g["ones_b"] = self.sb(gst, "ones_b", [128, 128], BF16)
                g["ones_f"] = self.sb(gst, "ones_f", [128, 128], F32)
                g["invf"] = self.sb(gst, "invf", [128, 1], F32)
                g["invc"] = self.sb(gst, "invc", [128, 64], F32)
                g["eps"] = self.sb(gst, "eps", [128, 1], F32)
                g["negpi"] = self.sb(gst, "negpi", [128, 1], F32)
                self.ps = [gst.enter_context(nc.psum_tensor(f"ps{i}", [128, 512], F32)) for i in range(8)]
                self.psb = [self.buf(f"ps{i}", persist=True) for i in range(8)]
                cb = self.cb = self.buf("consts", persist=True)
                self.dma(self.POOL, g["ident_b"][:], d["c_ident"][:, :], cb, writes=[cb])
                self.dma(self.SP, g["ident_f"][:], d["c_ident"][:, :], cb, writes=[cb])
                self.dma(self.POOL, g["perm_b"][:], d["c_perm"][:, :], cb, writes=[cb])
                self.dma(self.POOL, g["tri_b"][:], d["c_tri"][:, :], cb, writes=[cb])
                self.dma(self.SP, g["invf"][:], d["c_invf"][:, :], cb, writes=[cb])
                self.dma(self.SP, g["invc"][:], d["c_invc"][:, :], cb, writes=[cb])
                self.vmemset(g["ones_b"][:], 1.0, [cb])
                self.vmemset(g["ones_f"][:], 1.0, [cb])
                self.vmemset(g["eps"][:], EPS, [cb])
                self.vmemset(g["negpi"][:], -math.pi, [cb])
                self.barrier()
                self.scr = {n: self.buf("scr_" + n, merge=True, persist=True) for n in ("qT", "kT", "V", "mixT")}
                self.outb = [self.buf(f"out{t}", merge=True, persist=True) for t in range(NT)]
                for l in range(DEPTH):
                    src = d["x"] if l == 0 else d["out"]
                    self.phase_inproj(l, src)
                    self.end_phase()
                    if upto == f"inproj{l}":
                        break
                    self.phase_attn(l)
                    self.end_phase()
                    if upto == f"attn{l}":
                        break
                    self.phase_outproj(l, src)
                    self.end_phase()
                    if upto == f"outproj{l}":
                        break
                    self.phase_ffn(l)
                    self.end_phase()
                    if upto == f"ffn{l}":
                        break
                self.barrier()
        return nc

    def rotary_tables(self):
        nc, g, d, cb = self.nc, self.g, self.d, self.cb
        with ExitStack() as st:
            posi = self.sb(st, "posi", [128, S], I32)
            ang = self.sb(st, "ang", [128, S], F32)
            r = self.sb(st, "rr", [128, S], F32)
            kf = self.sb(st, "rkf", [128, S], F32)
            tb = self.buf("rot_tmp")
            self.dma(self.SP, posi[:], d["pos"][0:1, :].partition_broadcast(128), tb, writes=[tb])
            self.vcopy(ang[:], posi[:], [tb], [tb])
            self.ts(ang[:], ang[:], g["invf"][:, 0:1], None, ALU.mult, None, [tb, cb], [tb])
            two_pi = 2.0 * math.pi
            C1 = 6.28125
            C2 = two_pi - C1

            def reduce_(shift):
                if shift:
                    self.ts(r[:], ang[:], shift, None, ALU.add, None, [tb], [tb])
                else:
                    self.vcopy(r[:], ang[:], [tb], [tb])
                self.ts(kf[:], r[:], 1.0 / two_pi, None, ALU.mult, None, [tb], [tb])
                self.vcopy(posi[:], kf[:], [tb], [tb])
                self.vcopy(kf[:], posi[:], [tb], [tb])
                self.stt(r[:], kf[:], -C1, r[:], ALU.mult, ALU.add, [tb], [tb])
                self.stt(r[:], kf[:], -C2, r[:], ALU.mult, ALU.add, [tb], [tb])
                self.ts(kf[:], r[:], math.pi, -two_pi, ALU.is_gt, ALU.mult, [tb], [tb])
                self.tt(r[:], r[:], kf[:], ALU.add, [tb], [tb])
                self.ts(kf[:], r[:], -math.pi, two_pi, ALU.is_lt, ALU.mult, [tb], [tb])
                self.tt(r[:], r[:], kf[:], ALU.add, [tb], [tb])
                self.ts(r[:], r[:], 3.1415925, -3.1415925, ALU.min, ALU.max, [tb], [tb])

            reduce_(0.0)
            self.act(g["Sg"][:], r[:], AF.Sin, [tb, cb], [cb])
            self.ts(g["Sg"][0:64, :], g["Sg"][0:64, :], -1.0, None, ALU.mult, None, [cb], [cb])
            reduce_(0.5 * math.pi)
            self.act(g["C"][:], r[:], AF.Sin, [tb, cb], [cb])
            self.barrier()

    def load_col(self, dst_col, vec_ap, b):
        self.dma(self.SP, dst_col, vec_ap.rearrange("(p o) -> p o", o=1), b, writes=[b])

    def norm_tile(self, xt, xb, gB, gBb, hT, hTb, col0, tmp, idx, ident, route=None):
        g = self.g
        ssq, sb_ = tmp["ssq"][idx % 2]
        xs, xsb = tmp["xs"][idx % 2]
        junk, jb = xs, xsb
        self.act(junk[:], xt, AF.Square, [xb], [jb, sb_], accum_out=ssq[:, 0:1])
        self.act(ssq[:, 1:2], ssq[:, 0:1], AF.Sqrt, [sb_, self.cb], [sb_], scale=1.0 / D, bias=g["eps"][:, 0:1])
        self.recip(ssq[:, 2:3], ssq[:, 1:2], [sb_], [sb_])
        self.stt(xs[:], xt, ssq[:, 2:3], gB[:], ALU.mult, ALU.mult, [xb, sb_, gBb], [xsb])
        for g4 in range(4):
            pi = (idx * 4 + g4) % 4
            pst, psb = self.ps[pi], self.psb[pi]
            if route is None:
                pv = pst[:].bitcast(BF16)
                items = [(pv[:, j * 128:(j + 1) * 128], xs[:, (g4 * 4 + j) * 128:(g4 * 4 + j + 1) * 128])
                         for j in range(4)]
                self.transposes(items, ident, [xsb, self.cb], [psb])
                src = pv[:, 0:512].rearrange("p (c t) -> p c t", c=4)
            else:
                items = [(pst[:, j * 128:(j + 1) * 128], xs[:, (g4 * 4 + j) * 128:(g4 * 4 + j + 1) * 128])
                         for j in range(4)]
                self.transposes(items, ident, [xsb, self.cb], [psb])
                src = pst[:, 0:512].rearrange("p (c t) -> p c t", c=4)
            dst = hT[:, g4 * 4:(g4 + 1) * 4, col0:col0 + 128]
            if route is None:
                if g4 % 2 == 0:
                    self.vcopy(dst, src, [psb], [hTb])
                else:
                    self.acopy(dst, src, [psb], [hTb])
            else:
                h32, h32b = route["h32"][g4 % 2]
                self.vcopy(h32[:], src, [psb], [h32b])
                self.acopy(dst, h32[:], [h32b], [hTb])
                lp, lpb = self.ps[4], self.psb[4]
                items = [(lp[:, 0:NE], h32[:, j, :], route["rw"][:, g4 * 4 + j, :], (g4 == 0 and j == 0),
                          (g4 == 3 and j == 3)) for j in range(4)]
                if route.get("level", 3) >= 2:
                    self.mm_multi(items, [h32b, route["rwb"]], [lpb])
        if route is not None and route.get("level", 3) >= 3:
            self.route_tile(route, idx)

    def route_tile(self, route, idx):
        lp, lpb = self.ps[4], self.psb[4]
        lg, lgb = route["lg"][idx % 2]
        comb, combb = route["comb"], route["combb"]
        self.vcopy(lg[:, 0:8], lp[:, 0:NE], [lpb], [lgb])
        self.op(self.DVE, lambda: self.nc.vector.max(out=lg[:, 8:16], in_=lg[:, 0:8]), [lgb], [lgb])
        self.tt(lg[:, 16:17], lg[:, 8:9], lg[:, 9:10], ALU.subtract, [lgb], [lgb])
        self.act(lg[:, 17:18], lg[:, 16:17], AF.Sigmoid, [lgb], [lgb])
        self.ts(lg[:, 18:19], lg[:, 17:18], -1.0, 1.0, ALU.mult, ALU.add, [lgb], [lgb])
        self.ts(lg[:, 24:32], lg[:, 0:8], lg[:, 8:9], lg[:, 17:18], ALU.is_equal, ALU.mult, [lgb], [lgb])
        self.ts(lg[:, 32:40], lg[:, 0:8], lg[:, 9:10], lg[:, 18:19], ALU.is_equal, ALU.mult, [lgb], [lgb])
        self.tt(comb[:, idx, :], lg[:, 24:32], lg[:, 32:40], ALU.add, [lgb], [combb])

    def phase_inproj(self, l, src):
        nc, g, d = self.nc, self.g, self.d
        with ExitStack() as st:
            hT = self.sb(st, "hT", [128, 16, S], BF16)
            hTb = self.buf("hT")
            g["C"] = self.sb(st, "rotC", [128, S], F32)
            g["Sg"] = self.sb(st, "rotS", [128, S], F32)
            self.rotary_tables()
            with ExitStack() as st2:
                gB = self.sb(st2, "gB", [128, D], F32)
                gBb = self.buf("gB")
                self.dma(self.SP, gB[:], d["attn_norm_g"][l:l + 1, :].partition_broadcast(128), gBb, writes=[gBb])
                xts = [(self.sb(st2, f"xt{i}", [128, D], F32), self.buf(f"xt{i}")) for i in range(3)]
                tmp = {
                    "ssq": [(self.sb(st2, f"ssq{i}", [128, 4], F32), self.buf(f"ssq{i}")) for i in range(2)],
                    "xs": [(self.sb(st2, f"xs{i}", [128, D], BF16), self.buf(f"xs{i}")) for i in range(2)],
                }
                for tt in range(NT):
                    xt, xb = xts[tt % 3]
                    rd = [self.outb[tt]] if l > 0 else []
                    self.dma(self.SP, xt[:], src[tt * 128:(tt + 1) * 128, :], xb, reads=rd, writes=[xb])
                    self.norm_tile(xt[:], xb, gB, gBb, hT, hTb, tt * 128, tmp, tt, g["ident_b"][:])
                self.barrier()
            self.inproj_body(l, hT, hTb, st)

    def inproj_body(self, l, hT, hTb, st):
        nc, g, d = self.nc, self.g, self.d
        cb = self.cb
        W = d["w_in"]
        wsl = [(self.sb(st, f"wi{i}", [128, 16, 512], BF16), self.buf(f"wi{i}")) for i in range(2)]
        F = [(self.sb(st, f"F{i}", [128, 16 + S], F32), self.buf(f"F{i}")) for i in range(4)]
        sm = [(self.sb(st, f"sm{i}", [128, 512], F32), self.buf(f"sm{i}")) for i in range(6)]
        smb = [(self.sb(st, f"smb{i}", [128, 512], BF16), self.buf(f"smb{i}")) for i in range(4)]
        stg = [(self.sb(st, f"stg{i}", [128, S], BF16), self.buf(f"stg{i}")) for i in range(2)]
        vst = [(self.sb(st, f"vst{i}", [128, 512], BF16), self.buf(f"vst{i}")) for i in range(3)]
        pv = self.sb(st, "pvec", [128, 16], F32)
        pvb = self.buf("pvec")
        wp = self.sb(st, "wpool", [128, 4, 128], BF16)
        wpb = self.buf("wpool")
        self.load_col(pv[:, 0:1], d["q_norm_g"][l, :], pvb)
        self.load_col(pv[:, 1:2], d["k_norm_g"][l, :], pvb)
        for c in range(4):
            self.load_col(pv[:, 2 + c:3 + c], d["pool_scale"][l, c * 128:(c + 1) * 128], pvb)
        self.cw = self.sb(st, "convw", [128, 12], F32)
        for j in range(3):
            for c in range(4):
                self.load_col(self.cw[:, j * 4 + c:j * 4 + c + 1], d["conv_w"][l, j, c * 128:(c + 1) * 128], pvb)
        self.dma(self.POOL, wp[:], d["w_pool"][l].rearrange("g c d -> c g d"), wpb, writes=[wpb])
        for i in range(4):
            self.vmemset(F[i][0][:, 0:16], 0.0, [F[i][1]])

        units = []
        for c in range(8):
            units.append(("q", c, [(QOFF + c * 128, 128)]))
        for c in range(8):
            units.append(("k", c, [(KOFF + c * 128, 128)]))
        for vb in range(2):
            units.append(("v", vb, [(VOFF + vb * 512, 512)]))
        for c in range(4):
            units.append(("pool", c, [(POFF + c * 128, 128)]))
        for c in range(4):
            units.append(("conv", c, [(BOFF + c * 128, 128), (COFF + c * 128, 128), (UOFF + c * 128, 128)]))

        def load(ui):
            kind, idx, cols = units[ui]
            wt, wb = wsl[ui % 2]
            o = 0
            for (c0, n) in cols:
                self.dma(self.POOL, wt[:, :, o:o + n], W[l, :, c0:c0 + n].rearrange("(c p) n -> p c n", p=128), wb,
                         writes=[wb])
                o += n

        load(0)
        rr = [0]

        def nxt(lst):
            rr[0] += 1
            return lst[rr[0] % len(lst)]

        pcount = [0]

        def psn():
            pcount[0] += 1
            i = pcount[0] % 8
            return self.ps[i], self.psb[i]

        for ui, (kind, idx, cols) in enumerate(units):
            if ui + 1 < len(units):
                load(ui + 1)
            wt, wb = wsl[ui % 2]
            if kind in ("q", "k"):
                gcol = pv[:, 0:1] if kind == "q" else pv[:, 1:2]
                sg, sgb = nxt(stg)
                for tb in range(4):
                    tsl = slice(tb * 512, (tb + 1) * 512)
                    hb = [hTb]
                    pz, pzb = psn()
                    self.mm(pz[:], [(wt[:, kc, 0:128], hT[:, kc, tsl]) for kc in range(16)], [wb] + hb, [pzb])
                    qg, qgb = nxt(smb)
                    sq, sqb = nxt(smb)
                    self.act(qg[:], pz[:], AF.Copy, [pzb, pvb], [qgb], scale=gcol)
                    self.act(sq[:], pz[:], AF.Square, [pzb], [sqb])
                    pss, pssb = psn()
                    self.mm(pss[:], [(g["ones_b"][:], sq[:])], [sqb, cb], [pssb])
                    ppq, ppqb = psn()
                    self.mm(ppq[:], [(g["perm_b"][:], qg[:])], [qgb, cb], [ppqb])
                    rs, rsb = nxt(sm)
                    self.act(rs[:], pss[:], AF.Sqrt, [pssb, cb], [rsb], scale=1.0 / HD, bias=g["eps"][:, 0:1])
                    self.recip(rs[:], rs[:], [rsb], [rsb])
                    t1, t1b = nxt(sm)
                    t2, t2b = nxt(sm)
                    self.tt(t1[:], qg[:], g["C"][:, tsl], ALU.mult, [qgb, cb], [t1b])
                    self.tt(t2[:], ppq[:], g["Sg"][:, tsl], ALU.mult, [ppqb, cb], [t2b])
                    self.tt(t1[:], t1[:], t2[:], ALU.add, [t1b, t2b], [t1b])
                    self.tt(sg[:, tsl], t1[:], rs[:], ALU.mult, [t1b, rsb], [sgb])
                dst = d["qT"] if kind == "q" else d["kT"]
                self.dma(self.SP, dst[idx, :, :], sg[:], sgb, reads=[sgb], writes=[self.scr["qT" if kind == "q" else "kT"]])
            elif kind == "v":
                for tt in range(NT):
                    pz, pzb = psn()
                    self.mm(pz[:], [(hT[:, kc, tt * 128:(tt + 1) * 128], wt[:, kc, 0:512]) for kc in range(16)],
                            [wb, hTb], [pzb])
                    vs, vsb = nxt(vst)
                    if tt % 2 == 0:
                        self.vcopy(vs[:], pz[:], [pzb], [vsb])
                    else:
                        self.acopy(vs[:], pz[:], [pzb], [vsb])
                    self.dma(self.SP, d["V"][tt * 128:(tt + 1) * 128, idx * 512:(idx + 1) * 512], vs[:], vsb,
                             reads=[vsb], writes=[self.scr["V"]])
            elif kind == "pool":
                G, Gb = F[0]
                for tb in range(4):
                    pz, pzb = psn()
                    self.mm(pz[:], [(wt[:, kc, 0:128], hT[:, kc, tb * 512:(tb + 1) * 512]) for kc in range(16)],
                            [wb, hTb], [pzb])
                    self.acopy(G[:, 16 + tb * 512:16 + (tb + 1) * 512], pz[:], [pzb], [Gb])
                w = 2 << idx
                cur, curb = G, Gb
                sh = 1
                pp = 1
                while sh < w:
                    nx, nxb = F[pp]
                    self.tt(nx[:, 16:16 + S], cur[:, 16:16 + S], cur[:, 16 - sh:16 - sh + S], ALU.add, [curb], [nxb])
                    cur, curb = nx, nxb
                    pp = 3 - pp
                    sh *= 2
                po, pob = F[3]
                self.stt(po[:, 16:16 + S], cur[:, 16:16 + S], 1.0 / w, G[:, 16:16 + S], ALU.mult, ALU.subtract,
                         [curb, Gb], [pob])
                self.tt(po[:, 0:16], cur[:, 16:32], g["invc"][:, idx * 16:(idx + 1) * 16], ALU.mult, [curb, cb], [pob])
                self.tt(po[:, 16:32], po[:, 0:16], G[:, 16:32], ALU.subtract, [pob, Gb], [pob])
                pbf, pbfb = nxt(stg)
                self.vcopy(pbf[:], po[:, 16:16 + S], [pob], [pbfb])
                sg, sgb = nxt(stg)
                for tb in range(4):
                    pz, pzb = psn()
                    self.mm(pz[:], [(wp[:, idx, :], pbf[:, tb * 512:(tb + 1) * 512])], [wpb, pbfb], [pzb])
                    self.act(sg[:, tb * 512:(tb + 1) * 512], pz[:], AF.Copy, [pzb, pvb], [sgb],
                             scale=pv[:, 2 + idx:3 + idx])
                self.dma(self.SP, d["mixT"][8 + idx, :, :], sg[:], sgb, reads=[sgb], writes=[self.scr["mixT"]])
            else:
                U, Ub = F[0]
                Bt, Bb = F[1]
                Y, Yb = F[2]
                for tb in range(4):
                    tsl = slice(tb * 512, (tb + 1) * 512)
                    pB, pBb = psn()
                    self.mm(pB[:], [(wt[:, kc, 0:128], hT[:, kc, tsl]) for kc in range(16)], [wb, hTb], [pBb])
                    pC, pCb = psn()
                    self.mm(pC[:], [(wt[:, kc, 128:256], hT[:, kc, tsl]) for kc in range(16)], [wb, hTb], [pCb])
                    pH, pHb = psn()
                    self.mm(pH[:], [(wt[:, kc, 256:384], hT[:, kc, tsl]) for kc in range(16)], [wb, hTb], [pHb])
                    self.acopy(Bt[:, 16 + tb * 512:16 + (tb + 1) * 512], pB[:], [pBb], [Bb])
                    cc, ccb = nxt(sm)
                    self.acopy(cc[:], pC[:], [pCb], [ccb])
                    self.tt(U[:, 16 + tb * 512:16 + (tb + 1) * 512], pH[:], cc[:], ALU.mult, [pHb, ccb], [Ub])
                cw = self.cw
                self.ts(Y[:, 16:16 + S], U[:, 16:16 + S], cw[:, 8 + idx:9 + idx], None, ALU.mult, None, [Ub, pvb], [Yb])
                self.stt(Y[:, 16:16 + S], U[:, 15:15 + S], cw[:, 4 + idx:5 + idx], Y[:, 16:16 + S], ALU.mult, ALU.add,
                         [Ub, pvb, Yb], [Yb])
                self.stt(Y[:, 16:16 + S], U[:, 14:14 + S], cw[:, idx:idx + 1], Y[:, 16:16 + S], ALU.mult, ALU.add,
                         [Ub, pvb, Yb], [Yb])
                sg, sgb = nxt(stg)
                self.tt(sg[:], Y[:, 16:16 + S], Bt[:, 16:16 + S], ALU.mult, [Yb, Bb], [sgb])
                self.dma(self.SP, d["mixT"][12 + idx, :, :], sg[:], sgb, reads=[sgb], writes=[self.scr["mixT"]])

    def phase_attn(self, l):
        nc, g, d = self.nc, self.g, self.d
        cb = self.cb
        lam_init = 0.8 - 0.6 * math.exp(-0.3 * l)
        scale = HD ** -0.5
        with ExitStack() as st:
            lv = self.sb(st, "lv", [128, 16], F32)
            lvb = self.buf("lv")
            for j in range(4):
                self.load_col(lv[:, j:j + 1], d["lambda_vecs"][l, j, :], lvb)
            for c in range(2):
                self.load_col(lv[:, 4 + c:5 + c], d["attn_out_norm_g"][l, c * 128:(c + 1) * 128], lvb)
            self.tt(lv[:, 6:7], lv[:, 0:1], lv[:, 1:2], ALU.mult, [lvb], [lvb])
            self.tt(lv[:, 7:8], lv[:, 2:3], lv[:, 3:4], ALU.mult, [lvb], [lvb])
            p0, p0b = self.ps[0], self.psb[0]
            self.mm(p0[:, 0:2], [(g["ones_f"][:], lv[:, 6:8])], [lvb, cb], [p0b])
            self.act(lv[:, 8:10], p0[:, 0:2], AF.Exp, [p0b], [lvb])
            self.tt(lv[:, 10:11], lv[:, 8:9], lv[:, 9:10], ALU.subtract, [lvb], [lvb])
            self.ts(lv[:, 11:12], lv[:, 10:11], lam_init, -1.0, ALU.add, ALU.mult, [lvb], [lvb])
            self.ts(lv[:, 12:14], lv[:, 4:6], 1.0 - lam_init, None, ALU.mult, None, [lvb], [lvb])

            qh = [(self.sb(st, f"qh{i}", [128, 2, S], BF16), self.buf(f"qh{i}")) for i in range(2)]
            kh = [(self.sb(st, f"kh{i}", [128, 2, S], BF16), self.buf(f"kh{i}")) for i in range(2)]
            vh = [(self.sb(st, f"vh{i}", [128, 16, 256], BF16), self.buf(f"vh{i}")) for i in range(2)]
            pT = [(self.sb(st, f"pT{i}", [128, 512], BF16), self.buf(f"pT{i}")) for i in range(4)]
            rl = [(self.sb(st, f"rl{i}", [128, 512], F32), self.buf(f"rl{i}")) for i in range(2)]
            o1 = [(self.sb(st, f"o1{i}", [128, 512], F32), self.buf(f"o1{i}")) for i in range(2)]
            oo = [(self.sb(st, f"oo{i}", [128, 512], F32), self.buf(f"oo{i}")) for i in range(2)]
            sq = [(self.sb(st, f"sq{i}", [128, 512], BF16), self.buf(f"sq{i}")) for i in range(2)]
            rs, rsb = self.sb(st, "ars", [128, 512], F32), self.buf("ars")
            ast = [(self.sb(st, f"ast{i}", [128, S], BF16), self.buf(f"ast{i}")) for i in range(4)]

            def loadh(h):
                q, qb_ = qh[h % 2]
                k_, kb_ = kh[h % 2]
                v, vb_ = vh[h % 2]
                self.dma(self.SP, q[:], d["qT"][2 * h:2 * h + 2, :, :].rearrange("m p s -> p m s"), qb_,
                         reads=[self.scr["qT"]], writes=[qb_])
                self.dma(self.SP, k_[:], d["kT"][2 * h:2 * h + 2, :, :].rearrange("m p s -> p m s"), kb_,
                         reads=[self.scr["kT"]], writes=[kb_])
                self.dma(self.SP, v[:], d["V"][:, h * 256:(h + 1) * 256].rearrange("(t p) e -> p t e", p=128), vb_,
                         reads=[self.scr["V"]], writes=[vb_])

            loadh(0)
            pcnt = 0
            for h in range(NH):
                if h + 1 < NH:
                    loadh(h + 1)
                q, qb_ = qh[h % 2]
                k_, kb_ = kh[h % 2]
                v, vb_ = vh[h % 2]
                for qb in range(4):
                    q0 = qb * 512
                    nkt = 4 * qb + 4
                    for m in range(2):
                        O0, O0b = self.ps[2 + 3 * m], self.psb[2 + 3 * m]
                        O1, O1b = self.ps[3 + 3 * m], self.psb[3 + 3 * m]
                        L, Lb = self.ps[4 + 3 * m], self.psb[4 + 3 * m]
                        for kt in range(nkt):
                            j = kt - 4 * qb
                            c0 = max(j, 0) * 128
                            sc, scb = self.ps[pcnt % 2], self.psb[pcnt % 2]
                            p, pb = pT[pcnt % 4]
                            pcnt += 1
                            self.mm(sc[:, c0:512], [(k_[:, m, kt * 128:(kt + 1) * 128], q[:, m, q0 + c0:q0 + 512])],
                                    [kb_, qb_], [scb])
                            self.act(p[:, c0:512], sc[:, c0:512], AF.Exp, [scb], [pb], scale=scale)
                            if j >= 0:
                                self.tt(p[:, c0:c0 + 128], p[:, c0:c0 + 128], g["tri_b"][:], ALU.mult, [pb, cb], [pb])
                            first, last = (kt == 0), (kt == nkt - 1)
                            items = [
                                (O0[:, c0:512], v[:, kt, 0:128], p[:, c0:512], first, last),
                                (O1[:, c0:512], v[:, kt, 128:256], p[:, c0:512], first, last),
                                (L[:, c0:512], g["ones_b"][:], p[:, c0:512], first, last),
                            ]
                            self.mm_multi(items, [vb_, pb, cb], [O0b, O1b, Lb])
                    for m in range(2):
                        L, Lb = self.ps[4 + 3 * m], self.psb[4 + 3 * m]
                        self.recip(rl[m][0][:], L[:], [Lb], [rl[m][1]])
                    for ec in range(2):
                        A, Ab = self.ps[2 + ec], self.psb[2 + ec]
                        Bm, Bmb = self.ps[5 + ec], self.psb[5 + ec]
                        self.tt(o1[ec][0][:], A[:], rl[0][0][:], ALU.mult, [Ab, rl[0][1]], [o1[ec][1]])
                        self.tt(oo[ec][0][:], Bm[:], rl[1][0][:], ALU.mult, [Bmb, rl[1][1]], [oo[ec][1]])
                        self.stt(oo[ec][0][:], oo[ec][0][:], lv[:, 11:12], o1[ec][0][:], ALU.mult, ALU.add,
                                 [oo[ec][1], o1[ec][1], lvb], [oo[ec][1]])
                        self.act(sq[ec][0][:], oo[ec][0][:], AF.Square, [oo[ec][1]], [sq[ec][1]])
                    ssn, ssnb = self.ps[pcnt % 2], self.psb[pcnt % 2]
                    pcnt += 1
                    self.mm(ssn[:], [(g["ones_b"][:], sq[0][0][:]), (g["ones_b"][:], sq[1][0][:])],
                            [sq[0][1], sq[1][1], cb], [ssnb])
                    self.act(rs[:], ssn[:], AF.Sqrt, [ssnb, cb], [rsb], scale=1.0 / (2 * HD), bias=g["eps"][:, 0:1])
                    self.recip(rs[:], rs[:], [rsb], [rsb])
                    for ec in range(2):
                        a, ab = ast[(h % 2) * 2 + ec]
                        self.stt(a[:, q0:q0 + 512], oo[ec][0][:], lv[:, 12 + ec:13 + ec], rs[:], ALU.mult, ALU.mult,
                                 [oo[ec][1], rsb, lvb], [ab])
                for ec in range(2):
                    a, ab = ast[(h % 2) * 2 + ec]
                    self.dma(self.SP, d["mixT"][2 * h + ec, :, :], a[:], ab, reads=[ab], writes=[self.scr["mixT"]])

    def phase_outproj(self, l, src):
        nc, g, d = self.nc, self.g, self.d
        with ExitStack() as st:
            mix = self.sb(st, "mix", [128, 16, S], BF16)
            mixb = self.buf("mix")
            for c4 in range(4):
                self.dma(self.SP, mix[:, c4 * 4:(c4 + 1) * 4, :], d["mixT"][c4 * 4:(c4 + 1) * 4, :, :].rearrange("c p s -> p c s"),
                         mixb, reads=[self.scr["mixT"]], writes=[mixb])
            wsl = [(self.sb(st, f"wo{i}", [128, 16, 512], BF16), self.buf(f"wo{i}")) for i in range(2)]
            xts = [(self.sb(st, f"xo{i}", [128, 512], F32), self.buf(f"xo{i}")) for i in range(4)]

            def loadw(db):
                wt, wb = wsl[db % 2]
                self.dma(self.POOL, wt[:], d["w_out"][l, :, db * 512:(db + 1) * 512].rearrange("(c p) n -> p c n", p=128),
                         wb, writes=[wb])

            units = [(db, tt) for db in range(4) for tt in range(NT)]

            def loadx(ui):
                db, tt = units[ui]
                xt, xb = xts[ui % 4]
                rd = [self.outb[tt]] if l > 0 else []
                self.dma(self.SP, xt[:], src[tt * 128:(tt + 1) * 128, db * 512:(db + 1) * 512], xb, reads=rd, writes=[xb])

            loadw(0)
            loadx(0)
            loadx(1)
            for ui, (db, tt) in enumerate(units):
                if tt == 0 and db + 1 < 4:
                    loadw(db + 1)
                if ui + 2 < len(units):
                    loadx(ui + 2)
                wt, wb = wsl[db % 2]
                xt, xb = xts[ui % 4]
                pz, pzb = self.ps[ui % 4], self.psb[ui % 4]
                self.mm(pz[:], [(mix[:, kc, tt * 128:(tt + 1) * 128], wt[:, kc, :]) for kc in range(16)], [mixb, wb], [pzb])
                self.tt(xt[:], xt[:], pz[:], ALU.add, [xb, pzb], [xb])
                self.dma(self.SP, d["out"][tt * 128:(tt + 1) * 128, db * 512:(db + 1) * 512], xt[:], xb, reads=[xb],
                         writes=[self.outb[tt]])

    def phase_ffn(self, l):
        nc, g, d = self.nc, self.g, self.d
        cb = self.cb
        moe = (l % 2 == 1)
        j = l // 2
        ne = NE if moe else 1
        ff = FF_E if moe else FF_D
        nfb = ff // 256
        if self.debug and isinstance(self.debug, dict):
            ne = min(ne, self.debug.get("ne", ne))
            nfb = min(nfb, self.debug.get("nfb", nfb))
        with ExitStack() as st:
            gB = self.sb(st, "gBf", [128, D], F32)
            gBb = self.buf("gBf")
            self.dma(self.SP, gB[:], d["ffn_norm_g"][l:l + 1, :].partition_broadcast(128), gBb, writes=[gBb])
            acc = [(self.sb(st, f"acc{i}", [128, D], F32), self.buf(f"acc{i}")) for i in range(8)]
            hT = self.sb(st, "h2T", [128, 16, 1024], BF16)
            hTb = self.buf("h2T")
            wg = [(self.sb(st, f"wg{i}", [128, 16, 256], BF16), self.buf(f"wg{i}")) for i in range(2)]
            wu = [(self.sb(st, f"wu{i}", [128, 16, 256], BF16), self.buf(f"wu{i}")) for i in range(2)]
            wd = [(self.sb(st, f"wd{i}", [128, 2, D], BF16), self.buf(f"wd{i}")) for i in range(2)]
            sg = [(self.sb(st, f"sg{i}", [128, 512], F32), self.buf(f"sg{i}")) for i in range(2)]
            at = [(self.sb(st, f"at{i}", [128, 512], BF16), self.buf(f"at{i}")) for i in range(4)]
            tmp = {
                "ssq": [(self.sb(st, f"ssqf{i}", [128, 4], F32), self.buf(f"ssqf{i}")) for i in range(2)],
            }
            route = None
            use_route = moe and not (isinstance(self.debug, dict) and self.debug.get("noroute"))
            if not use_route and moe:
                tmp["xs"] = [(self.sb(st, f"xsf{i}", [128, D], BF16), self.buf(f"xsf{i}")) for i in range(2)]
            if use_route:
                tmp["xs"] = [(self.sb(st, f"xsf{i}", [128, D], F32), self.buf(f"xsf{i}")) for i in range(1)] * 2
                route = {
                    "h32": [(self.sb(st, f"h32{i}", [128, 4, 128], F32), self.buf(f"h32{i}")) for i in range(2)],
                    "rw": self.sb(st, "rw", [128, 16, NE], F32),
                    "rwb": self.buf("rw"),
                    "lg": [(self.sb(st, f"lg{i}", [128, 40], F32), self.buf(f"lg{i}")) for i in range(2)],
                    "comb": self.sb(st, "comb", [128, 8, NE], F32),
                    "combb": self.buf("comb"),
                }
                for c in range(16):
                    self.dma(self.POOL, route["rw"][:, c, :], d["router_w"][j, c * 128:(c + 1) * 128, :], route["rwb"],
                             writes=[route["rwb"]])
                if isinstance(self.debug, dict):
                    route["level"] = self.debug.get("level", 3)
            else:
                tmp["xs"] = [(self.sb(st, f"xsf{i}", [128, D], BF16), self.buf(f"xsf{i}")) for i in range(2)]

            if moe:
                Wg = lambda e, fb: d["moe_w_gate"][j, e, :, fb * 256:(fb + 1) * 256]
                Wu = lambda e, fb: d["moe_w_up"][j, e, :, fb * 256:(fb + 1) * 256]
                Wd = lambda e, fb: d["moe_w_down"][j, e, fb * 256:(fb + 1) * 256, :]
            else:
                Wg = lambda e, fb: d["dense_w_gate"][j, :, fb * 256:(fb + 1) * 256]
                Wu = lambda e, fb: d["dense_w_up"][j, :, fb * 256:(fb + 1) * 256]
                Wd = lambda e, fb: d["dense_w_down"][j, fb * 256:(fb + 1) * 256, :]

            for half in range(2):
                units = [(e, fb) for e in range(ne) for fb in range(nfb)]

                def loadw(ui):
                    e, fb = units[ui]
                    s_ = ui % 2
                    self.dma(self.POOL, wg[s_][0][:], Wg(e, fb).rearrange("(c p) n -> p c n", p=128), wg[s_][1],
                             writes=[wg[s_][1]])
                    self.dma(self.POOL, wu[s_][0][:], Wu(e, fb).rearrange("(c p) n -> p c n", p=128), wu[s_][1],
                             writes=[wu[s_][1]])
                    self.dma(self.POOL, wd[s_][0][:], Wd(e, fb).rearrange("(c p) n -> p c n", p=128), wd[s_][1],
                             writes=[wd[s_][1]])

                loadw(0)
                for i in range(8):
                    tt = half * 8 + i
                    self.dma(self.SP, acc[i][0][:], d["out"][tt * 128:(tt + 1) * 128, :], acc[i][1],
                             reads=[self.outb[tt]], writes=[acc[i][1]])
                for i in range(8):
                    self.norm_tile(acc[i][0][:], acc[i][1], gB, gBb, hT, hTb, i * 128, tmp, i,
                                   g["ident_f"][:] if use_route else g["ident_b"][:], route=route)
                pc = 0
                for ui, (e, fb) in enumerate(units):
                    if ui + 1 < len(units):
                        loadw(ui + 1)
                    s_ = ui % 2
                    wgt, wgb = wg[s_]
                    wut, wub = wu[s_]
                    wdt, wdb = wd[s_]
                    for tb in range(2):
                        tsl = slice(tb * 512, (tb + 1) * 512)
                        acts = []
                        for c in range(2):
                            pg, pgb = self.ps[c], self.psb[c]
                            pu, pub = self.ps[2 + c], self.psb[2 + c]
                            self.mm(pg[:], [(wgt[:, kc, c * 128:(c + 1) * 128], hT[:, kc, tsl]) for kc in range(16)],
                                    [wgb, hTb], [pgb])
                            self.mm(pu[:], [(wut[:, kc, c * 128:(c + 1) * 128], hT[:, kc, tsl]) for kc in range(16)],
                                    [wub, hTb], [pub])
                            sgt, sgb_ = sg[c]
                            self.act(sgt[:], pg[:], AF.Silu, [pgb], [sgb_])
                            a, ab = at[(tb * 2 + c) % 4]
                            self.tt(a[:], pu[:], sgt[:], ALU.mult, [pub, sgb_], [ab])
                            acts.append((a, ab))
                        for t4 in range(4):
                            ti = tb * 4 + t4
                            for dh in range(2):
                                pi = 4 + 2 * (pc % 2)
                                pc += 1
                                items = []
                                for db in range(2):
                                    for c in range(2):
                                        items.append((self.ps[pi + db][:], acts[c][0][:, t4 * 128:(t4 + 1) * 128],
                                                      wdt[:, c, dh * 1024 + db * 512:dh * 1024 + (db + 1) * 512],
                                                      c == 0, c == 1))
                                self.mm_multi(items, [acts[0][1], acts[1][1], wdb], [self.psb[pi], self.psb[pi + 1]])
                                for db in range(2):
                                    dsl = slice(dh * 1024 + db * 512, dh * 1024 + (db + 1) * 512)
                                    a_t, a_b = acc[ti]
                                    if use_route and route.get("level", 3) >= 3:
                                        self.stt(a_t[:, dsl], self.ps[pi + db][:], route["comb"][:, ti, e:e + 1],
                                                 a_t[:, dsl], ALU.mult, ALU.add,
                                                 [self.psb[pi + db], route["combb"], a_b], [a_b])
                                    else:
                                        self.tt(a_t[:, dsl], self.ps[pi + db][:], a_t[:, dsl], ALU.add,
                                                [self.psb[pi + db], a_b], [a_b])
                for i in range(8):
                    tt = half * 8 + i
                    self.dma(self.SP, d["out"][tt * 128:(tt + 1) * 128, :], acc[i][0][:], acc[i][1], reads=[acc[i][1]],
                             writes=[self.outb[tt]])
                self.barrier()


def _consts():
    ident = np.eye(128, dtype=np.float32)
    perm = np.zeros((128, 128), np.float32)
    for p in range(128):
        perm[p, (p + 64) % 128] = 1.0
    tri = (np.arange(128)[:, None] <= np.arange(128)[None, :]).astype(np.float32)
    jj = np.arange(0, HD, 2, dtype=np.float32) / np.float32(HD)
    invf64 = (1.0 / (np.float32(10000.0) ** jj)).astype(np.float32)
    invf = np.concatenate([invf64, invf64]).reshape(128, 1).astype(np.float32)
    invc = np.zeros((128, 64), np.float32)
    for gi, w in enumerate((2, 4, 8, 16)):
        invc[:, gi * 16:(gi + 1) * 16] = 1.0 / np.minimum(np.arange(16) + 1, w).astype(np.float32)
    return {"c_ident": ident, "c_perm": perm, "c_tri": tri, "c_invf": invf, "c_invc": invc}


_WNAMES = ["attn_norm_g", "w_in", "q_norm_g", "k_norm_g", "lambda_vecs", "attn_out_norm_g", "w_pool", "pool_scale",
           "conv_w", "w_out", "ffn_norm_g", "dense_w_gate", "dense_w_up", "dense_w_down", "router_w", "moe_w_gate",
           "moe_w_up", "moe_w_down"]


def make_in_maps(inputs, n_cores=N_CORES):
    consts = _consts()
    shared = {n: np.ascontiguousarray(np.asarray(inputs[n], dtype=np.float32)) for n in _WNAMES}
    shared.update(consts)
    x = np.asarray(inputs["x"], dtype=np.float32)
    pos = np.asarray(inputs["positions"], dtype=np.int32)
    maps = []
    for c in range(n_cores):
        m = dict(shared)
        m["x"] = np.ascontiguousarray(x[c])
        m["pos"] = np.ascontiguousarray(pos[c].reshape(1, S))
        maps.append(m)
    return maps


def kernel(**inputs):
    k = K()
    nc = k.build()
    in_maps = make_in_maps(inputs)
    res = run_bass_kernel_spmd(nc, in_maps, core_ids=list(range(N_CORES)))
    return np.stack([np.asarray(r["out"], dtype=np.float32) for r in res.results], axis=0)
```

```python
import math
from contextlib import ExitStack

import numpy as np
import concourse.bass as bass
import concourse.mybir as mybir
from concourse.bass_utils import run_bass_kernel_spmd

F32 = mybir.dt.float32
BF16 = mybir.dt.bfloat16
I32 = mybir.dt.int32
AF = mybir.ActivationFunctionType
ALU = mybir.AluOpType
AX = mybir.AxisListType

S = 2048
D = 2048
NT = S // 128
DEPTH = 2
HD = 128
NH = 4
IN_W = 5120
FF_D = 5632
FF_E = 7168
NE = 8
EPS = 1e-6
N_CORES = 8
QOFF, KOFF, VOFF, POFF, BOFF, COFF, UOFF = 0, 1024, 2048, 3072, 3584, 4096, 4608


class Tok:
    __slots__ = ("sem", "sid", "val")

    def __init__(self, sem, sid, val):
        self.sem, self.sid, self.val = sem, sid, val


class Buf:
    def __init__(self, k, name, merge=False):
        self.k, self.name, self.merge = k, name, merge
        self.w = {}
        self.r = {}
        self.dsem = None
        self.dcnt = 0

    def sem(self):
        if self.dsem is None:
            if self.k.free_dsems:
                self.dsem, self.dcnt = self.k.free_dsems.pop()
            else:
                self.dsem = self.k.new_sem("d_" + self.name)
        return self.dsem


class Eng:
    def __init__(self, k, name, eng):
        self.k, self.name, self.eng = k, name, eng
        self.sem = k.new_sem("e_" + name)
        self.cnt = 0
        self.waited = {}

    def wait(self, tok):
        if tok is None:
            return
        if self.waited.get(tok.sid, 0) < tok.val:
            self.eng.wait_ge(tok.sem, tok.val)
            self.waited[tok.sid] = tok.val

    def mark(self, ins):
        self.cnt += 1
        ins.then_inc(self.sem[0], 1)
        return Tok(self.sem[0], self.sem[1], self.cnt)


class K:
    def __init__(self, debug=None):
        self.debug = debug
        self.nc = bass.Bass("TRN2", target_bir_lowering=False)
        self.es = ExitStack()
        self.nsem = 0
        self.dma_toks = {}
        self.free_dsems = []
        self.live_bufs = []
        nc = self.nc
        self.PE = Eng(self, "pe", nc.tensor)
        self.ACT = Eng(self, "act", nc.scalar)
        self.DVE = Eng(self, "dve", nc.vector)
        self.POOL = Eng(self, "pool", nc.gpsimd)
        self.SP = Eng(self, "sp", nc.sync)
        self.engs = [self.PE, self.ACT, self.DVE, self.POOL, self.SP]

    def new_sem(self, name):
        self.nsem += 1
        h = self.es.enter_context(self.nc.semaphore(f"{name}_{self.nsem}"))
        return (h, self.nsem)

    def buf(self, name, merge=False, persist=False):
        b = Buf(self, name, merge)
        if not persist:
            self.live_bufs.append(b)
        return b

    def end_phase(self):
        self.barrier()
        for b in self.live_bufs:
            if b.dsem is not None:
                self.free_dsems.append((b.dsem, b.dcnt))
                b.dsem = None
        self.live_bufs = []

    def _deps(self, E, reads, writes):
        for b in reads:
            for t in b.w.values():
                E.wait(t)
        for b in writes:
            for t in b.w.values():
                E.wait(t)
            for t in b.r.values():
                E.wait(t)

    def _record(self, tok, reads, writes):
        for b in reads:
            b.r[tok.sid] = tok
        for b in writes:
            if b.merge:
                b.w[tok.sid] = tok
            else:
                b.w = {tok.sid: tok}
            b.r = {}

    def op(self, E, fn, reads=(), writes=()):
        self._deps(E, reads, writes)
        ins = fn()
        tok = E.mark(ins)
        self._record(tok, reads, writes)
        return tok

    def mm(self, out, pairs, reads, writes, transpose=False):
        E = self.PE
        self._deps(E, reads, writes)
        n = len(pairs)
        ins = None
        for i, (l, r) in enumerate(pairs):
            ins = self.nc.tensor.matmul(out, l, r, start=(i == 0), stop=(i == n - 1))
        tok = E.mark(ins)
        self._record(tok, reads, writes)
        return tok

    def mm_multi(self, items, reads, writes):
        E = self.PE
        self._deps(E, reads, writes)
        ins = None
        for (o, l, r, st, sp) in items:
            ins = self.nc.tensor.matmul(o, l, r, start=st, stop=sp)
        tok = E.mark(ins)
        self._record(tok, reads, writes)
        return tok

    def transposes(self, items, ident, reads, writes):
        E = self.PE
        self._deps(E, reads, writes)
        ins = None
        for (o, i) in items:
            ins = self.nc.tensor.transpose(o, i, ident)
        tok = E.mark(ins)
        self._record(tok, reads, writes)
        return tok

    def dma(self, Q, out, in_, sbuf, reads=(), writes=()):
        self._deps(Q, reads, writes)
        ins = Q.eng.dma_start(out=out, in_=in_)
        sem = sbuf.sem()
        sbuf.dcnt += 16
        ins.then_inc(sem[0], 16)
        tok = Tok(sem[0], sem[1], sbuf.dcnt)
        self.dma_toks[sem[1]] = tok
        self._record(tok, reads, writes)
        return tok

    def barrier(self):
        toks = [Tok(e.sem[0], e.sem[1], e.cnt) for e in self.engs if e.cnt > 0]
        toks += list(self.dma_toks.values())
        for e in self.engs:
            for t in toks:
                e.wait(t)

    def act(self, out, in_, func, reads, writes, scale=1.0, bias=None, accum_out=None):
        kw = {}
        if bias is not None:
            kw["bias"] = bias
        if accum_out is not None:
            kw["accum_out"] = accum_out
        return self.op(self.ACT, lambda: self.nc.scalar.activation(out=out, in_=in_, func=func, scale=scale, **kw),
                       reads, writes)

    def tt(self, out, in0, in1, op, reads, writes):
        return self.op(self.DVE, lambda: self.nc.vector.tensor_tensor(out=out, in0=in0, in1=in1, op=op), reads, writes)

    def ts(self, out, in0, s1, s2, op0, op1, reads, writes):
        if s2 is None:
            return self.op(self.DVE, lambda: self.nc.vector.tensor_scalar(out=out, in0=in0, scalar1=s1, scalar2=None,
                                                                          op0=op0), reads, writes)
        return self.op(self.DVE, lambda: self.nc.vector.tensor_scalar(out=out, in0=in0, scalar1=s1, scalar2=s2,
                                                                      op0=op0, op1=op1), reads, writes)

    def stt(self, out, in0, scalar, in1, op0, op1, reads, writes):
        return self.op(self.DVE, lambda: self.nc.vector.scalar_tensor_tensor(out=out, in0=in0, scalar=scalar, in1=in1,
                                                                             op0=op0, op1=op1), reads, writes)

    def recip(self, out, in_, reads, writes):
        return self.op(self.DVE, lambda: self.nc.vector.reciprocal(out=out, in_=in_), reads, writes)

    def vcopy(self, out, in_, reads, writes):
        return self.op(self.DVE, lambda: self.nc.vector.tensor_copy(out=out, in_=in_), reads, writes)

    def acopy(self, out, in_, reads, writes):
        return self.op(self.ACT, lambda: self.nc.scalar.copy(out=out, in_=in_), reads, writes)

    def vmemset(self, ap, val, writes):
        return self.op(self.DVE, lambda: self.nc.vector.memset(ap, val), (), writes)

    def sb(self, st, name, shape, dt):
        self.nsb = getattr(self, "nsb", 0) + 1
        return st.enter_context(self.nc.sbuf_tensor(f"{name}_{self.nsb}", shape, dt))

    def declare(self, skip=()):
        nc = self.nc
        d = {}
        self.inputs_declared = []

        def inp(name, shape, dt=F32):
            if name in skip:
                return
            self.inputs_declared.append(name)
            d[name] = nc.dram_tensor(name, shape, dt, kind="ExternalInput").ap()

        inp("x", [S, D])
        inp("pos", [1, S], I32)
        inp("attn_norm_g", [DEPTH, D])
        inp("w_in", [DEPTH, D, IN_W])
        inp("q_norm_g", [DEPTH, HD])
        inp("k_norm_g", [DEPTH, HD])
        inp("lambda_vecs", [DEPTH, 4, HD])
        inp("attn_out_norm_g", [DEPTH, 2 * HD])
        inp("w_pool", [DEPTH, 4, 128, 128])
        inp("pool_scale", [DEPTH, 512])
        inp("conv_w", [DEPTH, 3, 512])
        inp("w_out", [DEPTH, D, D])
        inp("ffn_norm_g", [DEPTH, D])
        inp("dense_w_gate", [1, D, FF_D])
        inp("dense_w_up", [1, D, FF_D])
        inp("dense_w_down", [1, FF_D, D])
        inp("router_w", [1, D, NE])
        inp("moe_w_gate", [1, NE, D, FF_E])
        inp("moe_w_up", [1, NE, D, FF_E])
        inp("moe_w_down", [1, NE, FF_E, D])
        inp("c_ident", [128, 128])
        inp("c_perm", [128, 128])
        inp("c_tri", [128, 128])
        inp("c_invf", [128, 1])
        inp("c_invc", [128, 64])
        d["out"] = nc.dram_tensor("out", [S, D], F32, kind="ExternalOutput").ap()
        skind = "ExternalOutput" if self.debug else "Internal"
        d["qT"] = nc.dram_tensor("s_qT", [8, 128, S], BF16, kind=skind).ap()
        d["kT"] = nc.dram_tensor("s_kT", [8, 128, S], BF16, kind=skind).ap()
        d["V"] = nc.dram_tensor("s_V", [S, 1024], BF16, kind=skind).ap()
        d["mixT"] = nc.dram_tensor("s_mixT", [16, 128, S], BF16, kind=skind).ap()
        self.d = d

    def build(self, upto="all", skip=()):
        nc = self.nc
        self.declare(skip)
        d = self.d
        with self.es:
            gst = ExitStack()
            with gst:
                g = self.g = {}
                g["ident_b"] = self.sb(gst, "ident_b", [128, 128], BF16)
                g["ident_f"] = self.sb(gst, "ident_f", [128, 128], F32)
                g["perm_b"] = self.sb(gst, "perm_b", [128, 128], BF16)
                g["tri_b"] = self.sb(gst, "tri_b", [128, 128], BF16)
                g["ones_b"] = self.sb(gst, "ones_b", [128, 128], BF16)
                g["ones_f"] = self.sb(gst, "ones_f", [128, 128], F32)
                g["invf"] = self.sb(gst, "invf", [128, 1], F32)
                g["invc"] = self.sb(gst, "invc", [128, 64], F32)
                g["eps"] = self.sb(gst, "eps", [128, 1], F32)
                g["negpi"] = self.sb(gst, "negpi", [128, 1], F32)
                self.ps = [gst.enter_context(nc.psum_tensor(f"ps{i}", [128, 512], F32)) for i in range(8)]
                self.psb = [self.buf(f"ps{i}", persist=True) for i in range(8)]
                cb = self.cb = self.buf("consts", persist=True)
                self.dma(self.POOL, g["ident_b"][:], d["c_ident"][:, :], cb, writes=[cb])
                self.dma(self.SP, g["ident_f"][:], d["c_ident"][:, :], cb, writes=[cb])
                self.dma(self.POOL, g["perm_b"][:], d["c_perm"][:, :], cb, writes=[cb])
                self.dma(self.POOL, g["tri_b"][:], d["c_tri"][:, :], cb, writes=[cb])
                self.dma(self.SP, g["invf"][:], d["c_invf"][:, :], cb, writes=[cb])
                self.dma(self.SP, g["invc"][:], d["c_invc"][:, :], cb, writes=[cb])
                self.vmemset(g["ones_b"][:], 1.0, [cb])
                self.vmemset(g["ones_f"][:], 1.0, [cb])
                self.vmemset(g["eps"][:], EPS, [cb])
                self.vmemset(g["negpi"][:], -math.pi, [cb])
                self.barrier()
                self.scr = {n: self.buf("scr_" + n, merge=True, persist=True) for n in ("qT", "kT", "V", "mixT")}
                self.outb = [self.buf(f"out{t}", merge=True, persist=True) for t in range(NT)]
                for l in range(DEPTH):
                    src = d["x"] if l == 0 else d["out"]
                    self.phase_inproj(l, src)
                    self.end_phase()
                    if upto == f"inproj{l}":
                        break
                    self.phase_attn(l)
                    self.end_phase()
                    if upto == f"attn{l}":
                        break
                    self.phase_outproj(l, src)
                    self.end_phase()
                    if upto == f"outproj{l}":
                        break
                    self.phase_ffn(l)
                    self.end_phase()
                    if upto == f"ffn{l}":
                        break
                self.barrier()
        return nc

    def rotary_tables(self):
        nc, g, d, cb = self.nc, self.g, self.d, self.cb
        with ExitStack() as st:
            posi = self.sb(st, "posi", [128, S], I32)
            ang = self.sb(st, "ang", [128, S], F32)
            r = self.sb(st, "rr", [128, S], F32)
            kf = self.sb(st, "rkf", [128, S], F32)
            tb = self.buf("rot_tmp")
            self.dma(self.SP, posi[:], d["pos"][0:1, :].partition_broadcast(128), tb, writes=[tb])
            self.vcopy(ang[:], posi[:], [tb], [tb])
            self.ts(ang[:], ang[:], g["invf"][:, 0:1], None, ALU.mult, None, [tb, cb], [tb])
            two_pi = 2.0 * math.pi
            C1 = 6.28125
            C2 = two_pi - C1

            def reduce_(shift):
                if shift:
                    self.ts(r[:], ang[:], shift, None, ALU.add, None, [tb], [tb])
                else:
                    self.vcopy(r[:], ang[:], [tb], [tb])
                self.ts(kf[:], r[:], 1.0 / two_pi, None, ALU.mult, None, [tb], [tb])
                self.vcopy(posi[:], kf[:], [tb], [tb])
                self.vcopy(kf[:], posi[:], [tb], [tb])
                self.stt(r[:], kf[:], -C1, r[:], ALU.mult, ALU.add, [tb], [tb])
                self.stt(r[:], kf[:], -C2, r[:], ALU.mult, ALU.add, [tb], [tb])
                self.ts(kf[:], r[:], math.pi, -two_pi, ALU.is_gt, ALU.mult, [tb], [tb])
                self.tt(r[:], r[:], kf[:], ALU.add, [tb], [tb])
                self.ts(kf[:], r[:], -math.pi, two_pi, ALU.is_lt, ALU.mult, [tb], [tb])
                self.tt(r[:], r[:], kf[:], ALU.add, [tb], [tb])
                self.ts(r[:], r[:], 3.1415925, -3.1415925, ALU.min, ALU.max, [tb], [tb])

            reduce_(0.0)
            self.act(g["Sg"][:], r[:], AF.Sin, [tb, cb], [cb])
            self.ts(g["Sg"][0:64, :], g["Sg"][0:64, :], -1.0, None, ALU.mult, None, [cb], [cb])
            reduce_(0.5 * math.pi)
            self.act(g["C"][:], r[:], AF.Sin, [tb, cb], [cb])
            self.barrier()

    def load_col(self, dst_col, vec_ap, b):
        self.dma(self.SP, dst_col, vec_ap.rearrange("(p o) -> p o", o=1), b, writes=[b])

    def norm_tile(self, xt, xb, gB, gBb, hT, hTb, col0, tmp, idx, ident, route=None):
        g = self.g
        ssq, sb_ = tmp["ssq"][idx % 2]
        xs, xsb = tmp["xs"][idx % 2]
        junk, jb = xs, xsb
        self.act(junk[:], xt, AF.Square, [xb], [jb, sb_], accum_out=ssq[:, 0:1])
        self.act(ssq[:, 1:2], ssq[:, 0:1], AF.Sqrt, [sb_, self.cb], [sb_], scale=1.0 / D, bias=g["eps"][:, 0:1])
        self.recip(ssq[:, 2:3], ssq[:, 1:2], [sb_], [sb_])
        self.stt(xs[:], xt, ssq[:, 2:3], gB[:], ALU.mult, ALU.mult, [xb, sb_, gBb], [xsb])
        for g4 in range(4):
            pi = (idx * 4 + g4) % 4
            pst, psb = self.ps[pi], self.psb[pi]
            if route is None:
                pv = pst[:].bitcast(BF16)
                items = [(pv[:, j * 128:(j + 1) * 128], xs[:, (g4 * 4 + j) * 128:(g4 * 4 + j + 1) * 128])
                         for j in range(4)]
                self.transposes(items, ident, [xsb, self.cb], [psb])
                src = pv[:, 0:512].rearrange("p (c t) -> p c t", c=4)
            else:
                items = [(pst[:, j * 128:(j + 1) * 128], xs[:, (g4 * 4 + j) * 128:(g4 * 4 + j + 1) * 128])
                         for j in range(4)]
                self.transposes(items, ident, [xsb, self.cb], [psb])
                src = pst[:, 0:512].rearrange("p (c t) -> p c t", c=4)
            dst = hT[:, g4 * 4:(g4 + 1) * 4, col0:col0 + 128]
            if route is None:
                if g4 % 2 == 0:
                    self.vcopy(dst, src, [psb], [hTb])
                else:
                    self.acopy(dst, src, [psb], [hTb])
            else:
                h32, h32b = route["h32"][g4 % 2]
                self.vcopy(h32[:], src, [psb], [h32b])
                self.acopy(dst, h32[:], [h32b], [hTb])
                lp, lpb = self.ps[4], self.psb[4]
                items = [(lp[:, 0:NE], h32[:, j, :], route["rw"][:, g4 * 4 + j, :], (g4 == 0 and j == 0),
                          (g4 == 3 and j == 3)) for j in range(4)]
                if route.get("level", 3) >= 2:
                    self.mm_multi(items, [h32b, route["rwb"]], [lpb])
        if route is not None and route.get("level", 3) >= 3:
            self.route_tile(route, idx)

    def route_tile(self, route, idx):
        lp, lpb = self.ps[4], self.psb[4]
        lg, lgb = route["lg"][idx % 2]
        comb, combb = route["comb"], route["combb"]
        self.vcopy(lg[:, 0:8], lp[:, 0:NE], [lpb], [lgb])
        self.op(self.DVE, lambda: self.nc.vector.max(out=lg[:, 8:16], in_=lg[:, 0:8]), [lgb], [lgb])
        self.tt(lg[:, 16:17], lg[:, 8:9], lg[:, 9:10], ALU.subtract, [lgb], [lgb])
        self.act(lg[:, 17:18], lg[:, 16:17], AF.Sigmoid, [lgb], [lgb])
        self.ts(lg[:, 18:19], lg[:, 17:18], -1.0, 1.0, ALU.mult, ALU.add, [lgb], [lgb])
        self.ts(lg[:, 24:32], lg[:, 0:8], lg[:, 8:9], lg[:, 17:18], ALU.is_equal, ALU.mult, [lgb], [lgb])
        self.ts(lg[:, 32:40], lg[:, 0:8], lg[:, 9:10], lg[:, 18:19], ALU.is_equal, ALU.mult, [lgb], [lgb])
        self.tt(comb[:, idx, :], lg[:, 24:32], lg[:, 32:40], ALU.add, [lgb], [combb])

    def phase_inproj(self, l, src):
        nc, g, d = self.nc, self.g, self.d
        with ExitStack() as st:
            hT = self.sb(st, "hT", [128, 16, S], BF16)
            hTb = self.buf("hT")
            g["C"] = self.sb(st, "rotC", [128, S], F32)
            g["Sg"] = self.sb(st, "rotS", [128, S], F32)
            self.rotary_tables()
            with ExitStack() as st2:
                gB = self.sb(st2, "gB", [128, D], F32)
                gBb = self.buf("gB")
                self.dma(self.SP, gB[:], d["attn_norm_g"][l:l + 1, :].partition_broadcast(128), gBb, writes=[gBb])
                xts = [(self.sb(st2, f"xt{i}", [128, D], F32), self.buf(f"xt{i}")) for i in range(3)]
                tmp = {
                    "ssq": [(self.sb(st2, f"ssq{i}", [128, 4], F32), self.buf(f"ssq{i}")) for i in range(2)],
                    "xs": [(self.sb(st2, f"xs{i}", [128, D], BF16), self.buf(f"xs{i}")) for i in range(2)],
                }
                for tt in range(NT):
                    xt, xb = xts[tt % 3]
                    rd = [self.outb[tt]] if l > 0 else []
                    self.dma(self.SP, xt[:], src[tt * 128:(tt + 1) * 128, :], xb, reads=rd, writes=[xb])
                    self.norm_tile(xt[:], xb, gB, gBb, hT, hTb, tt * 128, tmp, tt, g["ident_b"][:])
                self.barrier()
            self.inproj_body(l, hT, hTb, st)

    def inproj_body(self, l, hT, hTb, st):
        nc, g, d = self.nc, self.g, self.d
        cb = self.cb
        W = d["w_in"]
        wsl = [(self.sb(st, f"wi{i}", [128, 16, 512], BF16), self.buf(f"wi{i}")) for i in range(2)]
        F = [(self.sb(st, f"F{i}", [128, 16 + S], F32), self.buf(f"F{i}")) for i in range(4)]
        sm = [(self.sb(st, f"sm{i}", [128, 512], F32), self.buf(f"sm{i}")) for i in range(6)]
        smb = [(self.sb(st, f"smb{i}", [128, 512], BF16), self.buf(f"smb{i}")) for i in range(4)]
        stg = [(self.sb(st, f"stg{i}", [128, S], BF16), self.buf(f"stg{i}")) for i in range(2)]
        vst = [(self.sb(st, f"vst{i}", [128, 512], BF16), self.buf(f"vst{i}")) for i in range(3)]
        pv = self.sb(st, "pvec", [128, 16], F32)
        pvb = self.buf("pvec")
        wp = self.sb(st, "wpool", [128, 4, 128], BF16)
        wpb = self.buf("wpool")
        self.load_col(pv[:, 0:1], d["q_norm_g"][l, :], pvb)
        self.load_col(pv[:, 1:2], d["k_norm_g"][l, :], pvb)
        for c in range(4):
            self.load_col(pv[:, 2 + c:3 + c], d["pool_scale"][l, c * 128:(c + 1) * 128], pvb)
        self.cw = self.sb(st, "convw", [128, 12], F32)
        for j in range(3):
            for c in range(4):
                self.load_col(self.cw[:, j * 4 + c:j * 4 + c + 1], d["conv_w"][l, j, c * 128:(c + 1) * 128], pvb)
        self.dma(self.POOL, wp[:], d["w_pool"][l].rearrange("g c d -> c g d"), wpb, writes=[wpb])
        for i in range(4):
            self.vmemset(F[i][0][:, 0:16], 0.0, [F[i][1]])

        units = []
        for c in range(8):
            units.append(("q", c, [(QOFF + c * 128, 128)]))
        for c in range(8):
            units.append(("k", c, [(KOFF + c * 128, 128)]))
        for vb in range(2):
            units.append(("v", vb, [(VOFF + vb * 512, 512)]))
        for c in range(4):
            units.append(("pool", c, [(POFF + c * 128, 128)]))
        for c in range(4):
            units.append(("conv", c, [(BOFF + c * 128, 128), (COFF + c * 128, 128), (UOFF + c * 128, 128)]))

        def load(ui):
            kind, idx, cols = units[ui]
            wt, wb = wsl[ui % 2]
            o = 0
            for (c0, n) in cols:
                self.dma(self.POOL, wt[:, :, o:o + n], W[l, :, c0:c0 + n].rearrange("(c p) n -> p c n", p=128), wb,
                         writes=[wb])
                o += n

        load(0)
        rr = [0]

        def nxt(lst):
            rr[0] += 1
            return lst[rr[0] % len(lst)]

        pcount = [0]

        def psn():
            pcount[0] += 1
            i = pcount[0] % 8
            return self.ps[i], self.psb[i]

        for ui, (kind, idx, cols) in enumerate(units):
            if ui + 1 < len(units):
                load(ui + 1)
            wt, wb = wsl[ui % 2]
            if kind in ("q", "k"):
                gcol = pv[:, 0:1] if kind == "q" else pv[:, 1:2]
                sg, sgb = nxt(stg)
                def emit_main(tb_):
                    pz_, pzb_ = psn()
                    self.mm(pz_[:], [(wt[:, kc, 0:128], hT[:, kc, tb_ * 512:(tb_ + 1) * 512]) for kc in range(16)],
                            [wb, hTb], [pzb_])
                    return pz_, pzb_

                nxt_main = emit_main(0)
                for tb in range(4):
                    tsl = slice(tb * 512, (tb + 1) * 512)
                    pz, pzb = nxt_main
                    if tb + 1 < 4:
                        nxt_main = emit_main(tb + 1)
                    qg, qgb = nxt(smb)
                    sq, sqb = nxt(smb)
                    self.act(qg[:], pz[:], AF.Copy, [pzb, pvb], [qgb], scale=gcol)
                    self.act(sq[:], pz[:], AF.Square, [pzb], [sqb])
                    pss, pssb = psn()
                    self.mm(pss[:], [(g["ones_b"][:], sq[:])], [sqb, cb], [pssb])
                    ppq, ppqb = psn()
                    self.mm(ppq[:], [(g["perm_b"][:], qg[:])], [qgb, cb], [ppqb])
                    rs, rsb = nxt(sm)
                    self.act(rs[:], pss[:], AF.Sqrt, [pssb, cb], [rsb], scale=1.0 / HD, bias=g["eps"][:, 0:1])
                    self.recip(rs[:], rs[:], [rsb], [rsb])
                    t1, t1b = nxt(sm)
                    t2, t2b = nxt(sm)
                    self.tt(t1[:], qg[:], g["C"][:, tsl], ALU.mult, [qgb, cb], [t1b])
                    self.tt(t2[:], ppq[:], g["Sg"][:, tsl], ALU.mult, [ppqb, cb], [t2b])
                    self.tt(t1[:], t1[:], t2[:], ALU.add, [t1b, t2b], [t1b])
                    self.tt(sg[:, tsl], t1[:], rs[:], ALU.mult, [t1b, rsb], [sgb])
                dst = d["qT"] if kind == "q" else d["kT"]
                self.dma(self.SP, dst[idx, :, :], sg[:], sgb, reads=[sgb], writes=[self.scr["qT" if kind == "q" else "kT"]])
            elif kind == "v":
                for tt in range(NT):
                    pz, pzb = psn()
                    self.mm(pz[:], [(hT[:, kc, tt * 128:(tt + 1) * 128], wt[:, kc, 0:512]) for kc in range(16)],
                            [wb, hTb], [pzb])
                    vs, vsb = nxt(vst)
                    if tt % 2 == 0:
                        self.vcopy(vs[:], pz[:], [pzb], [vsb])
                    else:
                        self.acopy(vs[:], pz[:], [pzb], [vsb])
                    self.dma(self.SP, d["V"][tt * 128:(tt + 1) * 128, idx * 512:(idx + 1) * 512], vs[:], vsb,
                             reads=[vsb], writes=[self.scr["V"]])
            elif kind == "pool":
                G, Gb = F[0]
                for tb in range(4):
                    pz, pzb = psn()
                    self.mm(pz[:], [(wt[:, kc, 0:128], hT[:, kc, tb * 512:(tb + 1) * 512]) for kc in range(16)],
                            [wb, hTb], [pzb])
                    self.acopy(G[:, 16 + tb * 512:16 + (tb + 1) * 512], pz[:], [pzb], [Gb])
                w = 2 << idx
                cur, curb = G, Gb
                sh = 1
                pp = 1
                while sh < w:
                    nx, nxb = F[pp]
                    self.tt(nx[:, 16:16 + S], cur[:, 16:16 + S], cur[:, 16 - sh:16 - sh + S], ALU.add, [curb], [nxb])
                    cur, curb = nx, nxb
                    pp = 3 - pp
                    sh *= 2
                po, pob = F[3]
                self.stt(po[:, 16:16 + S], cur[:, 16:16 + S], 1.0 / w, G[:, 16:16 + S], ALU.mult, ALU.subtract,
                         [curb, Gb], [pob])
                self.tt(po[:, 0:16], cur[:, 16:32], g["invc"][:, idx * 16:(idx + 1) * 16], ALU.mult, [curb, cb], [pob])
                self.tt(po[:, 16:32], po[:, 0:16], G[:, 16:32], ALU.subtract, [pob, Gb], [pob])
                pbf, pbfb = nxt(stg)
                self.vcopy(pbf[:], po[:, 16:16 + S], [pob], [pbfb])
                sg, sgb = nxt(stg)
                for tb in range(4):
                    pz, pzb = psn()
                    self.mm(pz[:], [(wp[:, idx, :], pbf[:, tb * 512:(tb + 1) * 512])], [wpb, pbfb], [pzb])
                    self.act(sg[:, tb * 512:(tb + 1) * 512], pz[:], AF.Copy, [pzb, pvb], [sgb],
                             scale=pv[:, 2 + idx:3 + idx])
                self.dma(self.SP, d["mixT"][8 + idx, :, :], sg[:], sgb, reads=[sgb], writes=[self.scr["mixT"]])
            else:
                U, Ub = F[0]
                Bt, Bb = F[1]
                Y, Yb = F[2]
                for tb in range(4):
                    tsl = slice(tb * 512, (tb + 1) * 512)
                    pB, pBb = psn()
                    self.mm(pB[:], [(wt[:, kc, 0:128], hT[:, kc, tsl]) for kc in range(16)], [wb, hTb], [pBb])
                    pC, pCb = psn()
                    self.mm(pC[:], [(wt[:, kc, 128:256], hT[:, kc, tsl]) for kc in range(16)], [wb, hTb], [pCb])
                    pH, pHb = psn()
                    self.mm(pH[:], [(wt[:, kc, 256:384], hT[:, kc, tsl]) for kc in range(16)], [wb, hTb], [pHb])
                    self.acopy(Bt[:, 16 + tb * 512:16 + (tb + 1) * 512], pB[:], [pBb], [Bb])
                    cc, ccb = nxt(sm)
                    self.acopy(cc[:], pC[:], [pCb], [ccb])
                    self.tt(U[:, 16 + tb * 512:16 + (tb + 1) * 512], pH[:], cc[:], ALU.mult, [pHb, ccb], [Ub])
                cw = self.cw
                self.ts(Y[:, 16:16 + S], U[:, 16:16 + S], cw[:, 8 + idx:9 + idx], None, ALU.mult, None, [Ub, pvb], [Yb])
                self.stt(Y[:, 16:16 + S], U[:, 15:15 + S], cw[:, 4 + idx:5 + idx], Y[:, 16:16 + S], ALU.mult, ALU.add,
                         [Ub, pvb, Yb], [Yb])
                self.stt(Y[:, 16:16 + S], U[:, 14:14 + S], cw[:, idx:idx + 1], Y[:, 16:16 + S], ALU.mult, ALU.add,
                         [Ub, pvb, Yb], [Yb])
                sg, sgb = nxt(stg)
                self.tt(sg[:], Y[:, 16:16 + S], Bt[:, 16:16 + S], ALU.mult, [Yb, Bb], [sgb])
                self.dma(self.SP, d["mixT"][12 + idx, :, :], sg[:], sgb, reads=[sgb], writes=[self.scr["mixT"]])

    def phase_attn(self, l):
        nc, g, d = self.nc, self.g, self.d
        cb = self.cb
        lam_init = 0.8 - 0.6 * math.exp(-0.3 * l)
        scale = HD ** -0.5
        with ExitStack() as st:
            lv = self.sb(st, "lv", [128, 16], F32)
            lvb = self.buf("lv")
            for j in range(4):
                self.load_col(lv[:, j:j + 1], d["lambda_vecs"][l, j, :], lvb)
            for c in range(2):
                self.load_col(lv[:, 4 + c:5 + c], d["attn_out_norm_g"][l, c * 128:(c + 1) * 128], lvb)
            self.tt(lv[:, 6:7], lv[:, 0:1], lv[:, 1:2], ALU.mult, [lvb], [lvb])
            self.tt(lv[:, 7:8], lv[:, 2:3], lv[:, 3:4], ALU.mult, [lvb], [lvb])
            p0, p0b = self.ps[0], self.psb[0]
            self.mm(p0[:, 0:2], [(g["ones_f"][:], lv[:, 6:8])], [lvb, cb], [p0b])
            self.act(lv[:, 8:10], p0[:, 0:2], AF.Exp, [p0b], [lvb])
            self.tt(lv[:, 10:11], lv[:, 8:9], lv[:, 9:10], ALU.subtract, [lvb], [lvb])
            self.ts(lv[:, 11:12], lv[:, 10:11], lam_init, -1.0, ALU.add, ALU.mult, [lvb], [lvb])
            self.ts(lv[:, 12:14], lv[:, 4:6], 1.0 - lam_init, None, ALU.mult, None, [lvb], [lvb])

            qh = [(self.sb(st, f"qh{i}", [128, 2, S], BF16), self.buf(f"qh{i}")) for i in range(2)]
            kh = [(self.sb(st, f"kh{i}", [128, 2, S], BF16), self.buf(f"kh{i}")) for i in range(2)]
            vh = [(self.sb(st, f"vh{i}", [128, 16, 256], BF16), self.buf(f"vh{i}")) for i in range(2)]
            pT = [(self.sb(st, f"pT{i}", [128, 512], BF16), self.buf(f"pT{i}")) for i in range(4)]
            rl = [(self.sb(st, f"rl{i}", [128, 512], F32), self.buf(f"rl{i}")) for i in range(2)]
            o1 = [(self.sb(st, f"o1{i}", [128, 512], F32), self.buf(f"o1{i}")) for i in range(2)]
            oo = [(self.sb(st, f"oo{i}", [128, 512], F32), self.buf(f"oo{i}")) for i in range(2)]
            sq = [(self.sb(st, f"sq{i}", [128, 512], BF16), self.buf(f"sq{i}")) for i in range(2)]
            rs, rsb = self.sb(st, "ars", [128, 512], F32), self.buf("ars")
            ast = [(self.sb(st, f"ast{i}", [128, S], BF16), self.buf(f"ast{i}")) for i in range(4)]

            def loadh(h):
                q, qb_ = qh[h % 2]
                k_, kb_ = kh[h % 2]
                v, vb_ = vh[h % 2]
                self.dma(self.SP, q[:], d["qT"][2 * h:2 * h + 2, :, :].rearrange("m p s -> p m s"), qb_,
                         reads=[self.scr["qT"]], writes=[qb_])
                self.dma(self.SP, k_[:], d["kT"][2 * h:2 * h + 2, :, :].rearrange("m p s -> p m s"), kb_,
                         reads=[self.scr["kT"]], writes=[kb_])
                self.dma(self.SP, v[:], d["V"][:, h * 256:(h + 1) * 256].rearrange("(t p) e -> p t e", p=128), vb_,
                         reads=[self.scr["V"]], writes=[vb_])

            loadh(0)
            pcnt = 0
            for h in range(NH):
                if h + 1 < NH:
                    loadh(h + 1)
                q, qb_ = qh[h % 2]
                k_, kb_ = kh[h % 2]
                v, vb_ = vh[h % 2]
                for qb in range(4):
                    q0 = qb * 512
                    nkt = 4 * qb + 4
                    steps = [(m, kt) for m in range(2) for kt in range(nkt)]

                    def emit_sc(si):
                        m, kt = steps[si]
                        c0 = max(kt - 4 * qb, 0) * 128
                        gi = pcnt0 + si
                        sc, scb = self.ps[gi % 2], self.psb[gi % 2]
                        self.mm(sc[:, c0:512], [(k_[:, m, kt * 128:(kt + 1) * 128], q[:, m, q0 + c0:q0 + 512])],
                                [kb_, qb_], [scb])

                    pcnt0 = pcnt
                    emit_sc(0)
                    for si, (m, kt) in enumerate(steps):
                        if si + 1 < len(steps):
                            emit_sc(si + 1)
                        O0, O0b = self.ps[2 + 3 * m], self.psb[2 + 3 * m]
                        O1, O1b = self.ps[3 + 3 * m], self.psb[3 + 3 * m]
                        L, Lb = self.ps[4 + 3 * m], self.psb[4 + 3 * m]
                        j = kt - 4 * qb
                        c0 = max(j, 0) * 128
                        gi = pcnt0 + si
                        sc, scb = self.ps[gi % 2], self.psb[gi % 2]
                        p, pb = pT[gi % 4]
                        self.act(p[:, c0:512], sc[:, c0:512], AF.Exp, [scb], [pb], scale=scale)
                        if j >= 0:
                            self.tt(p[:, c0:c0 + 128], p[:, c0:c0 + 128], g["tri_b"][:], ALU.mult, [pb, cb], [pb])
                        first, last = (kt == 0), (kt == nkt - 1)
                        items = [
                            (O0[:, c0:512], v[:, kt, 0:128], p[:, c0:512], first, last),
                            (O1[:, c0:512], v[:, kt, 128:256], p[:, c0:512], first, last),
                            (L[:, c0:512], g["ones_b"][:], p[:, c0:512], first, last),
                        ]
                        self.mm_multi(items, [vb_, pb, cb], [O0b, O1b, Lb])
                    pcnt = pcnt0 + len(steps)
                    for m in range(2):
                        L, Lb = self.ps[4 + 3 * m], self.psb[4 + 3 * m]
                        self.recip(rl[m][0][:], L[:], [Lb], [rl[m][1]])
                    for ec in range(2):
                        A, Ab = self.ps[2 + ec], self.psb[2 + ec]
                        Bm, Bmb = self.ps[5 + ec], self.psb[5 + ec]
                        self.tt(o1[ec][0][:], A[:], rl[0][0][:], ALU.mult, [Ab, rl[0][1]], [o1[ec][1]])
                        self.tt(oo[ec][0][:], Bm[:], rl[1][0][:], ALU.mult, [Bmb, rl[1][1]], [oo[ec][1]])
                        self.stt(oo[ec][0][:], oo[ec][0][:], lv[:, 11:12], o1[ec][0][:], ALU.mult, ALU.add,
                                 [oo[ec][1], o1[ec][1], lvb], [oo[ec][1]])
                        self.act(sq[ec][0][:], oo[ec][0][:], AF.Square, [oo[ec][1]], [sq[ec][1]])
                    ssn, ssnb = self.ps[pcnt % 2], self.psb[pcnt % 2]
                    pcnt += 1
                    self.mm(ssn[:], [(g["ones_b"][:], sq[0][0][:]), (g["ones_b"][:], sq[1][0][:])],
                            [sq[0][1], sq[1][1], cb], [ssnb])
                    self.act(rs[:], ssn[:], AF.Sqrt, [ssnb, cb], [rsb], scale=1.0 / (2 * HD), bias=g["eps"][:, 0:1])
                    self.recip(rs[:], rs[:], [rsb], [rsb])
                    for ec in range(2):
                        a, ab = ast[(h % 2) * 2 + ec]
                        self.stt(a[:, q0:q0 + 512], oo[ec][0][:], lv[:, 12 + ec:13 + ec], rs[:], ALU.mult, ALU.mult,
                                 [oo[ec][1], rsb, lvb], [ab])
                for ec in range(2):
                    a, ab = ast[(h % 2) * 2 + ec]
                    self.dma(self.SP, d["mixT"][2 * h + ec, :, :], a[:], ab, reads=[ab], writes=[self.scr["mixT"]])

    def phase_outproj(self, l, src):
        nc, g, d = self.nc, self.g, self.d
        with ExitStack() as st:
            mix = self.sb(st, "mix", [128, 16, S], BF16)
            mixb = self.buf("mix")
            for c4 in range(4):
                self.dma(self.SP, mix[:, c4 * 4:(c4 + 1) * 4, :], d["mixT"][c4 * 4:(c4 + 1) * 4, :, :].rearrange("c p s -> p c s"),
                         mixb, reads=[self.scr["mixT"]], writes=[mixb])
            wsl = [(self.sb(st, f"wo{i}", [128, 16, 512], BF16), self.buf(f"wo{i}")) for i in range(2)]
            xts = [(self.sb(st, f"xo{i}", [128, 512], F32), self.buf(f"xo{i}")) for i in range(4)]

            def loadw(db):
                wt, wb = wsl[db % 2]
                self.dma(self.POOL, wt[:], d["w_out"][l, :, db * 512:(db + 1) * 512].rearrange("(c p) n -> p c n", p=128),
                         wb, writes=[wb])

            units = [(db, tt) for db in range(4) for tt in range(NT)]

            def loadx(ui):
                db, tt = units[ui]
                xt, xb = xts[ui % 4]
                rd = [self.outb[tt]] if l > 0 else []
                self.dma(self.SP, xt[:], src[tt * 128:(tt + 1) * 128, db * 512:(db + 1) * 512], xb, reads=rd, writes=[xb])

            loadw(0)
            loadx(0)
            loadx(1)
            for ui, (db, tt) in enumerate(units):
                if tt == 0 and db + 1 < 4:
                    loadw(db + 1)
                if ui + 2 < len(units):
                    loadx(ui + 2)
                wt, wb = wsl[db % 2]
                xt, xb = xts[ui % 4]
                pz, pzb = self.ps[ui % 4], self.psb[ui % 4]
                self.mm(pz[:], [(mix[:, kc, tt * 128:(tt + 1) * 128], wt[:, kc, :]) for kc in range(16)], [mixb, wb], [pzb])
                self.tt(xt[:], xt[:], pz[:], ALU.add, [xb, pzb], [xb])
                self.dma(self.SP, d["out"][tt * 128:(tt + 1) * 128, db * 512:(db + 1) * 512], xt[:], xb, reads=[xb],
                         writes=[self.outb[tt]])

    def phase_ffn(self, l):
        nc, g, d = self.nc, self.g, self.d
        cb = self.cb
        moe = (l % 2 == 1)
        j = l // 2
        ne = NE if moe else 1
        ff = FF_E if moe else FF_D
        nfb = ff // 256
        if self.debug and isinstance(self.debug, dict):
            ne = min(ne, self.debug.get("ne", ne))
            nfb = min(nfb, self.debug.get("nfb", nfb))
        with ExitStack() as st:
            gB = self.sb(st, "gBf", [128, D], F32)
            gBb = self.buf("gBf")
            self.dma(self.SP, gB[:], d["ffn_norm_g"][l:l + 1, :].partition_broadcast(128), gBb, writes=[gBb])
            acc = [(self.sb(st, f"acc{i}", [128, D], F32), self.buf(f"acc{i}")) for i in range(8)]
            hT = self.sb(st, "h2T", [128, 16, 1024], BF16)
            hTb = self.buf("h2T")
            wg = [(self.sb(st, f"wg{i}", [128, 16, 256], BF16), self.buf(f"wg{i}")) for i in range(2)]
            wu = [(self.sb(st, f"wu{i}", [128, 16, 256], BF16), self.buf(f"wu{i}")) for i in range(2)]
            wd = [(self.sb(st, f"wd{i}", [128, 2, D], BF16), self.buf(f"wd{i}")) for i in range(2)]
            sg = [(self.sb(st, f"sg{i}", [128, 512], F32), self.buf(f"sg{i}")) for i in range(2)]
            at = [(self.sb(st, f"at{i}", [128, 512], BF16), self.buf(f"at{i}")) for i in range(4)]
            tmp = {
                "ssq": [(self.sb(st, f"ssqf{i}", [128, 4], F32), self.buf(f"ssqf{i}")) for i in range(2)],
            }
            route = None
            use_route = moe and not (isinstance(self.debug, dict) and self.debug.get("noroute"))
            if not use_route and moe:
                tmp["xs"] = [(self.sb(st, f"xsf{i}", [128, D], BF16), self.buf(f"xsf{i}")) for i in range(2)]
            if use_route:
                tmp["xs"] = [(self.sb(st, f"xsf{i}", [128, D], F32), self.buf(f"xsf{i}")) for i in range(1)] * 2
                route = {
                    "h32": [(self.sb(st, f"h32{i}", [128, 4, 128], F32), self.buf(f"h32{i}")) for i in range(2)],
                    "rw": self.sb(st, "rw", [128, 16, NE], F32),
                    "rwb": self.buf("rw"),
                    "lg": [(self.sb(st, f"lg{i}", [128, 40], F32), self.buf(f"lg{i}")) for i in range(2)],
                    "comb": self.sb(st, "comb", [128, 8, NE], F32),
                    "combb": self.buf("comb"),
                }
                for c in range(16):
                    self.dma(self.POOL, route["rw"][:, c, :], d["router_w"][j, c * 128:(c + 1) * 128, :], route["rwb"],
                             writes=[route["rwb"]])
                if isinstance(self.debug, dict):
                    route["level"] = self.debug.get("level", 3)
            else:
                tmp["xs"] = [(self.sb(st, f"xsf{i}", [128, D], BF16), self.buf(f"xsf{i}")) for i in range(2)]

            if moe:
                Wg = lambda e, fb: d["moe_w_gate"][j, e, :, fb * 256:(fb + 1) * 256]
                Wu = lambda e, fb: d["moe_w_up"][j, e, :, fb * 256:(fb + 1) * 256]
                Wd = lambda e, fb: d["moe_w_down"][j, e, fb * 256:(fb + 1) * 256, :]
            else:
                Wg = lambda e, fb: d["dense_w_gate"][j, :, fb * 256:(fb + 1) * 256]
                Wu = lambda e, fb: d["dense_w_up"][j, :, fb * 256:(fb + 1) * 256]
                Wd = lambda e, fb: d["dense_w_down"][j, fb * 256:(fb + 1) * 256, :]

            for half in range(2):
                units = [(e, fb) for e in range(ne) for fb in range(nfb)]

                def load_gu(ui):
                    if ui >= len(units):
                        return
                    e, fb = units[ui]
                    s_ = ui % 2
                    self.dma(self.POOL, wg[s_][0][:], Wg(e, fb).rearrange("(c p) n -> p c n", p=128), wg[s_][1],
                             writes=[wg[s_][1]])
                    self.dma(self.POOL, wu[s_][0][:], Wu(e, fb).rearrange("(c p) n -> p c n", p=128), wu[s_][1],
                             writes=[wu[s_][1]])

                def load_d(ui):
                    if ui >= len(units):
                        return
                    e, fb = units[ui]
                    s_ = ui % 2
                    self.dma(self.POOL, wd[s_][0][:], Wd(e, fb).rearrange("(c p) n -> p c n", p=128), wd[s_][1],
                             writes=[wd[s_][1]])

                load_gu(0)
                load_d(0)
                load_gu(1)
                load_d(1)
                for i in range(8):
                    tt = half * 8 + i
                    self.dma(self.SP, acc[i][0][:], d["out"][tt * 128:(tt + 1) * 128, :], acc[i][1],
                             reads=[self.outb[tt]], writes=[acc[i][1]])
                for i in range(8):
                    self.norm_tile(acc[i][0][:], acc[i][1], gB, gBb, hT, hTb, i * 128, tmp, i,
                                   g["ident_f"][:] if use_route else g["ident_b"][:], route=route)
                steps = [(ui, tb) for ui in range(len(units)) for tb in range(2)]
                acts_of = {}

                def emit_gu(si):
                    ui, tb = steps[si]
                    s_ = ui % 2
                    wgt, wgb = wg[s_]
                    wut, wub = wu[s_]
                    tsl = slice(tb * 512, (tb + 1) * 512)
                    acts = []
                    for c in range(2):
                        pg, pgb = self.ps[c], self.psb[c]
                        pu, pub = self.ps[2 + c], self.psb[2 + c]
                        self.mm(pg[:], [(wgt[:, kc, c * 128:(c + 1) * 128], hT[:, kc, tsl]) for kc in range(16)],
                                [wgb, hTb], [pgb])
                        self.mm(pu[:], [(wut[:, kc, c * 128:(c + 1) * 128], hT[:, kc, tsl]) for kc in range(16)],
                                [wub, hTb], [pub])
                        sgt, sgb_ = sg[c]
                        self.act(sgt[:], pg[:], AF.Silu, [pgb], [sgb_])
                        a, ab = at[(tb * 2 + c) % 4]
                        self.tt(a[:], pu[:], sgt[:], ALU.mult, [pub, sgb_], [ab])
                        acts.append((a, ab))
                    acts_of[si] = acts
                    if tb == 1:
                        load_gu(ui + 2)

                pc = 0
                emit_gu(0)
                for si, (ui, tb) in enumerate(steps):
                    if si + 1 < len(steps):
                        emit_gu(si + 1)
                    e, fb = units[ui]
                    wdt, wdb = wd[ui % 2]
                    acts = acts_of.pop(si)
                    for t4 in range(4):
                        ti = tb * 4 + t4
                        for dh in range(2):
                            pi = 4 + 2 * (pc % 2)
                            pc += 1
                            items = []
                            for db in range(2):
                                for c in range(2):
                                    items.append((self.ps[pi + db][:], acts[c][0][:, t4 * 128:(t4 + 1) * 128],
                                                  wdt[:, c, dh * 1024 + db * 512:dh * 1024 + (db + 1) * 512],
                                                  c == 0, c == 1))
                            self.mm_multi(items, [acts[0][1], acts[1][1], wdb], [self.psb[pi], self.psb[pi + 1]])
                            for db in range(2):
                                dsl = slice(dh * 1024 + db * 512, dh * 1024 + (db + 1) * 512)
                                a_t, a_b = acc[ti]
                                if use_route and route.get("level", 3) >= 3:
                                    self.stt(a_t[:, dsl], self.ps[pi + db][:], route["comb"][:, ti, e:e + 1],
                                             a_t[:, dsl], ALU.mult, ALU.add,
                                             [self.psb[pi + db], route["combb"], a_b], [a_b])
                                else:
                                    self.tt(a_t[:, dsl], self.ps[pi + db][:], a_t[:, dsl], ALU.add,
                                            [self.psb[pi + db], a_b], [a_b])
                    if tb == 1:
                        load_d(ui + 2)
                for i in range(8):
                    tt = half * 8 + i
                    self.dma(self.SP, d["out"][tt * 128:(tt + 1) * 128, :], acc[i][0][:], acc[i][1], reads=[acc[i][1]],
                             writes=[self.outb[tt]])
                self.barrier()


def _consts():
    ident = np.eye(128, dtype=np.float32)
    perm = np.zeros((128, 128), np.float32)
    for p in range(128):
        perm[p, (p + 64) % 128] = 1.0
    tri = (np.arange(128)[:, None] <= np.arange(128)[None, :]).astype(np.float32)
    jj = np.arange(0, HD, 2, dtype=np.float32) / np.float32(HD)
    invf64 = (1.0 / (np.float32(10000.0) ** jj)).astype(np.float32)
    invf = np.concatenate([invf64, invf64]).reshape(128, 1).astype(np.float32)
    invc = np.zeros((128, 64), np.float32)
    for gi, w in enumerate((2, 4, 8, 16)):
        invc[:, gi * 16:(gi + 1) * 16] = 1.0 / np.minimum(np.arange(16) + 1, w).astype(np.float32)
    return {"c_ident": ident, "c_perm": perm, "c_tri": tri, "c_invf": invf, "c_invc": invc}


_WNAMES = ["attn_norm_g", "w_in", "q_norm_g", "k_norm_g", "lambda_vecs", "attn_out_norm_g", "w_pool", "pool_scale",
           "conv_w", "w_out", "ffn_norm_g", "dense_w_gate", "dense_w_up", "dense_w_down", "router_w", "moe_w_gate",
           "moe_w_up", "moe_w_down"]


def make_in_maps(inputs, n_cores=N_CORES):
    consts = _consts()
    shared = {n: np.ascontiguousarray(np.asarray(inputs[n], dtype=np.float32)) for n in _WNAMES}
    shared.update(consts)
    x = np.asarray(inputs["x"], dtype=np.float32)
    pos = np.asarray(inputs["positions"], dtype=np.int32)
    maps = []
    for c in range(n_cores):
        m = dict(shared)
        m["x"] = np.ascontiguousarray(x[c])
        m["pos"] = np.ascontiguousarray(pos[c].reshape(1, S))
        maps.append(m)
    return maps


def kernel(**inputs):
    k = K()
    nc = k.build()
    in_maps = make_in_maps(inputs)
    res = run_bass_kernel_spmd(nc, in_maps, core_ids=list(range(N_CORES)))
    return np.stack([np.asarray(r["out"], dtype=np.float32) for r in res.results], axis=0)
```

```python
import math
from contextlib import ExitStack

import numpy as np
import concourse.bass as bass
import concourse.mybir as mybir
from concourse.bass_utils import run_bass_kernel_spmd

F32 = mybir.dt.float32
BF16 = mybir.dt.bfloat16
I32 = mybir.dt.int32
AF = mybir.ActivationFunctionType
ALU = mybir.AluOpType
AX = mybir.AxisListType

S = 2048
D = 2048
NT = S // 128
DEPTH = 2
HD = 128
NH = 4
IN_W = 5120
FF_D = 5632
FF_E = 7168
NE = 8
EPS = 1e-6
N_CORES = 8
QOFF, KOFF, VOFF, POFF, BOFF, COFF, UOFF = 0, 1024, 2048, 3072, 3584, 4096, 4608


class Tok:
    __slots__ = ("sem", "sid", "val")

    def __init__(self, sem, sid, val):
        self.sem, self.sid, self.val = sem, sid, val


class Buf:
    def __init__(self, k, name, merge=False):
        self.k, self.name, self.merge = k, name, merge
        self.w = {}
        self.r = {}
        self.dsem = None
        self.dcnt = 0

    def sem(self):
        if self.dsem is None:
            if self.k.free_dsems:
                self.dsem, self.dcnt = self.k.free_dsems.pop()
            else:
                self.dsem = self.k.new_sem("d_" + self.name)
        return self.dsem


class Eng:
    def __init__(self, k, name, eng):
        self.k, self.name, self.eng = k, name, eng
        self.sem = k.new_sem("e_" + name)
        self.cnt = 0
        self.waited = {}

    def wait(self, tok):
        if tok is None:
            return
        if self.waited.get(tok.sid, 0) < tok.val:
            self.eng.wait_ge(tok.sem, tok.val)
            self.waited[tok.sid] = tok.val

    def mark(self, ins):
        self.cnt += 1
        ins.then_inc(self.sem[0], 1)
        return Tok(self.sem[0], self.sem[1], self.cnt)


class K:
    def __init__(self, debug=None):
        self.debug = debug
        self.nc = bass.Bass("TRN2", target_bir_lowering=False)
        self.es = ExitStack()
        self.nsem = 0
        self.dma_toks = {}
        self.free_dsems = []
        self.live_bufs = []
        nc = self.nc
        self.PE = Eng(self, "pe", nc.tensor)
        self.ACT = Eng(self, "act", nc.scalar)
        self.DVE = Eng(self, "dve", nc.vector)
        self.POOL = Eng(self, "pool", nc.gpsimd)
        self.SP = Eng(self, "sp", nc.sync)
        self.engs = [self.PE, self.ACT, self.DVE, self.POOL, self.SP]

    def new_sem(self, name):
        self.nsem += 1
        h = self.es.enter_context(self.nc.semaphore(f"{name}_{self.nsem}"))
        return (h, self.nsem)

    def buf(self, name, merge=False, persist=False):
        b = Buf(self, name, merge)
        if not persist:
            self.live_bufs.append(b)
        return b

    def end_phase(self):
        self.barrier()
        for b in self.live_bufs:
            if b.dsem is not None:
                self.free_dsems.append((b.dsem, b.dcnt))
                b.dsem = None
        self.live_bufs = []

    def _deps(self, E, reads, writes):
        for b in reads:
            for t in b.w.values():
                E.wait(t)
        for b in writes:
            for t in b.w.values():
                E.wait(t)
            for t in b.r.values():
                E.wait(t)

    def _record(self, tok, reads, writes):
        for b in reads:
            b.r[tok.sid] = tok
        for b in writes:
            if b.merge:
                b.w[tok.sid] = tok
            else:
                b.w = {tok.sid: tok}
            b.r = {}

    def op(self, E, fn, reads=(), writes=()):
        self._deps(E, reads, writes)
        ins = fn()
        tok = E.mark(ins)
        self._record(tok, reads, writes)
        return tok

    def mm(self, out, pairs, reads, writes, transpose=False):
        E = self.PE
        self._deps(E, reads, writes)
        n = len(pairs)
        ins = None
        for i, (l, r) in enumerate(pairs):
            ins = self.nc.tensor.matmul(out, l, r, start=(i == 0), stop=(i == n - 1))
        tok = E.mark(ins)
        self._record(tok, reads, writes)
        return tok

    def mm_multi(self, items, reads, writes):
        E = self.PE
        self._deps(E, reads, writes)
        ins = None
        for (o, l, r, st, sp) in items:
            ins = self.nc.tensor.matmul(o, l, r, start=st, stop=sp)
        tok = E.mark(ins)
        self._record(tok, reads, writes)
        return tok

    def transposes(self, items, ident, reads, writes):
        E = self.PE
        self._deps(E, reads, writes)
        ins = None
        for (o, i) in items:
            ins = self.nc.tensor.transpose(o, i, ident)
        tok = E.mark(ins)
        self._record(tok, reads, writes)
        return tok

    def dma(self, Q, out, in_, sbuf, reads=(), writes=()):
        self._deps(Q, reads, writes)
        ins = Q.eng.dma_start(out=out, in_=in_)
        sem = sbuf.sem()
        sbuf.dcnt += 16
        ins.then_inc(sem[0], 16)
        tok = Tok(sem[0], sem[1], sbuf.dcnt)
        self.dma_toks[sem[1]] = tok
        self._record(tok, reads, writes)
        return tok

    def barrier(self):
        toks = [Tok(e.sem[0], e.sem[1], e.cnt) for e in self.engs if e.cnt > 0]
        toks += list(self.dma_toks.values())
        for e in self.engs:
            for t in toks:
                e.wait(t)

    def act(self, out, in_, func, reads, writes, scale=1.0, bias=None, accum_out=None):
        kw = {}
        if bias is not None:
            kw["bias"] = bias
        if accum_out is not None:
            kw["accum_out"] = accum_out
        return self.op(self.ACT, lambda: self.nc.scalar.activation(out=out, in_=in_, func=func, scale=scale, **kw),
                       reads, writes)

    def tt(self, out, in0, in1, op, reads, writes):
        return self.op(self.DVE, lambda: self.nc.vector.tensor_tensor(out=out, in0=in0, in1=in1, op=op), reads, writes)

    def ts(self, out, in0, s1, s2, op0, op1, reads, writes):
        if s2 is None:
            return self.op(self.DVE, lambda: self.nc.vector.tensor_scalar(out=out, in0=in0, scalar1=s1, scalar2=None,
                                                                          op0=op0), reads, writes)
        return self.op(self.DVE, lambda: self.nc.vector.tensor_scalar(out=out, in0=in0, scalar1=s1, scalar2=s2,
                                                                      op0=op0, op1=op1), reads, writes)

    def stt(self, out, in0, scalar, in1, op0, op1, reads, writes):
        return self.op(self.DVE, lambda: self.nc.vector.scalar_tensor_tensor(out=out, in0=in0, scalar=scalar, in1=in1,
                                                                             op0=op0, op1=op1), reads, writes)

    def recip(self, out, in_, reads, writes):
        return self.op(self.DVE, lambda: self.nc.vector.reciprocal(out=out, in_=in_), reads, writes)

    def vcopy(self, out, in_, reads, writes):
        return self.op(self.DVE, lambda: self.nc.vector.tensor_copy(out=out, in_=in_), reads, writes)

    def acopy(self, out, in_, reads, writes):
        return self.op(self.ACT, lambda: self.nc.scalar.copy(out=out, in_=in_), reads, writes)

    def vmemset(self, ap, val, writes):
        return self.op(self.DVE, lambda: self.nc.vector.memset(ap, val), (), writes)

    def sb(self, st, name, shape, dt):
        self.nsb = getattr(self, "nsb", 0) + 1
        return st.enter_context(self.nc.sbuf_tensor(f"{name}_{self.nsb}", shape, dt))

    def declare(self, skip=()):
        nc = self.nc
        d = {}
        self.inputs_declared = []

        def inp(name, shape, dt=F32):
            if name in skip:
                return
            self.inputs_declared.append(name)
            d[name] = nc.dram_tensor(name, shape, dt, kind="ExternalInput").ap()

        inp("x", [S, D])
        inp("pos", [1, S], I32)
        inp("attn_norm_g", [DEPTH, D])
        inp("w_in", [DEPTH, D, IN_W])
        inp("q_norm_g", [DEPTH, HD])
        inp("k_norm_g", [DEPTH, HD])
        inp("lambda_vecs", [DEPTH, 4, HD])
        inp("attn_out_norm_g", [DEPTH, 2 * HD])
        inp("w_pool", [DEPTH, 4, 128, 128])
        inp("pool_scale", [DEPTH, 512])
        inp("conv_w", [DEPTH, 3, 512])
        inp("w_out", [DEPTH, D, D])
        inp("ffn_norm_g", [DEPTH, D])
        inp("dense_w_gate", [1, D, FF_D])
        inp("dense_w_up", [1, D, FF_D])
        inp("dense_w_down", [1, FF_D, D])
        inp("router_w", [1, D, NE])
        inp("moe_w_gate", [1, NE, D, FF_E])
        inp("moe_w_up", [1, NE, D, FF_E])
        inp("moe_w_down", [1, NE, FF_E, D])
        inp("c_ident", [128, 128])
        inp("c_perm", [128, 128])
        inp("c_tri", [128, 128])
        inp("c_invf", [128, 1])
        inp("c_invc", [128, 64])
        d["out"] = nc.dram_tensor("out", [S, D], F32, kind="ExternalOutput").ap()
        skind = "ExternalOutput" if self.debug else "Internal"
        d["qT"] = nc.dram_tensor("s_qT", [8, 128, S], BF16, kind=skind).ap()
        d["kT"] = nc.dram_tensor("s_kT", [8, 128, S], BF16, kind=skind).ap()
        d["V"] = nc.dram_tensor("s_V", [S, 1024], BF16, kind=skind).ap()
        d["mixT"] = nc.dram_tensor("s_mixT", [16, 128, S], BF16, kind=skind).ap()
        self.d = d

    def build(self, upto="all", skip=()):
        nc = self.nc
        self.declare(skip)
        d = self.d
        with self.es:
            gst = ExitStack()
            with gst:
                g = self.g = {}
                g["ident_b"] = self.sb(gst, "ident_b", [128, 128], BF16)
                g["ident_f"] = self.sb(gst, "ident_f", [128, 128], F32)
                g["perm_b"] = self.sb(gst, "perm_b", [128, 128], BF16)
                g["tri_b"] = self.sb(gst, "tri_b", [128, 128], BF16)
                g["ones_b"] = self.sb(gst, "ones_b", [128, 128], BF16)
                g["ones_f"] = self.sb(gst, "ones_f", [128, 128], F32)
                g["invf"] = self.sb(gst, "invf", [128, 1], F32)
                g["invc"] = self.sb(gst, "invc", [128, 64], F32)
                g["eps"] = self.sb(gst, "eps", [128, 1], F32)
                g["negpi"] = self.sb(gst, "negpi", [128, 1], F32)
                self.ps = [gst.enter_context(nc.psum_tensor(f"ps{i}", [128, 512], F32)) for i in range(8)]
                self.psb = [self.buf(f"ps{i}", persist=True) for i in range(8)]
                cb = self.cb = self.buf("consts", persist=True)
                self.dma(self.POOL, g["ident_b"][:], d["c_ident"][:, :], cb, writes=[cb])
                self.dma(self.SP, g["ident_f"][:], d["c_ident"][:, :], cb, writes=[cb])
                self.dma(self.POOL, g["perm_b"][:], d["c_perm"][:, :], cb, writes=[cb])
                self.dma(self.POOL, g["tri_b"][:], d["c_tri"][:, :], cb, writes=[cb])
                self.dma(self.SP, g["invf"][:], d["c_invf"][:, :], cb, writes=[cb])
                self.dma(self.SP, g["invc"][:], d["c_invc"][:, :], cb, writes=[cb])
                self.vmemset(g["ones_b"][:], 1.0, [cb])
                self.vmemset(g["ones_f"][:], 1.0, [cb])
                self.vmemset(g["eps"][:], EPS, [cb])
                self.vmemset(g["negpi"][:], -math.pi, [cb])
                self.barrier()
                self.scr = {n: self.buf("scr_" + n, merge=True, persist=True) for n in ("qT", "kT", "V", "mixT")}
                self.outb = [self.buf(f"out{t}", merge=True, persist=True) for t in range(NT)]
                for l in range(DEPTH):
                    src = d["x"] if l == 0 else d["out"]
                    self.phase_inproj(l, src)
                    self.end_phase()
                    if upto == f"inproj{l}":
                        break
                    self.phase_attn(l)
                    self.end_phase()
                    if upto == f"attn{l}":
                        break
                    self.phase_outproj(l, src)
                    self.end_phase()
                    if upto == f"outproj{l}":
                        break
                    self.phase_ffn(l)
                    self.end_phase()
                    if upto == f"ffn{l}":
                        break
                self.barrier()
        return nc

    def rotary_tables(self):
        nc, g, d, cb = self.nc, self.g, self.d, self.cb
        with ExitStack() as st:
            posi = self.sb(st, "posi", [128, S], I32)
            ang = self.sb(st, "ang", [128, S], F32)
            r = self.sb(st, "rr", [128, S], F32)
            kf = self.sb(st, "rkf", [128, S], F32)
            tb = self.buf("rot_tmp")
            self.dma(self.SP, posi[:], d["pos"][0:1, :].partition_broadcast(128), tb, writes=[tb])
            self.vcopy(ang[:], posi[:], [tb], [tb])
            self.ts(ang[:], ang[:], g["invf"][:, 0:1], None, ALU.mult, None, [tb, cb], [tb])
            two_pi = 2.0 * math.pi
            C1 = 6.28125
            C2 = two_pi - C1

            def reduce_(shift):
                if shift:
                    self.ts(r[:], ang[:], shift, None, ALU.add, None, [tb], [tb])
                else:
                    self.vcopy(r[:], ang[:], [tb], [tb])
                self.ts(kf[:], r[:], 1.0 / two_pi, None, ALU.mult, None, [tb], [tb])
                self.vcopy(posi[:], kf[:], [tb], [tb])
                self.vcopy(kf[:], posi[:], [tb], [tb])
                self.stt(r[:], kf[:], -C1, r[:], ALU.mult, ALU.add, [tb], [tb])
                self.stt(r[:], kf[:], -C2, r[:], ALU.mult, ALU.add, [tb], [tb])
                self.ts(kf[:], r[:], math.pi, -two_pi, ALU.is_gt, ALU.mult, [tb], [tb])
                self.tt(r[:], r[:], kf[:], ALU.add, [tb], [tb])
                self.ts(kf[:], r[:], -math.pi, two_pi, ALU.is_lt, ALU.mult, [tb], [tb])
                self.tt(r[:], r[:], kf[:], ALU.add, [tb], [tb])
                self.ts(r[:], r[:], 3.1415925, -3.1415925, ALU.min, ALU.max, [tb], [tb])

            reduce_(0.0)
            self.act(g["Sg"][:], r[:], AF.Sin, [tb, cb], [cb])
            self.ts(g["Sg"][0:64, :], g["Sg"][0:64, :], -1.0, None, ALU.mult, None, [cb], [cb])
            reduce_(0.5 * math.pi)
            self.act(g["C"][:], r[:], AF.Sin, [tb, cb], [cb])
            self.barrier()

    def load_col(self, dst_col, vec_ap, b):
        self.dma(self.SP, dst_col, vec_ap.rearrange("(p o) -> p o", o=1), b, writes=[b])

    def norm_tile(self, xt, xb, gB, gBb, hT, hTb, col0, tmp, idx, ident, route=None):
        g = self.g
        ssq, sb_ = tmp["ssq"][idx % 2]
        xs, xsb = tmp["xs"][idx % 2]
        junk, jb = xs, xsb
        self.act(junk[:], xt, AF.Square, [xb], [jb, sb_], accum_out=ssq[:, 0:1])
        self.act(ssq[:, 1:2], ssq[:, 0:1], AF.Sqrt, [sb_, self.cb], [sb_], scale=1.0 / D, bias=g["eps"][:, 0:1])
        self.recip(ssq[:, 2:3], ssq[:, 1:2], [sb_], [sb_])
        self.stt(xs[:], xt, ssq[:, 2:3], gB[:], ALU.mult, ALU.mult, [xb, sb_, gBb], [xsb])
        for g4 in range(4):
            pi = (idx * 4 + g4) % 4
            pst, psb = self.ps[pi], self.psb[pi]
            if route is None:
                pv = pst[:].bitcast(BF16)
                items = [(pv[:, j * 128:(j + 1) * 128], xs[:, (g4 * 4 + j) * 128:(g4 * 4 + j + 1) * 128])
                         for j in range(4)]
                self.transposes(items, ident, [xsb, self.cb], [psb])
                src = pv[:, 0:512].rearrange("p (c t) -> p c t", c=4)
            else:
                items = [(pst[:, j * 128:(j + 1) * 128], xs[:, (g4 * 4 + j) * 128:(g4 * 4 + j + 1) * 128])
                         for j in range(4)]
                self.transposes(items, ident, [xsb, self.cb], [psb])
                src = pst[:, 0:512].rearrange("p (c t) -> p c t", c=4)
            dst = hT[:, g4 * 4:(g4 + 1) * 4, col0:col0 + 128]
            if route is None:
                if g4 % 2 == 0:
                    self.vcopy(dst, src, [psb], [hTb])
                else:
                    self.acopy(dst, src, [psb], [hTb])
            else:
                h32, h32b = route["h32"][g4 % 2]
                self.vcopy(h32[:], src, [psb], [h32b])
                self.acopy(dst, h32[:], [h32b], [hTb])
                lp, lpb = self.ps[4], self.psb[4]
                items = [(lp[:, 0:NE], h32[:, j, :], route["rw"][:, g4 * 4 + j, :], (g4 == 0 and j == 0),
                          (g4 == 3 and j == 3)) for j in range(4)]
                if route.get("level", 3) >= 2:
                    self.mm_multi(items, [h32b, route["rwb"]], [lpb])
        if route is not None and route.get("level", 3) >= 3:
            self.route_tile(route, idx)

    def route_tile(self, route, idx):
        lp, lpb = self.ps[4], self.psb[4]
        lg, lgb = route["lg"][idx % 2]
        comb, combb = route["comb"], route["combb"]
        self.vcopy(lg[:, 0:8], lp[:, 0:NE], [lpb], [lgb])
        self.op(self.DVE, lambda: self.nc.vector.max(out=lg[:, 8:16], in_=lg[:, 0:8]), [lgb], [lgb])
        self.tt(lg[:, 16:17], lg[:, 8:9], lg[:, 9:10], ALU.subtract, [lgb], [lgb])
        self.act(lg[:, 17:18], lg[:, 16:17], AF.Sigmoid, [lgb], [lgb])
        self.ts(lg[:, 18:19], lg[:, 17:18], -1.0, 1.0, ALU.mult, ALU.add, [lgb], [lgb])
        self.ts(lg[:, 24:32], lg[:, 0:8], lg[:, 8:9], lg[:, 17:18], ALU.is_equal, ALU.mult, [lgb], [lgb])
        self.ts(lg[:, 32:40], lg[:, 0:8], lg[:, 9:10], lg[:, 18:19], ALU.is_equal, ALU.mult, [lgb], [lgb])
        self.tt(comb[:, idx, :], lg[:, 24:32], lg[:, 32:40], ALU.add, [lgb], [combb])

    def phase_inproj(self, l, src):
        nc, g, d = self.nc, self.g, self.d
        with ExitStack() as st:
            hT = self.sb(st, "hT", [128, 16, S], BF16)
            hTb = self.buf("hT")
            g["C"] = self.sb(st, "rotC", [128, S], F32)
            g["Sg"] = self.sb(st, "rotS", [128, S], F32)
            self.rotary_tables()
            with ExitStack() as st2:
                gB = self.sb(st2, "gB", [128, D], F32)
                gBb = self.buf("gB")
                self.dma(self.SP, gB[:], d["attn_norm_g"][l:l + 1, :].partition_broadcast(128), gBb, writes=[gBb])
                xts = [(self.sb(st2, f"xt{i}", [128, D], F32), self.buf(f"xt{i}")) for i in range(3)]
                tmp = {
                    "ssq": [(self.sb(st2, f"ssq{i}", [128, 4], F32), self.buf(f"ssq{i}")) for i in range(2)],
                    "xs": [(self.sb(st2, f"xs{i}", [128, D], BF16), self.buf(f"xs{i}")) for i in range(2)],
                }
                for tt in range(NT):
                    xt, xb = xts[tt % 3]
                    rd = [self.outb[tt]] if l > 0 else []
                    self.dma(self.SP, xt[:], src[tt * 128:(tt + 1) * 128, :], xb, reads=rd, writes=[xb])
                    self.norm_tile(xt[:], xb, gB, gBb, hT, hTb, tt * 128, tmp, tt, g["ident_b"][:])
                self.barrier()
            self.inproj_body(l, hT, hTb, st)

    def inproj_body(self, l, hT, hTb, st):
        nc, g, d = self.nc, self.g, self.d
        cb = self.cb
        W = d["w_in"]
        wsl = [(self.sb(st, f"wi{i}", [128, 16, 512], BF16), self.buf(f"wi{i}")) for i in range(2)]
        F = [(self.sb(st, f"F{i}", [128, 16 + S], F32), self.buf(f"F{i}")) for i in range(4)]
        sm = [(self.sb(st, f"sm{i}", [128, 512], F32), self.buf(f"sm{i}")) for i in range(6)]
        smb = [(self.sb(st, f"smb{i}", [128, 512], BF16), self.buf(f"smb{i}")) for i in range(4)]
        stg = [(self.sb(st, f"stg{i}", [128, S], BF16), self.buf(f"stg{i}")) for i in range(2)]
        vst = [(self.sb(st, f"vst{i}", [128, 512], BF16), self.buf(f"vst{i}")) for i in range(3)]
        pv = self.sb(st, "pvec", [128, 16], F32)
        pvb = self.buf("pvec")
        wp = self.sb(st, "wpool", [128, 4, 128], BF16)
        wpb = self.buf("wpool")
        self.load_col(pv[:, 0:1], d["q_norm_g"][l, :], pvb)
        self.load_col(pv[:, 1:2], d["k_norm_g"][l, :], pvb)
        for c in range(4):
            self.load_col(pv[:, 2 + c:3 + c], d["pool_scale"][l, c * 128:(c + 1) * 128], pvb)
        self.cw = self.sb(st, "convw", [128, 12], F32)
        for j in range(3):
            for c in range(4):
                self.load_col(self.cw[:, j * 4 + c:j * 4 + c + 1], d["conv_w"][l, j, c * 128:(c + 1) * 128], pvb)
        self.dma(self.POOL, wp[:], d["w_pool"][l].rearrange("g c d -> c g d"), wpb, writes=[wpb])
        for i in range(4):
            self.vmemset(F[i][0][:, 0:16], 0.0, [F[i][1]])

        units = []
        for c in range(8):
            units.append(("q", c, [(QOFF + c * 128, 128)]))
        for c in range(8):
            units.append(("k", c, [(KOFF + c * 128, 128)]))
        for vb in range(2):
            units.append(("v", vb, [(VOFF + vb * 512, 512)]))
        for c in range(4):
            units.append(("pool", c, [(POFF + c * 128, 128)]))
        for c in range(4):
            units.append(("conv", c, [(BOFF + c * 128, 128), (COFF + c * 128, 128), (UOFF + c * 128, 128)]))

        def load(ui):
            kind, idx, cols = units[ui]
            wt, wb = wsl[ui % 2]
            o = 0
            for (c0, n) in cols:
                self.dma(self.POOL, wt[:, :, o:o + n], W[l, :, c0:c0 + n].rearrange("(c p) n -> p c n", p=128), wb,
                         writes=[wb])
                o += n

        load(0)
        rr = [0]

        def nxt(lst):
            rr[0] += 1
            return lst[rr[0] % len(lst)]

        pcount = [0]

        def psn():
            pcount[0] += 1
            i = pcount[0] % 8
            return self.ps[i], self.psb[i]

        for ui, (kind, idx, cols) in enumerate(units):
            if ui + 1 < len(units):
                load(ui + 1)
            wt, wb = wsl[ui % 2]
            if kind in ("q", "k"):
                gcol = pv[:, 0:1] if kind == "q" else pv[:, 1:2]
                sg, sgb = nxt(stg)
                def emit_main(tb_):
                    pz_, pzb_ = psn()
                    self.mm(pz_[:], [(wt[:, kc, 0:128], hT[:, kc, tb_ * 512:(tb_ + 1) * 512]) for kc in range(16)],
                            [wb, hTb], [pzb_])
                    return pz_, pzb_

                nxt_main = emit_main(0)
                for tb in range(4):
                    tsl = slice(tb * 512, (tb + 1) * 512)
                    pz, pzb = nxt_main
                    if tb + 1 < 4:
                        nxt_main = emit_main(tb + 1)
                    qg, qgb = nxt(smb)
                    sq, sqb = nxt(smb)
                    self.act(qg[:], pz[:], AF.Copy, [pzb, pvb], [qgb], scale=gcol)
                    self.act(sq[:], pz[:], AF.Square, [pzb], [sqb])
                    pss, pssb = psn()
                    self.mm(pss[:], [(g["ones_b"][:], sq[:])], [sqb, cb], [pssb])
                    ppq, ppqb = psn()
                    self.mm(ppq[:], [(g["perm_b"][:], qg[:])], [qgb, cb], [ppqb])
                    rs, rsb = nxt(sm)
                    self.act(rs[:], pss[:], AF.Sqrt, [pssb, cb], [rsb], scale=1.0 / HD, bias=g["eps"][:, 0:1])
                    self.recip(rs[:], rs[:], [rsb], [rsb])
                    t1, t1b = nxt(sm)
                    t2, t2b = nxt(sm)
                    self.tt(t1[:], qg[:], g["C"][:, tsl], ALU.mult, [qgb, cb], [t1b])
                    self.tt(t2[:], ppq[:], g["Sg"][:, tsl], ALU.mult, [ppqb, cb], [t2b])
                    self.tt(t1[:], t1[:], t2[:], ALU.add, [t1b, t2b], [t1b])
                    self.tt(sg[:, tsl], t1[:], rs[:], ALU.mult, [t1b, rsb], [sgb])
                dst = d["qT"] if kind == "q" else d["kT"]
                self.dma(self.SP, dst[idx, :, :], sg[:], sgb, reads=[sgb], writes=[self.scr["qT" if kind == "q" else "kT"]])
            elif kind == "v":
                for tt in range(NT):
                    pz, pzb = psn()
                    self.mm(pz[:], [(hT[:, kc, tt * 128:(tt + 1) * 128], wt[:, kc, 0:512]) for kc in range(16)],
                            [wb, hTb], [pzb])
                    vs, vsb = nxt(vst)
                    if tt % 2 == 0:
                        self.vcopy(vs[:], pz[:], [pzb], [vsb])
                    else:
                        self.acopy(vs[:], pz[:], [pzb], [vsb])
                    self.dma(self.SP, d["V"][tt * 128:(tt + 1) * 128, idx * 512:(idx + 1) * 512], vs[:], vsb,
                             reads=[vsb], writes=[self.scr["V"]])
            elif kind == "pool":
                G, Gb = F[0]
                for tb in range(4):
                    pz, pzb = psn()
                    self.mm(pz[:], [(wt[:, kc, 0:128], hT[:, kc, tb * 512:(tb + 1) * 512]) for kc in range(16)],
                            [wb, hTb], [pzb])
                    self.acopy(G[:, 16 + tb * 512:16 + (tb + 1) * 512], pz[:], [pzb], [Gb])
                w = 2 << idx
                cur, curb = G, Gb
                sh = 1
                pp = 1
                while sh < w:
                    nx, nxb = F[pp]
                    self.tt(nx[:, 16:16 + S], cur[:, 16:16 + S], cur[:, 16 - sh:16 - sh + S], ALU.add, [curb], [nxb])
                    cur, curb = nx, nxb
                    pp = 3 - pp
                    sh *= 2
                po, pob = F[3]
                self.stt(po[:, 16:16 + S], cur[:, 16:16 + S], 1.0 / w, G[:, 16:16 + S], ALU.mult, ALU.subtract,
                         [curb, Gb], [pob])
                self.tt(po[:, 0:16], cur[:, 16:32], g["invc"][:, idx * 16:(idx + 1) * 16], ALU.mult, [curb, cb], [pob])
                self.tt(po[:, 16:32], po[:, 0:16], G[:, 16:32], ALU.subtract, [pob, Gb], [pob])
                pbf, pbfb = nxt(stg)
                self.vcopy(pbf[:], po[:, 16:16 + S], [pob], [pbfb])
                sg, sgb = nxt(stg)
                for tb in range(4):
                    pz, pzb = psn()
                    self.mm(pz[:], [(wp[:, idx, :], pbf[:, tb * 512:(tb + 1) * 512])], [wpb, pbfb], [pzb])
                    self.act(sg[:, tb * 512:(tb + 1) * 512], pz[:], AF.Copy, [pzb, pvb], [sgb],
                             scale=pv[:, 2 + idx:3 + idx])
                self.dma(self.SP, d["mixT"][8 + idx, :, :], sg[:], sgb, reads=[sgb], writes=[self.scr["mixT"]])
            else:
                U, Ub = F[0]
                Bt, Bb = F[1]
                Y, Yb = F[2]
                for tb in range(4):
                    tsl = slice(tb * 512, (tb + 1) * 512)
                    pB, pBb = psn()
                    self.mm(pB[:], [(wt[:, kc, 0:128], hT[:, kc, tsl]) for kc in range(16)], [wb, hTb], [pBb])
                    pC, pCb = psn()
                    self.mm(pC[:], [(wt[:, kc, 128:256], hT[:, kc, tsl]) for kc in range(16)], [wb, hTb], [pCb])
                    pH, pHb = psn()
                    self.mm(pH[:], [(wt[:, kc, 256:384], hT[:, kc, tsl]) for kc in range(16)], [wb, hTb], [pHb])
                    self.acopy(Bt[:, 16 + tb * 512:16 + (tb + 1) * 512], pB[:], [pBb], [Bb])
                    cc, ccb = nxt(sm)
                    self.acopy(cc[:], pC[:], [pCb], [ccb])
                    self.tt(U[:, 16 + tb * 512:16 + (tb + 1) * 512], pH[:], cc[:], ALU.mult, [pHb, ccb], [Ub])
                cw = self.cw
                self.ts(Y[:, 16:16 + S], U[:, 16:16 + S], cw[:, 8 + idx:9 + idx], None, ALU.mult, None, [Ub, pvb], [Yb])
                self.stt(Y[:, 16:16 + S], U[:, 15:15 + S], cw[:, 4 + idx:5 + idx], Y[:, 16:16 + S], ALU.mult, ALU.add,
                         [Ub, pvb, Yb], [Yb])
                self.stt(Y[:, 16:16 + S], U[:, 14:14 + S], cw[:, idx:idx + 1], Y[:, 16:16 + S], ALU.mult, ALU.add,
                         [Ub, pvb, Yb], [Yb])
                sg, sgb = nxt(stg)
                self.tt(sg[:], Y[:, 16:16 + S], Bt[:, 16:16 + S], ALU.mult, [Yb, Bb], [sgb])
                self.dma(self.SP, d["mixT"][12 + idx, :, :], sg[:], sgb, reads=[sgb], writes=[self.scr["mixT"]])

    def phase_attn(self, l):
        nc, g, d = self.nc, self.g, self.d
        cb = self.cb
        lam_init = 0.8 - 0.6 * math.exp(-0.3 * l)
        scale = HD ** -0.5
        with ExitStack() as st:
            lv = self.sb(st, "lv", [128, 16], F32)
            lvb = self.buf("lv")
            for j in range(4):
                self.load_col(lv[:, j:j + 1], d["lambda_vecs"][l, j, :], lvb)
            for c in range(2):
                self.load_col(lv[:, 4 + c:5 + c], d["attn_out_norm_g"][l, c * 128:(c + 1) * 128], lvb)
            self.tt(lv[:, 6:7], lv[:, 0:1], lv[:, 1:2], ALU.mult, [lvb], [lvb])
            self.tt(lv[:, 7:8], lv[:, 2:3], lv[:, 3:4], ALU.mult, [lvb], [lvb])
            p0, p0b = self.ps[0], self.psb[0]
            self.mm(p0[:, 0:2], [(g["ones_f"][:], lv[:, 6:8])], [lvb, cb], [p0b])
            self.act(lv[:, 8:10], p0[:, 0:2], AF.Exp, [p0b], [lvb])
            self.tt(lv[:, 10:11], lv[:, 8:9], lv[:, 9:10], ALU.subtract, [lvb], [lvb])
            self.ts(lv[:, 11:12], lv[:, 10:11], lam_init, -1.0, ALU.add, ALU.mult, [lvb], [lvb])
            self.ts(lv[:, 12:14], lv[:, 4:6], 1.0 - lam_init, None, ALU.mult, None, [lvb], [lvb])

            qh = [(self.sb(st, f"qh{i}", [128, 2, S], BF16), self.buf(f"qh{i}")) for i in range(2)]
            kh = [(self.sb(st, f"kh{i}", [128, 2, S], BF16), self.buf(f"kh{i}")) for i in range(2)]
            vh = [(self.sb(st, f"vh{i}", [128, 16, 256], BF16), self.buf(f"vh{i}")) for i in range(2)]
            pT = [(self.sb(st, f"pT{i}", [128, 512], BF16), self.buf(f"pT{i}")) for i in range(4)]
            rl = [(self.sb(st, f"rl{i}", [128, 512], F32), self.buf(f"rl{i}")) for i in range(2)]
            o1 = [(self.sb(st, f"o1{i}", [128, 512], F32), self.buf(f"o1{i}")) for i in range(2)]
            oo = [(self.sb(st, f"oo{i}", [128, 512], F32), self.buf(f"oo{i}")) for i in range(2)]
            sq = [(self.sb(st, f"sq{i}", [128, 512], BF16), self.buf(f"sq{i}")) for i in range(2)]
            rs, rsb = self.sb(st, "ars", [128, 512], F32), self.buf("ars")
            ast = [(self.sb(st, f"ast{i}", [128, S], BF16), self.buf(f"ast{i}")) for i in range(4)]

            def loadh(h):
                q, qb_ = qh[h % 2]
                k_, kb_ = kh[h % 2]
                v, vb_ = vh[h % 2]
                self.dma(self.SP, q[:], d["qT"][2 * h:2 * h + 2, :, :].rearrange("m p s -> p m s"), qb_,
                         reads=[self.scr["qT"]], writes=[qb_])
                self.dma(self.SP, k_[:], d["kT"][2 * h:2 * h + 2, :, :].rearrange("m p s -> p m s"), kb_,
                         reads=[self.scr["kT"]], writes=[kb_])
                self.dma(self.SP, v[:], d["V"][:, h * 256:(h + 1) * 256].rearrange("(t p) e -> p t e", p=128), vb_,
                         reads=[self.scr["V"]], writes=[vb_])

            loadh(0)
            pcnt = 0
            for h in range(NH):
                if h + 1 < NH:
                    loadh(h + 1)
                q, qb_ = qh[h % 2]
                k_, kb_ = kh[h % 2]
                v, vb_ = vh[h % 2]
                for qb in range(4):
                    q0 = qb * 512
                    nkt = 4 * qb + 4
                    steps = [(m, kt) for m in range(2) for kt in range(nkt)]

                    def emit_sc(si):
                        m, kt = steps[si]
                        c0 = max(kt - 4 * qb, 0) * 128
                        gi = pcnt0 + si
                        sc, scb = self.ps[gi % 2], self.psb[gi % 2]
                        self.mm(sc[:, c0:512], [(k_[:, m, kt * 128:(kt + 1) * 128], q[:, m, q0 + c0:q0 + 512])],
                                [kb_, qb_], [scb])

                    pcnt0 = pcnt
                    emit_sc(0)
                    for si, (m, kt) in enumerate(steps):
                        if si + 1 < len(steps):
                            emit_sc(si + 1)
                        O0, O0b = self.ps[2 + 3 * m], self.psb[2 + 3 * m]
                        O1, O1b = self.ps[3 + 3 * m], self.psb[3 + 3 * m]
                        L, Lb = self.ps[4 + 3 * m], self.psb[4 + 3 * m]
                        j = kt - 4 * qb
                        c0 = max(j, 0) * 128
                        gi = pcnt0 + si
                        sc, scb = self.ps[gi % 2], self.psb[gi % 2]
                        p, pb = pT[gi % 4]
                        self.act(p[:, c0:512], sc[:, c0:512], AF.Exp, [scb], [pb], scale=scale)
                        if j >= 0:
                            self.tt(p[:, c0:c0 + 128], p[:, c0:c0 + 128], g["tri_b"][:], ALU.mult, [pb, cb], [pb])
                        first, last = (kt == 0), (kt == nkt - 1)
                        items = [
                            (O0[:, c0:512], v[:, kt, 0:128], p[:, c0:512], first, last),
                            (O1[:, c0:512], v[:, kt, 128:256], p[:, c0:512], first, last),
                            (L[:, c0:512], g["ones_b"][:], p[:, c0:512], first, last),
                        ]
                        self.mm_multi(items, [vb_, pb, cb], [O0b, O1b, Lb])
                    pcnt = pcnt0 + len(steps)
                    for m in range(2):
                        L, Lb = self.ps[4 + 3 * m], self.psb[4 + 3 * m]
                        self.recip(rl[m][0][:], L[:], [Lb], [rl[m][1]])
                    for ec in range(2):
                        A, Ab = self.ps[2 + ec], self.psb[2 + ec]
                        Bm, Bmb = self.ps[5 + ec], self.psb[5 + ec]
                        self.tt(o1[ec][0][:], A[:], rl[0][0][:], ALU.mult, [Ab, rl[0][1]], [o1[ec][1]])
                        self.tt(oo[ec][0][:], Bm[:], rl[1][0][:], ALU.mult, [Bmb, rl[1][1]], [oo[ec][1]])
                        self.stt(oo[ec][0][:], oo[ec][0][:], lv[:, 11:12], o1[ec][0][:], ALU.mult, ALU.add,
                                 [oo[ec][1], o1[ec][1], lvb], [oo[ec][1]])
                        self.act(sq[ec][0][:], oo[ec][0][:], AF.Square, [oo[ec][1]], [sq[ec][1]])
                    ssn, ssnb = self.ps[pcnt % 2], self.psb[pcnt % 2]
                    pcnt += 1
                    self.mm(ssn[:], [(g["ones_b"][:], sq[0][0][:]), (g["ones_b"][:], sq[1][0][:])],
                            [sq[0][1], sq[1][1], cb], [ssnb])
                    self.act(rs[:], ssn[:], AF.Sqrt, [ssnb, cb], [rsb], scale=1.0 / (2 * HD), bias=g["eps"][:, 0:1])
                    self.recip(rs[:], rs[:], [rsb], [rsb])
                    for ec in range(2):
                        a, ab = ast[(h % 2) * 2 + ec]
                        self.stt(a[:, q0:q0 + 512], oo[ec][0][:], lv[:, 12 + ec:13 + ec], rs[:], ALU.mult, ALU.mult,
                                 [oo[ec][1], rsb, lvb], [ab])
                for ec in range(2):
                    a, ab = ast[(h % 2) * 2 + ec]
                    self.dma(self.SP, d["mixT"][2 * h + ec, :, :], a[:], ab, reads=[ab], writes=[self.scr["mixT"]])

    def phase_outproj(self, l, src):
        nc, g, d = self.nc, self.g, self.d
        with ExitStack() as st:
            mix = self.sb(st, "mix", [128, 16, S], BF16)
            mixb = self.buf("mix")
            for c4 in range(4):
                self.dma(self.SP, mix[:, c4 * 4:(c4 + 1) * 4, :], d["mixT"][c4 * 4:(c4 + 1) * 4, :, :].rearrange("c p s -> p c s"),
                         mixb, reads=[self.scr["mixT"]], writes=[mixb])
            wsl = [(self.sb(st, f"wo{i}", [128, 16, 512], BF16), self.buf(f"wo{i}")) for i in range(2)]
            xts = [(self.sb(st, f"xo{i}", [128, 512], F32), self.buf(f"xo{i}")) for i in range(4)]

            def loadw(db):
                wt, wb = wsl[db % 2]
                self.dma(self.POOL, wt[:], d["w_out"][l, :, db * 512:(db + 1) * 512].rearrange("(c p) n -> p c n", p=128),
                         wb, writes=[wb])

            units = [(db, tt) for db in range(4) for tt in range(NT)]

            def loadx(ui):
                db, tt = units[ui]
                xt, xb = xts[ui % 4]
                rd = [self.outb[tt]] if l > 0 else []
                self.dma(self.SP, xt[:], src[tt * 128:(tt + 1) * 128, db * 512:(db + 1) * 512], xb, reads=rd, writes=[xb])

            loadw(0)
            loadx(0)
            loadx(1)
            for ui, (db, tt) in enumerate(units):
                if tt == 0 and db + 1 < 4:
                    loadw(db + 1)
                if ui + 2 < len(units):
                    loadx(ui + 2)
                wt, wb = wsl[db % 2]
                xt, xb = xts[ui % 4]
                pz, pzb = self.ps[ui % 4], self.psb[ui % 4]
                self.mm(pz[:], [(mix[:, kc, tt * 128:(tt + 1) * 128], wt[:, kc, :]) for kc in range(16)], [mixb, wb], [pzb])
                self.tt(xt[:], xt[:], pz[:], ALU.add, [xb, pzb], [xb])
                self.dma(self.SP, d["out"][tt * 128:(tt + 1) * 128, db * 512:(db + 1) * 512], xt[:], xb, reads=[xb],
                         writes=[self.outb[tt]])

    def phase_ffn(self, l):
        nc, g, d = self.nc, self.g, self.d
        cb = self.cb
        moe = (l % 2 == 1)
        j = l // 2
        ne = NE if moe else 1
        ff = FF_E if moe else FF_D
        nfb = ff // 256
        if self.debug and isinstance(self.debug, dict):
            ne = min(ne, self.debug.get("ne", ne))
            nfb = min(nfb, self.debug.get("nfb", nfb))
        with ExitStack() as st:
            gB = self.sb(st, "gBf", [128, D], F32)
            gBb = self.buf("gBf")
            self.dma(self.SP, gB[:], d["ffn_norm_g"][l:l + 1, :].partition_broadcast(128), gBb, writes=[gBb])
            acc = [(self.sb(st, f"acc{i}", [128, D], F32), self.buf(f"acc{i}")) for i in range(8)]
            hT = self.sb(st, "h2T", [128, 16, 1024], BF16)
            hTb = self.buf("h2T")
            wg = [(self.sb(st, f"wg{i}", [128, 16, 256], BF16), self.buf(f"wg{i}")) for i in range(2)]
            wu = [(self.sb(st, f"wu{i}", [128, 16, 256], BF16), self.buf(f"wu{i}")) for i in range(2)]
            wd = [(self.sb(st, f"wd{i}", [128, 2, D], BF16), self.buf(f"wd{i}")) for i in range(2)]
            sg = [(self.sb(st, f"sg{i}", [128, 512], F32), self.buf(f"sg{i}")) for i in range(2)]
            at = [(self.sb(st, f"at{i}", [128, 512], BF16), self.buf(f"at{i}")) for i in range(4)]
            tmp = {
                "ssq": [(self.sb(st, f"ssqf{i}", [128, 4], F32), self.buf(f"ssqf{i}")) for i in range(2)],
            }
            route = None
            use_route = moe and not (isinstance(self.debug, dict) and self.debug.get("noroute"))
            if not use_route and moe:
                tmp["xs"] = [(self.sb(st, f"xsf{i}", [128, D], BF16), self.buf(f"xsf{i}")) for i in range(2)]
            if use_route:
                tmp["xs"] = [(self.sb(st, f"xsf{i}", [128, D], F32), self.buf(f"xsf{i}")) for i in range(1)] * 2
                route = {
                    "h32": [(self.sb(st, f"h32{i}", [128, 4, 128], F32), self.buf(f"h32{i}")) for i in range(2)],
                    "rw": self.sb(st, "rw", [128, 16, NE], F32),
                    "rwb": self.buf("rw"),
                    "lg": [(self.sb(st, f"lg{i}", [128, 40], F32), self.buf(f"lg{i}")) for i in range(2)],
                    "comb": self.sb(st, "comb", [128, 8, NE], F32),
                    "combb": self.buf("comb"),
                }
                for c in range(16):
                    self.dma(self.POOL, route["rw"][:, c, :], d["router_w"][j, c * 128:(c + 1) * 128, :], route["rwb"],
                             writes=[route["rwb"]])
                if isinstance(self.debug, dict):
                    route["level"] = self.debug.get("level", 3)
            else:
                tmp["xs"] = [(self.sb(st, f"xsf{i}", [128, D], BF16), self.buf(f"xsf{i}")) for i in range(2)]

            if moe:
                Wg = lambda e, fb: d["moe_w_gate"][j, e, :, fb * 256:(fb + 1) * 256]
                Wu = lambda e, fb: d["moe_w_up"][j, e, :, fb * 256:(fb + 1) * 256]
                Wd = lambda e, fb: d["moe_w_down"][j, e, fb * 256:(fb + 1) * 256, :]
            else:
                Wg = lambda e, fb: d["dense_w_gate"][j, :, fb * 256:(fb + 1) * 256]
                Wu = lambda e, fb: d["dense_w_up"][j, :, fb * 256:(fb + 1) * 256]
                Wd = lambda e, fb: d["dense_w_down"][j, fb * 256:(fb + 1) * 256, :]

            for half in range(2):
                units = [(e, fb) for e in range(ne) for fb in range(nfb)]

                def load_gu(ui):
                    if ui >= len(units):
                        return
                    e, fb = units[ui]
                    s_ = ui % 2
                    self.dma(self.POOL, wg[s_][0][:], Wg(e, fb).rearrange("(c p) n -> p c n", p=128), wg[s_][1],
                             writes=[wg[s_][1]])
                    self.dma(self.POOL, wu[s_][0][:], Wu(e, fb).rearrange("(c p) n -> p c n", p=128), wu[s_][1],
                             writes=[wu[s_][1]])

                def load_d(ui):
                    if ui >= len(units):
                        return
                    e, fb = units[ui]
                    s_ = ui % 2
                    self.dma(self.POOL, wd[s_][0][:], Wd(e, fb).rearrange("(c p) n -> p c n", p=128), wd[s_][1],
                             writes=[wd[s_][1]])

                load_gu(0)
                load_d(0)
                load_gu(1)
                load_d(1)
                for i in range(8):
                    tt = half * 8 + i
                    self.dma(self.SP, acc[i][0][:], d["out"][tt * 128:(tt + 1) * 128, :], acc[i][1],
                             reads=[self.outb[tt]], writes=[acc[i][1]])
                for i in range(8):
                    self.norm_tile(acc[i][0][:], acc[i][1], gB, gBb, hT, hTb, i * 128, tmp, i,
                                   g["ident_f"][:] if use_route else g["ident_b"][:], route=route)
                steps = [(ui, tb) for ui in range(len(units)) for tb in range(2)]
                acts_of = {}

                def gu_part(si, q):
                    ui, tb = steps[si]
                    s_ = ui % 2
                    c = q // 2
                    tsl = slice(tb * 512, (tb + 1) * 512)
                    sgt, sgb_ = sg[c]
                    if q % 2 == 0:
                        wgt, wgb = wg[s_]
                        pg, pgb = self.ps[c], self.psb[c]
                        self.mm(pg[:], [(wgt[:, kc, c * 128:(c + 1) * 128], hT[:, kc, tsl]) for kc in range(16)],
                                [wgb, hTb], [pgb])
                        self.act(sgt[:], pg[:], AF.Silu, [pgb], [sgb_])
                    else:
                        wut, wub = wu[s_]
                        pu, pub = self.ps[2 + c], self.psb[2 + c]
                        self.mm(pu[:], [(wut[:, kc, c * 128:(c + 1) * 128], hT[:, kc, tsl]) for kc in range(16)],
                                [wub, hTb], [pub])
                        a, ab = at[(tb * 2 + c) % 4]
                        self.tt(a[:], pu[:], sgt[:], ALU.mult, [pub, sgb_], [ab])
                        acts_of.setdefault(si, []).append((a, ab))
                        if q == 3 and tb == 1:
                            load_gu(ui + 2)

                pcs = [0]

                def down_group(si, gidx):
                    ui, tb = steps[si]
                    e, fb = units[ui]
                    wdt, wdb = wd[ui % 2]
                    acts = acts_of[si]
                    t4, dh = gidx // 2, gidx % 2
                    ti = tb * 4 + t4
                    pi = 4 + 2 * (pcs[0] % 2)
                    pcs[0] += 1
                    items = []
                    for db in range(2):
                        for c in range(2):
                            items.append((self.ps[pi + db][:], acts[c][0][:, t4 * 128:(t4 + 1) * 128],
                                          wdt[:, c, dh * 1024 + db * 512:dh * 1024 + (db + 1) * 512],
                                          c == 0, c == 1))
                    self.mm_multi(items, [acts[0][1], acts[1][1], wdb], [self.psb[pi], self.psb[pi + 1]])
                    for db in range(2):
                        dsl = slice(dh * 1024 + db * 512, dh * 1024 + (db + 1) * 512)
                        a_t, a_b = acc[ti]
                        if use_route and route.get("level", 3) >= 3:
                            self.stt(a_t[:, dsl], self.ps[pi + db][:], route["comb"][:, ti, e:e + 1],
                                     a_t[:, dsl], ALU.mult, ALU.add,
                                     [self.psb[pi + db], route["combb"], a_b], [a_b])
                        else:
                            self.tt(a_t[:, dsl], self.ps[pi + db][:], a_t[:, dsl], ALU.add,
                                    [self.psb[pi + db], a_b], [a_b])

                for q in range(4):
                    gu_part(0, q)
                for si, (ui, tb) in enumerate(steps):
                    for q in range(4):
                        if si + 1 < len(steps):
                            gu_part(si + 1, q)
                        down_group(si, 2 * q)
                        down_group(si, 2 * q + 1)
                    acts_of.pop(si)
                    if tb == 1:
                        load_d(ui + 2)
                for i in range(8):
                    tt = half * 8 + i
                    self.dma(self.SP, d["out"][tt * 128:(tt + 1) * 128, :], acc[i][0][:], acc[i][1], reads=[acc[i][1]],
                             writes=[self.outb[tt]])
                self.barrier()


def _consts():
    ident = np.eye(128, dtype=np.float32)
    perm = np.zeros((128, 128), np.float32)
    for p in range(128):
        perm[p, (p + 64) % 128] = 1.0
    tri = (np.arange(128)[:, None] <= np.arange(128)[None, :]).astype(np.float32)
    jj = np.arange(0, HD, 2, dtype=np.float32) / np.float32(HD)
    invf64 = (1.0 / (np.float32(10000.0) ** jj)).astype(np.float32)
    invf = np.concatenate([invf64, invf64]).reshape(128, 1).astype(np.float32)
    invc = np.zeros((128, 64), np.float32)
    for gi, w in enumerate((2, 4, 8, 16)):
        invc[:, gi * 16:(gi + 1) * 16] = 1.0 / np.minimum(np.arange(16) + 1, w).astype(np.float32)
    return {"c_ident": ident, "c_perm": perm, "c_tri": tri, "c_invf": invf, "c_invc": invc}


_WNAMES = ["attn_norm_g", "w_in", "q_norm_g", "k_norm_g", "lambda_vecs", "attn_out_norm_g", "w_pool", "pool_scale",
           "conv_w", "w_out", "ffn_norm_g", "dense_w_gate", "dense_w_up", "dense_w_down", "router_w", "moe_w_gate",
           "moe_w_up", "moe_w_down"]


def make_in_maps(inputs, n_cores=N_CORES):
    consts = _consts()
    shared = {n: np.ascontiguousarray(np.asarray(inputs[n], dtype=np.float32)) for n in _WNAMES}
    shared.update(consts)
    x = np.asarray(inputs["x"], dtype=np.float32)
    pos = np.asarray(inputs["positions"], dtype=np.int32)
    maps = []
    for c in range(n_cores):
        m = dict(shared)
        m["x"] = np.ascontiguousarray(x[c])
        m["pos"] = np.ascontiguousarray(pos[c].reshape(1, S))
        maps.append(m)
    return maps


def kernel(**inputs):
    k = K()
    nc = k.build()
    in_maps = make_in_maps(inputs)
    res = run_bass_kernel_spmd(nc, in_maps, core_ids=list(range(N_CORES)))
    return np.stack([np.asarray(r["out"], dtype=np.float32) for r in res.results], axis=0)
```

```python
import math
from contextlib import ExitStack

import numpy as np
import concourse.bass as bass
import concourse.mybir as mybir
from concourse.bass_utils import run_bass_kernel_spmd

F32 = mybir.dt.float32
BF16 = mybir.dt.bfloat16
I32 = mybir.dt.int32
AF = mybir.ActivationFunctionType
ALU = mybir.AluOpType
AX = mybir.AxisListType

S = 2048
D = 2048
NT = S // 128
DEPTH = 2
HD = 128
NH = 4
IN_W = 5120
FF_D = 5632
FF_E = 7168
NE = 8
EPS = 1e-6
N_CORES = 8
ATTACH_WAITS = True
QOFF, KOFF, VOFF, POFF, BOFF, COFF, UOFF = 0, 1024, 2048, 3072, 3584, 4096, 4608


class Tok:
    __slots__ = ("sem", "sid", "val")

    def __init__(self, sem, sid, val):
        self.sem, self.sid, self.val = sem, sid, val


class Buf:
    def __init__(self, k, name, merge=False):
        self.k, self.name, self.merge = k, name, merge
        self.w = {}
        self.r = {}
        self.dsem = None
        self.dcnt = 0

    def sem(self):
        if self.dsem is None:
            if self.k.free_dsems:
                self.dsem, self.dcnt = self.k.free_dsems.pop()
            else:
                self.dsem = self.k.new_sem("d_" + self.name)
        return self.dsem


class Eng:
    def __init__(self, k, name, eng):
        self.k, self.name, self.eng = k, name, eng
        self.sem = k.new_sem("e_" + name)
        self.cnt = 0
        self.waited = {}

    def wait(self, tok):
        if tok is None:
            return
        if self.waited.get(tok.sid, 0) < tok.val:
            self.eng.wait_ge(tok.sem, tok.val)
            self.waited[tok.sid] = tok.val

    def mark(self, ins):
        self.cnt += 1
        ins.then_inc(self.sem[0], 1)
        return Tok(self.sem[0], self.sem[1], self.cnt)


class K:
    def __init__(self, debug=None):
        self.debug = debug
        self.nc = bass.Bass("TRN2", target_bir_lowering=False)
        self.es = ExitStack()
        self.nsem = 0
        self.dma_toks = {}
        self.free_dsems = []
        self.live_bufs = []
        nc = self.nc
        self.PE = Eng(self, "pe", nc.tensor)
        self.ACT = Eng(self, "act", nc.scalar)
        self.DVE = Eng(self, "dve", nc.vector)
        self.POOL = Eng(self, "pool", nc.gpsimd)
        self.SP = Eng(self, "sp", nc.sync)
        self.engs = [self.PE, self.ACT, self.DVE, self.POOL, self.SP]

    def new_sem(self, name):
        self.nsem += 1
        h = self.es.enter_context(self.nc.semaphore(f"{name}_{self.nsem}"))
        return (h, self.nsem)

    def buf(self, name, merge=False, persist=False):
        b = Buf(self, name, merge)
        if not persist:
            self.live_bufs.append(b)
        return b

    def end_phase(self):
        self.barrier()
        for b in self.live_bufs:
            if b.dsem is not None:
                self.free_dsems.append((b.dsem, b.dcnt))
                b.dsem = None
        self.live_bufs = []

    def _deps(self, E, reads, writes):
        need = {}

        def consider(t):
            if E.waited.get(t.sid, 0) < t.val and (t.sid not in need or need[t.sid].val < t.val):
                need[t.sid] = t

        for b in reads:
            for t in b.w.values():
                consider(t)
        for b in writes:
            for t in b.w.values():
                consider(t)
            for t in b.r.values():
                consider(t)
        toks = list(need.values())
        for t in toks[:-1]:
            E.wait(t)
        return toks[-1] if toks else None

    def _attach(self, E, ins, pend):
        if pend is not None:
            if ATTACH_WAITS:
                ins._wait_ge(pend.sem, pend.val)
                E.waited[pend.sid] = pend.val
            else:
                raise AssertionError("pending wait must be emitted before the instruction")

    def _pre(self, E, reads, writes):
        pend = self._deps(E, reads, writes)
        if pend is not None and not ATTACH_WAITS:
            E.wait(pend)
            pend = None
        return pend

    def _record(self, tok, reads, writes):
        for b in reads:
            b.r[tok.sid] = tok
        for b in writes:
            if b.merge:
                b.w[tok.sid] = tok
            else:
                b.w = {tok.sid: tok}
            b.r = {}

    def op(self, E, fn, reads=(), writes=()):
        pend = self._pre(E, reads, writes)
        ins = fn()
        self._attach(E, ins, pend)
        tok = E.mark(ins)
        self._record(tok, reads, writes)
        return tok

    def mm(self, out, pairs, reads, writes, transpose=False):
        E = self.PE
        pend = self._pre(E, reads, writes)
        n = len(pairs)
        ins = None
        for i, (l, r) in enumerate(pairs):
            ins = self.nc.tensor.matmul(out, l, r, start=(i == 0), stop=(i == n - 1))
            if i == 0:
                self._attach(E, ins, pend)
        tok = E.mark(ins)
        self._record(tok, reads, writes)
        return tok

    def mm_multi(self, items, reads, writes):
        E = self.PE
        pend = self._pre(E, reads, writes)
        ins = None
        for i, (o, l, r, st, sp) in enumerate(items):
            ins = self.nc.tensor.matmul(o, l, r, start=st, stop=sp)
            if i == 0:
                self._attach(E, ins, pend)
        tok = E.mark(ins)
        self._record(tok, reads, writes)
        return tok

    def transposes(self, items, ident, reads, writes):
        E = self.PE
        pend = self._pre(E, reads, writes)
        ins = None
        for n_, (o, i) in enumerate(items):
            ins = self.nc.tensor.transpose(o, i, ident)
            if n_ == 0:
                self._attach(E, ins, pend)
        tok = E.mark(ins)
        self._record(tok, reads, writes)
        return tok

    def dma(self, Q, out, in_, sbuf, reads=(), writes=()):
        pend = self._deps(Q, reads, writes)
        if pend is not None:
            Q.wait(pend)
        ins = Q.eng.dma_start(out=out, in_=in_)
        sem = sbuf.sem()
        sbuf.dcnt += 16
        ins.then_inc(sem[0], 16)
        tok = Tok(sem[0], sem[1], sbuf.dcnt)
        self.dma_toks[sem[1]] = tok
        self._record(tok, reads, writes)
        return tok

    def barrier(self):
        toks = [Tok(e.sem[0], e.sem[1], e.cnt) for e in self.engs if e.cnt > 0]
        toks += list(self.dma_toks.values())
        for e in self.engs:
            for t in toks:
                e.wait(t)

    def act(self, out, in_, func, reads, writes, scale=1.0, bias=None, accum_out=None):
        kw = {}
        if bias is not None:
            kw["bias"] = bias
        if accum_out is not None:
            kw["accum_out"] = accum_out
        return self.op(self.ACT, lambda: self.nc.scalar.activation(out=out, in_=in_, func=func, scale=scale, **kw),
                       reads, writes)

    def tt(self, out, in0, in1, op, reads, writes):
        return self.op(self.DVE, lambda: self.nc.vector.tensor_tensor(out=out, in0=in0, in1=in1, op=op), reads, writes)

    def ts(self, out, in0, s1, s2, op0, op1, reads, writes):
        if s2 is None:
            return self.op(self.DVE, lambda: self.nc.vector.tensor_scalar(out=out, in0=in0, scalar1=s1, scalar2=None,
                                                                          op0=op0), reads, writes)
        return self.op(self.DVE, lambda: self.nc.vector.tensor_scalar(out=out, in0=in0, scalar1=s1, scalar2=s2,
                                                                      op0=op0, op1=op1), reads, writes)

    def stt(self, out, in0, scalar, in1, op0, op1, reads, writes):
        return self.op(self.DVE, lambda: self.nc.vector.scalar_tensor_tensor(out=out, in0=in0, scalar=scalar, in1=in1,
                                                                             op0=op0, op1=op1), reads, writes)

    def recip(self, out, in_, reads, writes):
        return self.op(self.DVE, lambda: self.nc.vector.reciprocal(out=out, in_=in_), reads, writes)

    def vcopy(self, out, in_, reads, writes):
        return self.op(self.DVE, lambda: self.nc.vector.tensor_copy(out=out, in_=in_), reads, writes)

    def acopy(self, out, in_, reads, writes):
        return self.op(self.ACT, lambda: self.nc.scalar.copy(out=out, in_=in_), reads, writes)

    def vmemset(self, ap, val, writes):
        return self.op(self.DVE, lambda: self.nc.vector.memset(ap, val), (), writes)

    def sb(self, st, name, shape, dt):
        self.nsb = getattr(self, "nsb", 0) + 1
        return st.enter_context(self.nc.sbuf_tensor(f"{name}_{self.nsb}", shape, dt))

    def declare(self, skip=()):
        nc = self.nc
        d = {}
        self.inputs_declared = []

        def inp(name, shape, dt=F32):
            if name in skip:
                return
            self.inputs_declared.append(name)
            d[name] = nc.dram_tensor(name, shape, dt, kind="ExternalInput").ap()

        inp("x", [S, D])
        inp("pos", [1, S], I32)
        inp("attn_norm_g", [DEPTH, D])
        inp("w_in", [DEPTH, D, IN_W])
        inp("q_norm_g", [DEPTH, HD])
        inp("k_norm_g", [DEPTH, HD])
        inp("lambda_vecs", [DEPTH, 4, HD])
        inp("attn_out_norm_g", [DEPTH, 2 * HD])
        inp("w_pool", [DEPTH, 4, 128, 128])
        inp("pool_scale", [DEPTH, 512])
        inp("conv_w", [DEPTH, 3, 512])
        inp("w_out", [DEPTH, D, D])
        inp("ffn_norm_g", [DEPTH, D])
        inp("dense_w_gate", [1, D, FF_D])
        inp("dense_w_up", [1, D, FF_D])
        inp("dense_w_down", [1, FF_D, D])
        inp("router_w", [1, D, NE])
        inp("moe_w_gate", [1, NE, D, FF_E])
        inp("moe_w_up", [1, NE, D, FF_E])
        inp("moe_w_down", [1, NE, FF_E, D])
        inp("c_ident", [128, 128])
        inp("c_perm", [128, 128])
        inp("c_tri", [128, 128])
        inp("c_invf", [128, 1])
        inp("c_invc", [128, 64])
        d["out"] = nc.dram_tensor("out", [S, D], F32, kind="ExternalOutput").ap()
        skind = "ExternalOutput" if self.debug else "Internal"
        d["qT"] = nc.dram_tensor("s_qT", [8, 128, S], BF16, kind=skind).ap()
        d["kT"] = nc.dram_tensor("s_kT", [8, 128, S], BF16, kind=skind).ap()
        d["V"] = nc.dram_tensor("s_V", [S, 1024], BF16, kind=skind).ap()
        d["mixT"] = nc.dram_tensor("s_mixT", [16, 128, S], BF16, kind=skind).ap()
        self.d = d

    def build(self, upto="all", skip=()):
        nc = self.nc
        self.declare(skip)
        d = self.d
        with self.es:
            gst = ExitStack()
            with gst:
                g = self.g = {}
                g["ident_b"] = self.sb(gst, "ident_b", [128, 128], BF16)
                g["ident_f"] = self.sb(gst, "ident_f", [128, 128], F32)
                g["perm_b"] = self.sb(gst, "perm_b", [128, 128], BF16)
                g["tri_b"] = self.sb(gst, "tri_b", [128, 128], BF16)
                g["ones_b"] = self.sb(gst, "ones_b", [128, 128], BF16)
                g["ones_f"] = self.sb(gst, "ones_f", [128, 128], F32)
                g["invf"] = self.sb(gst, "invf", [128, 1], F32)
                g["invc"] = self.sb(gst, "invc", [128, 64], F32)
                g["eps"] = self.sb(gst, "eps", [128, 1], F32)
                g["negpi"] = self.sb(gst, "negpi", [128, 1], F32)
                self.ps = [gst.enter_context(nc.psum_tensor(f"ps{i}", [128, 512], F32)) for i in range(8)]
                self.psb = [self.buf(f"ps{i}", persist=True) for i in range(8)]
                cb = self.cb = self.buf("consts", persist=True)
                self.dma(self.POOL, g["ident_b"][:], d["c_ident"][:, :], cb, writes=[cb])
                self.dma(self.SP, g["ident_f"][:], d["c_ident"][:, :], cb, writes=[cb])
                self.dma(self.POOL, g["perm_b"][:], d["c_perm"][:, :], cb, writes=[cb])
                self.dma(self.POOL, g["tri_b"][:], d["c_tri"][:, :], cb, writes=[cb])
                self.dma(self.SP, g["invf"][:], d["c_invf"][:, :], cb, writes=[cb])
                self.dma(self.SP, g["invc"][:], d["c_invc"][:, :], cb, writes=[cb])
                self.vmemset(g["ones_b"][:], 1.0, [cb])
                self.vmemset(g["ones_f"][:], 1.0, [cb])
                self.vmemset(g["eps"][:], EPS, [cb])
                self.vmemset(g["negpi"][:], -math.pi, [cb])
                self.barrier()
                self.scr = {n: self.buf("scr_" + n, merge=True, persist=True) for n in ("qT", "kT", "V", "mixT")}
                self.outb = [self.buf(f"out{t}", merge=True, persist=True) for t in range(NT)]
                for l in range(DEPTH):
                    src = d["x"] if l == 0 else d["out"]
                    self.phase_inproj(l, src)
                    self.end_phase()
                    if upto == f"inproj{l}":
                        break
                    self.phase_attn(l)
                    self.end_phase()
                    if upto == f"attn{l}":
                        break
                    self.phase_outproj(l, src)
                    self.end_phase()
                    if upto == f"outproj{l}":
                        break
                    self.phase_ffn(l)
                    self.end_phase()
                    if upto == f"ffn{l}":
                        break
                self.barrier()
        return nc

    def rotary_tables(self):
        nc, g, d, cb = self.nc, self.g, self.d, self.cb
        with ExitStack() as st:
            posi = self.sb(st, "posi", [128, S], I32)
            ang = self.sb(st, "ang", [128, S], F32)
            r = self.sb(st, "rr", [128, S], F32)
            kf = self.sb(st, "rkf", [128, S], F32)
            tb = self.buf("rot_tmp")
            self.dma(self.SP, posi[:], d["pos"][0:1, :].partition_broadcast(128), tb, writes=[tb])
            self.vcopy(ang[:], posi[:], [tb], [tb])
            self.ts(ang[:], ang[:], g["invf"][:, 0:1], None, ALU.mult, None, [tb, cb], [tb])
            two_pi = 2.0 * math.pi
            C1 = 6.28125
            C2 = two_pi - C1

            def reduce_(shift):
                if shift:
                    self.ts(r[:], ang[:], shift, None, ALU.add, None, [tb], [tb])
                else:
                    self.vcopy(r[:], ang[:], [tb], [tb])
                self.ts(kf[:], r[:], 1.0 / two_pi, None, ALU.mult, None, [tb], [tb])
                self.vcopy(posi[:], kf[:], [tb], [tb])
                self.vcopy(kf[:], posi[:], [tb], [tb])
                self.stt(r[:], kf[:], -C1, r[:], ALU.mult, ALU.add, [tb], [tb])
                self.stt(r[:], kf[:], -C2, r[:], ALU.mult, ALU.add, [tb], [tb])
                self.ts(kf[:], r[:], math.pi, -two_pi, ALU.is_gt, ALU.mult, [tb], [tb])
                self.tt(r[:], r[:], kf[:], ALU.add, [tb], [tb])
                self.ts(kf[:], r[:], -math.pi, two_pi, ALU.is_lt, ALU.mult, [tb], [tb])
                self.tt(r[:], r[:], kf[:], ALU.add, [tb], [tb])
                self.ts(r[:], r[:], 3.1415925, -3.1415925, ALU.min, ALU.max, [tb], [tb])

            reduce_(0.0)
            self.act(g["Sg"][:], r[:], AF.Sin, [tb, cb], [cb])
            self.ts(g["Sg"][0:64, :], g["Sg"][0:64, :], -1.0, None, ALU.mult, None, [cb], [cb])
            reduce_(0.5 * math.pi)
            self.act(g["C"][:], r[:], AF.Sin, [tb, cb], [cb])
            self.barrier()

    def load_col(self, dst_col, vec_ap, b):
        self.dma(self.SP, dst_col, vec_ap.rearrange("(p o) -> p o", o=1), b, writes=[b])

    def norm_tile(self, xt, xb, gB, gBb, hT, hTb, col0, tmp, idx, ident, route=None):
        g = self.g
        ssq, sb_ = tmp["ssq"][idx % 2]
        xs, xsb = tmp["xs"][idx % 2]
        junk, jb = xs, xsb
        self.act(junk[:], xt, AF.Square, [xb], [jb, sb_], accum_out=ssq[:, 0:1])
        self.act(ssq[:, 1:2], ssq[:, 0:1], AF.Sqrt, [sb_, self.cb], [sb_], scale=1.0 / D, bias=g["eps"][:, 0:1])
        self.recip(ssq[:, 2:3], ssq[:, 1:2], [sb_], [sb_])
        self.stt(xs[:], xt, ssq[:, 2:3], gB[:], ALU.mult, ALU.mult, [xb, sb_, gBb], [xsb])
        for g4 in range(4):
            pi = (idx * 4 + g4) % 4
            pst, psb = self.ps[pi], self.psb[pi]
            if route is None:
                pv = pst[:].bitcast(BF16)
                items = [(pv[:, j * 128:(j + 1) * 128], xs[:, (g4 * 4 + j) * 128:(g4 * 4 + j + 1) * 128])
                         for j in range(4)]
                self.transposes(items, ident, [xsb, self.cb], [psb])
                src = pv[:, 0:512].rearrange("p (c t) -> p c t", c=4)
            else:
                items = [(pst[:, j * 128:(j + 1) * 128], xs[:, (g4 * 4 + j) * 128:(g4 * 4 + j + 1) * 128])
                         for j in range(4)]
                self.transposes(items, ident, [xsb, self.cb], [psb])
                src = pst[:, 0:512].rearrange("p (c t) -> p c t", c=4)
            dst = hT[:, g4 * 4:(g4 + 1) * 4, col0:col0 + 128]
            if route is None:
                if g4 % 2 == 0:
                    self.vcopy(dst, src, [psb], [hTb])
                else:
                    self.acopy(dst, src, [psb], [hTb])
            else:
                h32, h32b = route["h32"][g4 % 2]
                self.vcopy(h32[:], src, [psb], [h32b])
                self.acopy(dst, h32[:], [h32b], [hTb])
                lp, lpb = self.ps[4], self.psb[4]
                items = [(lp[:, 0:NE], h32[:, j, :], route["rw"][:, g4 * 4 + j, :], (g4 == 0 and j == 0),
                          (g4 == 3 and j == 3)) for j in range(4)]
                if route.get("level", 3) >= 2:
                    self.mm_multi(items, [h32b, route["rwb"]], [lpb])
        if route is not None and route.get("level", 3) >= 3:
            self.route_tile(route, idx)

    def route_tile(self, route, idx):
        lp, lpb = self.ps[4], self.psb[4]
        lg, lgb = route["lg"][idx % 2]
        comb, combb = route["comb"], route["combb"]
        self.vcopy(lg[:, 0:8], lp[:, 0:NE], [lpb], [lgb])
        self.op(self.DVE, lambda: self.nc.vector.max(out=lg[:, 8:16], in_=lg[:, 0:8]), [lgb], [lgb])
        self.tt(lg[:, 16:17], lg[:, 8:9], lg[:, 9:10], ALU.subtract, [lgb], [lgb])
        self.act(lg[:, 17:18], lg[:, 16:17], AF.Sigmoid, [lgb], [lgb])
        self.ts(lg[:, 18:19], lg[:, 17:18], -1.0, 1.0, ALU.mult, ALU.add, [lgb], [lgb])
        self.ts(lg[:, 24:32], lg[:, 0:8], lg[:, 8:9], lg[:, 17:18], ALU.is_equal, ALU.mult, [lgb], [lgb])
        self.ts(lg[:, 32:40], lg[:, 0:8], lg[:, 9:10], lg[:, 18:19], ALU.is_equal, ALU.mult, [lgb], [lgb])
        self.tt(lg[:, 19:20], lg[:, 8:9], lg[:, 9:10], ALU.is_equal, [lgb], [lgb])
        self.ts(lg[:, 19:20], lg[:, 19:20], -0.5, 1.0, ALU.mult, ALU.add, [lgb], [lgb])
        self.tt(lg[:, 24:32], lg[:, 24:32], lg[:, 32:40], ALU.add, [lgb], [lgb])
        self.ts(comb[:, idx, :], lg[:, 24:32], lg[:, 19:20], None, ALU.mult, None, [lgb], [combb])

    def phase_inproj(self, l, src):
        nc, g, d = self.nc, self.g, self.d
        with ExitStack() as st:
            hT = self.sb(st, "hT", [128, 16, S], BF16)
            hTb = self.buf("hT")
            g["C"] = self.sb(st, "rotC", [128, S], F32)
            g["Sg"] = self.sb(st, "rotS", [128, S], F32)
            self.rotary_tables()
            with ExitStack() as st2:
                gB = self.sb(st2, "gB", [128, D], F32)
                gBb = self.buf("gB")
                self.dma(self.SP, gB[:], d["attn_norm_g"][l:l + 1, :].partition_broadcast(128), gBb, writes=[gBb])
                xts = [(self.sb(st2, f"xt{i}", [128, D], F32), self.buf(f"xt{i}")) for i in range(3)]
                tmp = {
                    "ssq": [(self.sb(st2, f"ssq{i}", [128, 4], F32), self.buf(f"ssq{i}")) for i in range(2)],
                    "xs": [(self.sb(st2, f"xs{i}", [128, D], BF16), self.buf(f"xs{i}")) for i in range(2)],
                }
                for tt in range(NT):
                    xt, xb = xts[tt % 3]
                    rd = [self.outb[tt]] if l > 0 else []
                    self.dma(self.SP, xt[:], src[tt * 128:(tt + 1) * 128, :], xb, reads=rd, writes=[xb])
                    self.norm_tile(xt[:], xb, gB, gBb, hT, hTb, tt * 128, tmp, tt, g["ident_b"][:])
                self.barrier()
            self.inproj_body(l, hT, hTb, st)

    def inproj_body(self, l, hT, hTb, st):
        nc, g, d = self.nc, self.g, self.d
        cb = self.cb
        W = d["w_in"]
        wsl = [(self.sb(st, f"wi{i}", [128, 16, 512], BF16), self.buf(f"wi{i}")) for i in range(2)]
        F = [(self.sb(st, f"F{i}", [128, 16 + S], F32), self.buf(f"F{i}")) for i in range(4)]
        sm = [(self.sb(st, f"sm{i}", [128, 512], F32), self.buf(f"sm{i}")) for i in range(6)]
        smb = [(self.sb(st, f"smb{i}", [128, 512], BF16), self.buf(f"smb{i}")) for i in range(4)]
        stg = [(self.sb(st, f"stg{i}", [128, S], BF16), self.buf(f"stg{i}")) for i in range(2)]
        vst = [(self.sb(st, f"vst{i}", [128, 512], BF16), self.buf(f"vst{i}")) for i in range(3)]
        pv = self.sb(st, "pvec", [128, 16], F32)
        pvb = self.buf("pvec")
        wp = self.sb(st, "wpool", [128, 4, 128], BF16)
        wpb = self.buf("wpool")
        self.load_col(pv[:, 0:1], d["q_norm_g"][l, :], pvb)
        self.load_col(pv[:, 1:2], d["k_norm_g"][l, :], pvb)
        for c in range(4):
            self.load_col(pv[:, 2 + c:3 + c], d["pool_scale"][l, c * 128:(c + 1) * 128], pvb)
        self.cw = self.sb(st, "convw", [128, 12], F32)
        for j in range(3):
            for c in range(4):
                self.load_col(self.cw[:, j * 4 + c:j * 4 + c + 1], d["conv_w"][l, j, c * 128:(c + 1) * 128], pvb)
        self.dma(self.POOL, wp[:], d["w_pool"][l].rearrange("g c d -> c g d"), wpb, writes=[wpb])
        for i in range(4):
            self.vmemset(F[i][0][:, 0:16], 0.0, [F[i][1]])

        units = []
        for c in range(8):
            units.append(("q", c, [(QOFF + c * 128, 128)]))
        for c in range(8):
            units.append(("k", c, [(KOFF + c * 128, 128)]))
        for vb in range(2):
            units.append(("v", vb, [(VOFF + vb * 512, 512)]))
        for c in range(4):
            units.append(("pool", c, [(POFF + c * 128, 128)]))
        for c in range(4):
            units.append(("conv", c, [(BOFF + c * 128, 128), (COFF + c * 128, 128), (UOFF + c * 128, 128)]))

        def load(ui):
            kind, idx, cols = units[ui]
            wt, wb = wsl[ui % 2]
            o = 0
            for (c0, n) in cols:
                self.dma(self.POOL, wt[:, :, o:o + n], W[l, :, c0:c0 + n].rearrange("(c p) n -> p c n", p=128), wb,
                         writes=[wb])
                o += n

        load(0)
        rr = [0]

        def nxt(lst):
            rr[0] += 1
            return lst[rr[0] % len(lst)]

        pcount = [0]

        def psn():
            pcount[0] += 1
            i = pcount[0] % 8
            return self.ps[i], self.psb[i]

        for ui, (kind, idx, cols) in enumerate(units):
            if ui + 1 < len(units):
                load(ui + 1)
            wt, wb = wsl[ui % 2]
            if kind in ("q", "k"):
                gcol = pv[:, 0:1] if kind == "q" else pv[:, 1:2]
                sg, sgb = nxt(stg)
                def emit_main(tb_):
                    pz_, pzb_ = psn()
                    self.mm(pz_[:], [(wt[:, kc, 0:128], hT[:, kc, tb_ * 512:(tb_ + 1) * 512]) for kc in range(16)],
                            [wb, hTb], [pzb_])
                    return pz_, pzb_

                nxt_main = emit_main(0)
                for tb in range(4):
                    tsl = slice(tb * 512, (tb + 1) * 512)
                    pz, pzb = nxt_main
                    if tb + 1 < 4:
                        nxt_main = emit_main(tb + 1)
                    qg, qgb = nxt(smb)
                    sq, sqb = nxt(smb)
                    self.act(qg[:], pz[:], AF.Copy, [pzb, pvb], [qgb], scale=gcol)
                    self.act(sq[:], pz[:], AF.Square, [pzb], [sqb])
                    pss, pssb = psn()
                    self.mm(pss[:], [(g["ones_b"][:], sq[:])], [sqb, cb], [pssb])
                    ppq, ppqb = psn()
                    self.mm(ppq[:], [(g["perm_b"][:], qg[:])], [qgb, cb], [ppqb])
                    rs, rsb = nxt(sm)
                    self.act(rs[:], pss[:], AF.Sqrt, [pssb, cb], [rsb], scale=1.0 / HD, bias=g["eps"][:, 0:1])
                    self.recip(rs[:], rs[:], [rsb], [rsb])
                    t1, t1b = nxt(sm)
                    t2, t2b = nxt(sm)
                    self.tt(t1[:], qg[:], g["C"][:, tsl], ALU.mult, [qgb, cb], [t1b])
                    self.tt(t2[:], ppq[:], g["Sg"][:, tsl], ALU.mult, [ppqb, cb], [t2b])
                    self.tt(t1[:], t1[:], t2[:], ALU.add, [t1b, t2b], [t1b])
                    self.tt(sg[:, tsl], t1[:], rs[:], ALU.mult, [t1b, rsb], [sgb])
                dst = d["qT"] if kind == "q" else d["kT"]
                self.dma(self.SP, dst[idx, :, :], sg[:], sgb, reads=[sgb], writes=[self.scr["qT" if kind == "q" else "kT"]])
            elif kind == "v":
                for tt in range(NT):
                    pz, pzb = psn()
                    self.mm(pz[:], [(hT[:, kc, tt * 128:(tt + 1) * 128], wt[:, kc, 0:512]) for kc in range(16)],
                            [wb, hTb], [pzb])
                    vs, vsb = nxt(vst)
                    if tt % 2 == 0:
                        self.vcopy(vs[:], pz[:], [pzb], [vsb])
                    else:
                        self.acopy(vs[:], pz[:], [pzb], [vsb])
                    self.dma(self.SP, d["V"][tt * 128:(tt + 1) * 128, idx * 512:(idx + 1) * 512], vs[:], vsb,
                             reads=[vsb], writes=[self.scr["V"]])
            elif kind == "pool":
                G, Gb = F[0]
                for tb in range(4):
                    pz, pzb = psn()
                    self.mm(pz[:], [(wt[:, kc, 0:128], hT[:, kc, tb * 512:(tb + 1) * 512]) for kc in range(16)],
                            [wb, hTb], [pzb])
                    self.acopy(G[:, 16 + tb * 512:16 + (tb + 1) * 512], pz[:], [pzb], [Gb])
                w = 2 << idx
                cur, curb = G, Gb
                sh = 1
                pp = 1
                while sh < w:
                    nx, nxb = F[pp]
                    self.tt(nx[:, 16:16 + S], cur[:, 16:16 + S], cur[:, 16 - sh:16 - sh + S], ALU.add, [curb], [nxb])
                    cur, curb = nx, nxb
                    pp = 3 - pp
                    sh *= 2
                po, pob = F[3]
                self.stt(po[:, 16:16 + S], cur[:, 16:16 + S], 1.0 / w, G[:, 16:16 + S], ALU.mult, ALU.subtract,
                         [curb, Gb], [pob])
                self.tt(po[:, 0:16], cur[:, 16:32], g["invc"][:, idx * 16:(idx + 1) * 16], ALU.mult, [curb, cb], [pob])
                self.tt(po[:, 16:32], po[:, 0:16], G[:, 16:32], ALU.subtract, [pob, Gb], [pob])
                pbf, pbfb = nxt(stg)
                self.vcopy(pbf[:], po[:, 16:16 + S], [pob], [pbfb])
                sg, sgb = nxt(stg)
                for tb in range(4):
                    pz, pzb = psn()
                    self.mm(pz[:], [(wp[:, idx, :], pbf[:, tb * 512:(tb + 1) * 512])], [wpb, pbfb], [pzb])
                    self.act(sg[:, tb * 512:(tb + 1) * 512], pz[:], AF.Copy, [pzb, pvb], [sgb],
                             scale=pv[:, 2 + idx:3 + idx])
                self.dma(self.SP, d["mixT"][8 + idx, :, :], sg[:], sgb, reads=[sgb], writes=[self.scr["mixT"]])
            else:
                U, Ub = F[0]
                Bt, Bb = F[1]
                Y, Yb = F[2]
                for tb in range(4):
                    tsl = slice(tb * 512, (tb + 1) * 512)
                    pB, pBb = psn()
                    self.mm(pB[:], [(wt[:, kc, 0:128], hT[:, kc, tsl]) for kc in range(16)], [wb, hTb], [pBb])
                    pC, pCb = psn()
                    self.mm(pC[:], [(wt[:, kc, 128:256], hT[:, kc, tsl]) for kc in range(16)], [wb, hTb], [pCb])
                    pH, pHb = psn()
                    self.mm(pH[:], [(wt[:, kc, 256:384], hT[:, kc, tsl]) for kc in range(16)], [wb, hTb], [pHb])
                    self.acopy(Bt[:, 16 + tb * 512:16 + (tb + 1) * 512], pB[:], [pBb], [Bb])
                    cc, ccb = nxt(sm)
                    self.acopy(cc[:], pC[:], [pCb], [ccb])
                    self.tt(U[:, 16 + tb * 512:16 + (tb + 1) * 512], pH[:], cc[:], ALU.mult, [pHb, ccb], [Ub])
                cw = self.cw
                self.ts(Y[:, 16:16 + S], U[:, 16:16 + S], cw[:, 8 + idx:9 + idx], None, ALU.mult, None, [Ub, pvb], [Yb])
                self.stt(Y[:, 16:16 + S], U[:, 15:15 + S], cw[:, 4 + idx:5 + idx], Y[:, 16:16 + S], ALU.mult, ALU.add,
                         [Ub, pvb, Yb], [Yb])
                self.stt(Y[:, 16:16 + S], U[:, 14:14 + S], cw[:, idx:idx + 1], Y[:, 16:16 + S], ALU.mult, ALU.add,
                         [Ub, pvb, Yb], [Yb])
                sg, sgb = nxt(stg)
                self.tt(sg[:], Y[:, 16:16 + S], Bt[:, 16:16 + S], ALU.mult, [Yb, Bb], [sgb])
                self.dma(self.SP, d["mixT"][12 + idx, :, :], sg[:], sgb, reads=[sgb], writes=[self.scr["mixT"]])

    def phase_attn(self, l):
        nc, g, d = self.nc, self.g, self.d
        cb = self.cb
        lam_init = 0.8 - 0.6 * math.exp(-0.3 * l)
        scale = HD ** -0.5
        with ExitStack() as st:
            lv = self.sb(st, "lv", [128, 16], F32)
            lvb = self.buf("lv")
            for j in range(4):
                self.load_col(lv[:, j:j + 1], d["lambda_vecs"][l, j, :], lvb)
            for c in range(2):
                self.load_col(lv[:, 4 + c:5 + c], d["attn_out_norm_g"][l, c * 128:(c + 1) * 128], lvb)
            self.tt(lv[:, 6:7], lv[:, 0:1], lv[:, 1:2], ALU.mult, [lvb], [lvb])
            self.tt(lv[:, 7:8], lv[:, 2:3], lv[:, 3:4], ALU.mult, [lvb], [lvb])
            p0, p0b = self.ps[0], self.psb[0]
            self.mm(p0[:, 0:2], [(g["ones_f"][:], lv[:, 6:8])], [lvb, cb], [p0b])
            self.act(lv[:, 8:10], p0[:, 0:2], AF.Exp, [p0b], [lvb])
            self.tt(lv[:, 10:11], lv[:, 8:9], lv[:, 9:10], ALU.subtract, [lvb], [lvb])
            self.ts(lv[:, 11:12], lv[:, 10:11], lam_init, -1.0, ALU.add, ALU.mult, [lvb], [lvb])
            self.ts(lv[:, 12:14], lv[:, 4:6], 1.0 - lam_init, None, ALU.mult, None, [lvb], [lvb])

            qh = [(self.sb(st, f"qh{i}", [128, 2, S], BF16), self.buf(f"qh{i}")) for i in range(2)]
            kh = [(self.sb(st, f"kh{i}", [128, 2, S], BF16), self.buf(f"kh{i}")) for i in range(2)]
            vh = [(self.sb(st, f"vh{i}", [128, 16, 256], BF16), self.buf(f"vh{i}")) for i in range(2)]
            pT = [(self.sb(st, f"pT{i}", [128, 512], BF16), self.buf(f"pT{i}")) for i in range(4)]
            rl = [(self.sb(st, f"rl{i}", [128, 512], F32), self.buf(f"rl{i}")) for i in range(2)]
            o1 = [(self.sb(st, f"o1{i}", [128, 512], F32), self.buf(f"o1{i}")) for i in range(2)]
            oo = [(self.sb(st, f"oo{i}", [128, 512], F32), self.buf(f"oo{i}")) for i in range(2)]
            sq = [(self.sb(st, f"sq{i}", [128, 512], BF16), self.buf(f"sq{i}")) for i in range(2)]
            rs, rsb = self.sb(st, "ars", [128, 512], F32), self.buf("ars")
            ast = [(self.sb(st, f"ast{i}", [128, S], BF16), self.buf(f"ast{i}")) for i in range(4)]

            def loadh(h):
                q, qb_ = qh[h % 2]
                k_, kb_ = kh[h % 2]
                v, vb_ = vh[h % 2]
                self.dma(self.SP, q[:], d["qT"][2 * h:2 * h + 2, :, :].rearrange("m p s -> p m s"), qb_,
                         reads=[self.scr["qT"]], writes=[qb_])
                self.dma(self.SP, k_[:], d["kT"][2 * h:2 * h + 2, :, :].rearrange("m p s -> p m s"), kb_,
                         reads=[self.scr["kT"]], writes=[kb_])
                self.dma(self.SP, v[:], d["V"][:, h * 256:(h + 1) * 256].rearrange("(t p) e -> p t e", p=128), vb_,
                         reads=[self.scr["V"]], writes=[vb_])

            loadh(0)
            pcnt = 0
            pending = []
            for h in range(NH):
                if h + 1 < NH:
                    loadh(h + 1)
                q, qb_ = qh[h % 2]
                k_, kb_ = kh[h % 2]
                v, vb_ = vh[h % 2]
                for qb in range(4):
                    q0 = qb * 512
                    nkt = 4 * qb + 4
                    steps = [(m, kt) for m in range(2) for kt in range(nkt)]

                    def emit_sc(si):
                        m, kt = steps[si]
                        c0 = max(kt - 4 * qb, 0) * 128
                        gi = pcnt0 + si
                        sc, scb = self.ps[gi % 2], self.psb[gi % 2]
                        self.mm(sc[:, c0:512], [(k_[:, m, kt * 128:(kt + 1) * 128], q[:, m, q0 + c0:q0 + 512])],
                                [kb_, qb_], [scb])

                    pcnt0 = pcnt
                    emit_sc(0)
                    for si, (m, kt) in enumerate(steps):
                        if si + 1 < len(steps):
                            emit_sc(si + 1)
                        O0, O0b = self.ps[2 + 3 * m], self.psb[2 + 3 * m]
                        O1, O1b = self.ps[3 + 3 * m], self.psb[3 + 3 * m]
                        L, Lb = self.ps[4 + 3 * m], self.psb[4 + 3 * m]
                        j = kt - 4 * qb
                        c0 = max(j, 0) * 128
                        gi = pcnt0 + si
                        sc, scb = self.ps[gi % 2], self.psb[gi % 2]
                        p, pb = pT[gi % 4]
                        self.act(p[:, c0:512], sc[:, c0:512], AF.Exp, [scb], [pb], scale=scale)
                        if j >= 0:
                            self.tt(p[:, c0:c0 + 128], p[:, c0:c0 + 128], g["tri_b"][:], ALU.mult, [pb, cb], [pb])
                        first, last = (kt == 0), (kt == nkt - 1)
                        items = [
                            (O0[:, c0:512], v[:, kt, 0:128], p[:, c0:512], first, last),
                            (O1[:, c0:512], v[:, kt, 128:256], p[:, c0:512], first, last),
                            (L[:, c0:512], g["ones_b"][:], p[:, c0:512], first, last),
                        ]
                        self.mm_multi(items, [vb_, pb, cb], [O0b, O1b, Lb])
                        if si == 1 and pending:
                            pending.pop(0)()
                        if m == 0 and last:
                            self.recip(rl[0][0][:], L[:], [Lb], [rl[0][1]])
                            for ec in range(2):
                                A, Ab = self.ps[2 + ec], self.psb[2 + ec]
                                self.tt(o1[ec][0][:], A[:], rl[0][0][:], ALU.mult, [Ab, rl[0][1]], [o1[ec][1]])
                    pcnt = pcnt0 + len(steps)
                    L, Lb = self.ps[7], self.psb[7]
                    self.recip(rl[1][0][:], L[:], [Lb], [rl[1][1]])
                    for ec in range(2):
                        Bm, Bmb = self.ps[5 + ec], self.psb[5 + ec]
                        self.tt(oo[ec][0][:], Bm[:], rl[1][0][:], ALU.mult, [Bmb, rl[1][1]], [oo[ec][1]])
                        self.stt(oo[ec][0][:], oo[ec][0][:], lv[:, 11:12], o1[ec][0][:], ALU.mult, ALU.add,
                                 [oo[ec][1], o1[ec][1], lvb], [oo[ec][1]])
                        self.act(sq[ec][0][:], oo[ec][0][:], AF.Square, [oo[ec][1]], [sq[ec][1]])

                    def finish(h=h, qb=qb, q0=q0):
                        ssn, ssnb = self.ps[7], self.psb[7]
                        self.mm(ssn[:], [(g["ones_b"][:], sq[0][0][:]), (g["ones_b"][:], sq[1][0][:])],
                                [sq[0][1], sq[1][1], cb], [ssnb])
                        self.act(rs[:], ssn[:], AF.Sqrt, [ssnb, cb], [rsb], scale=1.0 / (2 * HD), bias=g["eps"][:, 0:1])
                        self.recip(rs[:], rs[:], [rsb], [rsb])
                        for ec in range(2):
                            a, ab = ast[(h % 2) * 2 + ec]
                            self.stt(a[:, q0:q0 + 512], oo[ec][0][:], lv[:, 12 + ec:13 + ec], rs[:], ALU.mult, ALU.mult,
                                     [oo[ec][1], rsb, lvb], [ab])
                        if qb == 3:
                            for ec in range(2):
                                a, ab = ast[(h % 2) * 2 + ec]
                                self.dma(self.SP, d["mixT"][2 * h + ec, :, :], a[:], ab, reads=[ab],
                                         writes=[self.scr["mixT"]])

                    pending.append(finish)
            while pending:
                pending.pop(0)()

    def phase_outproj(self, l, src):
        nc, g, d = self.nc, self.g, self.d
        with ExitStack() as st:
            mix = self.sb(st, "mix", [128, 16, S], BF16)
            mixb = self.buf("mix")
            for c4 in range(4):
                self.dma(self.SP, mix[:, c4 * 4:(c4 + 1) * 4, :], d["mixT"][c4 * 4:(c4 + 1) * 4, :, :].rearrange("c p s -> p c s"),
                         mixb, reads=[self.scr["mixT"]], writes=[mixb])
            wsl = [(self.sb(st, f"wo{i}", [128, 16, 512], BF16), self.buf(f"wo{i}")) for i in range(2)]
            xts = [(self.sb(st, f"xo{i}", [128, 512], F32), self.buf(f"xo{i}")) for i in range(4)]

            def loadw(db):
                wt, wb = wsl[db % 2]
                self.dma(self.POOL, wt[:], d["w_out"][l, :, db * 512:(db + 1) * 512].rearrange("(c p) n -> p c n", p=128),
                         wb, writes=[wb])

            units = [(db, tt) for db in range(4) for tt in range(NT)]

            def loadx(ui):
                db, tt = units[ui]
                xt, xb = xts[ui % 4]
                rd = [self.outb[tt]] if l > 0 else []
                self.dma(self.SP, xt[:], src[tt * 128:(tt + 1) * 128, db * 512:(db + 1) * 512], xb, reads=rd, writes=[xb])

            loadw(0)
            loadx(0)
            loadx(1)
            for ui, (db, tt) in enumerate(units):
                if tt == 0 and db + 1 < 4:
                    loadw(db + 1)
                if ui + 2 < len(units):
                    loadx(ui + 2)
                wt, wb = wsl[db % 2]
                xt, xb = xts[ui % 4]
                pz, pzb = self.ps[ui % 4], self.psb[ui % 4]
                self.mm(pz[:], [(mix[:, kc, tt * 128:(tt + 1) * 128], wt[:, kc, :]) for kc in range(16)], [mixb, wb], [pzb])
                self.tt(xt[:], xt[:], pz[:], ALU.add, [xb, pzb], [xb])
                self.dma(self.SP, d["out"][tt * 128:(tt + 1) * 128, db * 512:(db + 1) * 512], xt[:], xb, reads=[xb],
                         writes=[self.outb[tt]])

    def phase_ffn(self, l):
        nc, g, d = self.nc, self.g, self.d
        cb = self.cb
        moe = (l % 2 == 1)
        j = l // 2
        ne = NE if moe else 1
        ff = FF_E if moe else FF_D
        nfb = ff // 256
        if self.debug and isinstance(self.debug, dict):
            ne = min(ne, self.debug.get("ne", ne))
            nfb = min(nfb, self.debug.get("nfb", nfb))
        with ExitStack() as st:
            gB = self.sb(st, "gBf", [128, D], F32)
            gBb = self.buf("gBf")
            self.dma(self.SP, gB[:], d["ffn_norm_g"][l:l + 1, :].partition_broadcast(128), gBb, writes=[gBb])
            acc = [(self.sb(st, f"acc{i}", [128, D], F32), self.buf(f"acc{i}")) for i in range(8)]
            hT = self.sb(st, "h2T", [128, 16, 1024], BF16)
            hTb = self.buf("h2T")
            wg = [(self.sb(st, f"wg{i}", [128, 16, 256], BF16), self.buf(f"wg{i}")) for i in range(2)]
            wu = [(self.sb(st, f"wu{i}", [128, 16, 256], BF16), self.buf(f"wu{i}")) for i in range(2)]
            wd = [(self.sb(st, f"wd{i}", [128, 2, D], BF16), self.buf(f"wd{i}")) for i in range(2)]
            sg = [(self.sb(st, f"sg{i}", [128, 512], F32), self.buf(f"sg{i}")) for i in range(2)]
            at = [(self.sb(st, f"at{i}", [128, 512], BF16), self.buf(f"at{i}")) for i in range(4)]
            tmp = {
                "ssq": [(self.sb(st, f"ssqf{i}", [128, 4], F32), self.buf(f"ssqf{i}")) for i in range(2)],
            }
            route = None
            use_route = moe and not (isinstance(self.debug, dict) and self.debug.get("noroute"))
            if not use_route and moe:
                tmp["xs"] = [(self.sb(st, f"xsf{i}", [128, D], BF16), self.buf(f"xsf{i}")) for i in range(2)]
            if use_route:
                tmp["xs"] = [(self.sb(st, f"xsf{i}", [128, D], F32), self.buf(f"xsf{i}")) for i in range(1)] * 2
                route = {
                    "h32": [(self.sb(st, f"h32{i}", [128, 4, 128], F32), self.buf(f"h32{i}")) for i in range(2)],
                    "rw": self.sb(st, "rw", [128, 16, NE], F32),
                    "rwb": self.buf("rw"),
                    "lg": [(self.sb(st, f"lg{i}", [128, 40], F32), self.buf(f"lg{i}")) for i in range(2)],
                    "comb": self.sb(st, "comb", [128, 8, NE], F32),
                    "combb": self.buf("comb"),
                }
                for c in range(16):
                    self.dma(self.POOL, route["rw"][:, c, :], d["router_w"][j, c * 128:(c + 1) * 128, :], route["rwb"],
                             writes=[route["rwb"]])
                if isinstance(self.debug, dict):
                    route["level"] = self.debug.get("level", 3)
            else:
                tmp["xs"] = [(self.sb(st, f"xsf{i}", [128, D], BF16), self.buf(f"xsf{i}")) for i in range(2)]

            if moe:
                Wg = lambda e, fb: d["moe_w_gate"][j, e, :, fb * 256:(fb + 1) * 256]
                Wu = lambda e, fb: d["moe_w_up"][j, e, :, fb * 256:(fb + 1) * 256]
                Wd = lambda e, fb: d["moe_w_down"][j, e, fb * 256:(fb + 1) * 256, :]
            else:
                Wg = lambda e, fb: d["dense_w_gate"][j, :, fb * 256:(fb + 1) * 256]
                Wu = lambda e, fb: d["dense_w_up"][j, :, fb * 256:(fb + 1) * 256]
                Wd = lambda e, fb: d["dense_w_down"][j, fb * 256:(fb + 1) * 256, :]

            for half in range(2):
                units = [(e, fb) for e in range(ne) for fb in range(nfb)]

                def load_gu(ui):
                    if ui >= len(units):
                        return
                    e, fb = units[ui]
                    s_ = ui % 2
                    self.dma(self.POOL, wg[s_][0][:], Wg(e, fb).rearrange("(c p) n -> p c n", p=128), wg[s_][1],
                             writes=[wg[s_][1]])
                    self.dma(self.POOL, wu[s_][0][:], Wu(e, fb).rearrange("(c p) n -> p c n", p=128), wu[s_][1],
                             writes=[wu[s_][1]])

                def load_d(ui):
                    if ui >= len(units):
                        return
                    e, fb = units[ui]
                    s_ = ui % 2
                    self.dma(self.POOL, wd[s_][0][:], Wd(e, fb).rearrange("(c p) n -> p c n", p=128), wd[s_][1],
                             writes=[wd[s_][1]])

                load_gu(0)
                load_d(0)
                load_gu(1)
                load_d(1)
                for i in range(8):
                    tt = half * 8 + i
                    self.dma(self.SP, acc[i][0][:], d["out"][tt * 128:(tt + 1) * 128, :], acc[i][1],
                             reads=[self.outb[tt]], writes=[acc[i][1]])
                for i in range(8):
                    self.norm_tile(acc[i][0][:], acc[i][1], gB, gBb, hT, hTb, i * 128, tmp, i,
                                   g["ident_f"][:] if use_route else g["ident_b"][:], route=route)
                steps = [(ui, tb) for ui in range(len(units)) for tb in range(2)]
                acts_of = {}

                def gu_part(si, q):
                    ui, tb = steps[si]
                    s_ = ui % 2
                    c = q // 2
                    tsl = slice(tb * 512, (tb + 1) * 512)
                    sgt, sgb_ = sg[c]
                    if q % 2 == 0:
                        wgt, wgb = wg[s_]
                        pg, pgb = self.ps[c], self.psb[c]
                        self.mm(pg[:], [(wgt[:, kc, c * 128:(c + 1) * 128], hT[:, kc, tsl]) for kc in range(16)],
                                [wgb, hTb], [pgb])
                        self.act(sgt[:], pg[:], AF.Silu, [pgb], [sgb_])
                    else:
                        wut, wub = wu[s_]
                        pu, pub = self.ps[2 + c], self.psb[2 + c]
                        self.mm(pu[:], [(wut[:, kc, c * 128:(c + 1) * 128], hT[:, kc, tsl]) for kc in range(16)],
                                [wub, hTb], [pub])
                        a, ab = at[(tb * 2 + c) % 4]
                        self.tt(a[:], pu[:], sgt[:], ALU.mult, [pub, sgb_], [ab])
                        acts_of.setdefault(si, []).append((a, ab))
                        if q == 3 and tb == 1:
                            load_gu(ui + 2)

                pcs = [0]

                def down_group(si, gidx):
                    ui, tb = steps[si]
                    e, fb = units[ui]
                    wdt, wdb = wd[ui % 2]
                    acts = acts_of[si]
                    t4, dh = gidx // 2, gidx % 2
                    ti = tb * 4 + t4
                    pi = 4 + 2 * (pcs[0] % 2)
                    pcs[0] += 1
                    items = []
                    for db in range(2):
                        for c in range(2):
                            items.append((self.ps[pi + db][:], acts[c][0][:, t4 * 128:(t4 + 1) * 128],
                                          wdt[:, c, dh * 1024 + db * 512:dh * 1024 + (db + 1) * 512],
                                          c == 0, c == 1))
                    self.mm_multi(items, [acts[0][1], acts[1][1], wdb], [self.psb[pi], self.psb[pi + 1]])
                    for db in range(2):
                        dsl = slice(dh * 1024 + db * 512, dh * 1024 + (db + 1) * 512)
                        a_t, a_b = acc[ti]
                        if use_route and route.get("level", 3) >= 3:
                            self.stt(a_t[:, dsl], self.ps[pi + db][:], route["comb"][:, ti, e:e + 1],
                                     a_t[:, dsl], ALU.mult, ALU.add,
                                     [self.psb[pi + db], route["combb"], a_b], [a_b])
                        else:
                            self.tt(a_t[:, dsl], self.ps[pi + db][:], a_t[:, dsl], ALU.add,
                                    [self.psb[pi + db], a_b], [a_b])

                for q in range(4):
                    gu_part(0, q)
                for si, (ui, tb) in enumerate(steps):
                    for q in range(4):
                        if si + 1 < len(steps):
                            gu_part(si + 1, q)
                        down_group(si, 2 * q)
                        down_group(si, 2 * q + 1)
                    acts_of.pop(si)
                    if tb == 1:
                        load_d(ui + 2)
                for i in range(8):
                    tt = half * 8 + i
                    self.dma(self.SP, d["out"][tt * 128:(tt + 1) * 128, :], acc[i][0][:], acc[i][1], reads=[acc[i][1]],
                             writes=[self.outb[tt]])
                self.barrier()


def _consts():
    ident = np.eye(128, dtype=np.float32)
    perm = np.zeros((128, 128), np.float32)
    for p in range(128):
        perm[p, (p + 64) % 128] = 1.0
    tri = (np.arange(128)[:, None] <= np.arange(128)[None, :]).astype(np.float32)
    jj = np.arange(0, HD, 2, dtype=np.float32) / np.float32(HD)
    invf64 = (1.0 / (np.float32(10000.0) ** jj)).astype(np.float32)
    invf = np.concatenate([invf64, invf64]).reshape(128, 1).astype(np.float32)
    invc = np.zeros((128, 64), np.float32)
    for gi, w in enumerate((2, 4, 8, 16)):
        invc[:, gi * 16:(gi + 1) * 16] = 1.0 / np.minimum(np.arange(16) + 1, w).astype(np.float32)
    return {"c_ident": ident, "c_perm": perm, "c_tri": tri, "c_invf": invf, "c_invc": invc}


_WNAMES = ["attn_norm_g", "w_in", "q_norm_g", "k_norm_g", "lambda_vecs", "attn_out_norm_g", "w_pool", "pool_scale",
           "conv_w", "w_out", "ffn_norm_g", "dense_w_gate", "dense_w_up", "dense_w_down", "router_w", "moe_w_gate",
           "moe_w_up", "moe_w_down"]


def make_in_maps(inputs, n_cores=N_CORES):
    consts = _consts()
    shared = {n: np.ascontiguousarray(np.asarray(inputs[n], dtype=np.float32)) for n in _WNAMES}
    shared.update(consts)
    x = np.asarray(inputs["x"], dtype=np.float32)
    pos = np.asarray(inputs["positions"], dtype=np.int32)
    maps = []
    for c in range(n_cores):
        m = dict(shared)
        m["x"] = np.ascontiguousarray(x[c])
        m["pos"] = np.ascontiguousarray(pos[c].reshape(1, S))
        maps.append(m)
    return maps


def kernel(**inputs):
    k = K()
    nc = k.build()
    in_maps = make_in_maps(inputs)
    res = run_bass_kernel_spmd(nc, in_maps, core_ids=list(range(N_CORES)))
    return np.stack([np.asarray(r["out"], dtype=np.float32) for r in res.results], axis=0)
```
